# Optimizing a Trainium2 kernel written in Bass

```python
import numpy as np
import jax
import jax.numpy as jnp
from jax import lax

D_MODEL = 1024
BATCH = 8
SEQ = 2048
DEPTH = 2

GRID_W = 64
CTX_LEN = 256
Q_BLOCK = 128
ROPE_THETA = 10000.0
NORM_EPS = 1e-6
N_MOD = 6

GQA_HEADS = 8
GQA_KV_HEADS = 2
GQA_GROUP = GQA_HEADS // GQA_KV_HEADS
GQA_HEAD_DIM = 64
GQA_WIDTH = GQA_HEADS * GQA_HEAD_DIM

LRU_WIDTH = 512
LRU_BLOCKS = 8
LRU_BLOCK_W = LRU_WIDTH // LRU_BLOCKS
CONV_WIDTH = 4
CONV_PAD = (1, 2)
LRU_C = 8.0

MLA_HEADS = 8
MLA_NOPE = 64
MLA_ROPE = 32
MLA_V = 64
MLA_Q_LORA = 384
MLA_KV_LORA = 256
MLA_WIDTH = MLA_HEADS * MLA_V

N_BRANCHES = 3
IN_WIDTHS = (GQA_WIDTH, GQA_KV_HEADS * GQA_HEAD_DIM, GQA_KV_HEADS * GQA_HEAD_DIM, LRU_WIDTH, LRU_WIDTH,
             MLA_Q_LORA, MLA_KV_LORA, MLA_ROPE, N_BRANCHES * D_MODEL)
IN_WIDTH = sum(IN_WIDTHS)

N_EXPERTS = 16
N_GROUPS = 4
EXPERTS_PER_GROUP = N_EXPERTS // N_GROUPS
TOPK_GROUPS = 1
GROUP_SCORE_TOPK = 2
TOP_K = 2
EXPERT_FF = 512
ROUTED_SCALE = 1.0

kernel_name = 'hybrid_gqa_rglru_mla_moe_prefix'


def rms_norm(x, g):
    xf = x.astype(jnp.float32)
    y = xf * lax.rsqrt(jnp.mean(xf * xf, axis=-1, keepdims=True) + NORM_EPS)
    return (y * g.astype(jnp.float32)).astype(x.dtype)


def modulate(h, shift, scale):
    return h * (1 + scale) + shift


def axial_rope_tables(rows, dim):
    r, col = jnp.meshgrid(jnp.arange(rows, dtype=jnp.float32), jnp.arange(GRID_W, dtype=jnp.float32), indexing='ij')
    quarter = dim // 4
    inv_freq = ROPE_THETA ** (-jnp.arange(quarter, dtype=jnp.float32) / quarter)
    ang_r = r.reshape(-1, 1) * inv_freq
    ang_c = col.reshape(-1, 1) * inv_freq
    return (jnp.cos(ang_r), jnp.sin(ang_r), jnp.cos(ang_c), jnp.sin(ang_c))


def _rotate(x, cos, sin):
    x1, x2 = jnp.split(x, 2, axis=-1)
    cos = cos[None, :, None, :].astype(x.dtype)
    sin = sin[None, :, None, :].astype(x.dtype)
    return jnp.concatenate([x1 * cos - x2 * sin, x2 * cos + x1 * sin], axis=-1)


def apply_axial_rope(x, tables):
    cr, sr, cc, sc = tables
    x_row, x_col = jnp.split(x, 2, axis=-1)
    return jnp.concatenate([_rotate(x_row, cr, sr), _rotate(x_col, cc, sc)], axis=-1)


def block_attention(q, k, v, scale):
    b, sq, hkv, g, dk = q.shape
    nb = sq // Q_BLOCK
    qb = jnp.moveaxis(q.reshape(b, nb, Q_BLOCK, hkv, g, dk), 1, 0)

    def one_block(qblk):
        s = jnp.einsum('bqhgd,bkhd->bhgqk', qblk, k, preferred_element_type=jnp.float32) * scale
        p = jax.nn.softmax(s, axis=-1).astype(v.dtype)
        return jnp.einsum('bhgqk,bkhe->bqhge', p, v)

    o = lax.map(one_block, qb)
    return jnp.moveaxis(o, 0, 1).reshape(b, sq, -1)


def centred_dwconv(x, w, b):
    y = lax.conv_general_dilated(x, w[:, None, :].astype(x.dtype), window_strides=(1,), padding=[CONV_PAD],
                                 dimension_numbers=('NWC', 'WIO', 'NWC'), feature_group_count=x.shape[-1])
    return y + b


def rglru_coeffs(u, w_a, b_a, w_i, b_i, lam):
    bsz, t, width = u.shape
    uf = u.astype(jnp.float32)
    ub = uf.reshape(bsz, t, LRU_BLOCKS, LRU_BLOCK_W)
    r = jax.nn.sigmoid(jnp.einsum('btnk,nkj->btnj', ub, w_a.astype(jnp.float32)).reshape(bsz, t, width)
                       + b_a.astype(jnp.float32))
    i = jax.nn.sigmoid(jnp.einsum('btnk,nkj->btnj', ub, w_i.astype(jnp.float32)).reshape(bsz, t, width)
                       + b_i.astype(jnp.float32))
    log_a = -LRU_C * r * jax.nn.softplus(-lam.astype(jnp.float32))
    a = jnp.exp(log_a)
    gated_x = jnp.sqrt(-jnp.expm1(2.0 * log_a)) * (i * uf)
    return a, gated_x


def _scan_combine(left, right):
    a_l, b_l = left
    a_r, b_r = right
    return a_l * a_r, a_r * b_l + b_r


def linear_recurrence(a, b, h0, reverse):
    edge = -1 if reverse else 0
    b = b.at[:, edge].add(a[:, edge] * h0)
    _, h = lax.associative_scan(_scan_combine, (a, b), reverse=reverse, axis=1)
    return h


def token_mixer(hc, hl, need_ctx, rope_gqa, rope_mla, w_in, gqa_q_norm, gqa_k_norm, conv_w, conv_b,
                lru_w_a, lru_b_a, lru_w_i, lru_b_i, lru_lam, mla_q_a_norm, mla_w_qb, mla_kv_a_norm, mla_w_kvb,
                w_branch_attn, w_branch_lru, w_branch_mla, w_out):
    bsz, n_ctx, _ = hc.shape
    n_lat = hl.shape[1]
    h = jnp.concatenate([hc, hl], axis=1)
    z = h @ w_in
    zq, zk, zv, zx, zy, zcq, zckv, zkr, zg = jnp.split(z, np.cumsum(IN_WIDTHS)[:-1].tolist(), axis=-1)
    q0 = 0 if need_ctx else n_ctx
    lq = n_ctx - q0
    nq = n_ctx + n_lat - q0

    q = rms_norm(zq[:, q0:].reshape(bsz, nq, GQA_HEADS, GQA_HEAD_DIM), gqa_q_norm)
    k = rms_norm(zk.reshape(bsz, -1, GQA_KV_HEADS, GQA_HEAD_DIM), gqa_k_norm)
    v = zv.reshape(bsz, -1, GQA_KV_HEADS, GQA_HEAD_DIM)
    k = jnp.concatenate([k[:, :n_ctx], apply_axial_rope(k[:, n_ctx:], rope_gqa)], axis=1)
    q_lat = apply_axial_rope(q[:, lq:], rope_gqa).reshape(bsz, n_lat, GQA_KV_HEADS, GQA_GROUP, GQA_HEAD_DIM)
    gqa_scale = GQA_HEAD_DIM ** -0.5
    o_attn = block_attention(q_lat, k, v, gqa_scale)
    if need_ctx:
        q_ctx = q[:, :lq].reshape(bsz, n_ctx, GQA_KV_HEADS, GQA_GROUP, GQA_HEAD_DIM)
        o_attn = jnp.concatenate([block_attention(q_ctx, k[:, :n_ctx], v[:, :n_ctx], gqa_scale), o_attn], axis=1)

    u_ctx = centred_dwconv(zx[:, :n_ctx], conv_w, conv_b)
    u_lat = centred_dwconv(zx[:, n_ctx:], conv_w, conv_b)
    lat_states = []
    ctx_states = []
    for d, reverse in enumerate((False, True)):
        params = (lru_w_a[d], lru_b_a[d], lru_w_i[d], lru_b_i[d], lru_lam[d])
        a_c, x_c = rglru_coeffs(u_ctx, *params)
        hs_c = linear_recurrence(a_c, x_c, jnp.zeros_like(x_c[:, 0]), reverse)
        h_end = hs_c[:, 0] if reverse else hs_c[:, -1]
        a_l, x_l = rglru_coeffs(u_lat, *params)
        lat_states.append(linear_recurrence(a_l, x_l, h_end, reverse))
        if need_ctx:
            ctx_states.append(hs_c)
    rec = lat_states[0] + lat_states[1]
    if need_ctx:
        rec = jnp.concatenate([ctx_states[0] + ctx_states[1], rec], axis=1)
    o_lru = rec.astype(h.dtype) * jax.nn.gelu(zy[:, q0:], approximate=True)

    cq = rms_norm(zcq[:, q0:], mla_q_a_norm)
    qm = (cq @ mla_w_qb).reshape(bsz, nq, MLA_HEADS, MLA_NOPE + MLA_ROPE)
    ckv = rms_norm(zckv, mla_kv_a_norm)
    kv = (ckv @ mla_w_kvb).reshape(bsz, -1, MLA_HEADS, MLA_NOPE + MLA_V)
    k_nope, vm = jnp.split(kv, [MLA_NOPE], axis=-1)
    k_rope = zkr[:, :, None, :]
    k_rope = jnp.concatenate([k_rope[:, :n_ctx], apply_axial_rope(k_rope[:, n_ctx:], rope_mla)], axis=1)
    km = jnp.concatenate([k_nope, jnp.broadcast_to(k_rope, k_nope.shape[:-1] + (MLA_ROPE,))], axis=-1)
    q_nope, q_rope = jnp.split(qm, [MLA_NOPE], axis=-1)
    q_lat_m = jnp.concatenate([q_nope[:, lq:], apply_axial_rope(q_rope[:, lq:], rope_mla)], axis=-1)
    mla_scale = (MLA_NOPE + MLA_ROPE) ** -0.5
    o_mla = block_attention(q_lat_m[:, :, :, None, :], km, vm, mla_scale)
    if need_ctx:
        o_mla_ctx = block_attention(qm[:, :lq][:, :, :, None, :], km[:, :n_ctx], vm[:, :n_ctx], mla_scale)
        o_mla = jnp.concatenate([o_mla_ctx, o_mla], axis=1)

    g_attn, g_lru, g_mla = jnp.split(jax.nn.sigmoid(zg[:, q0:]), N_BRANCHES, axis=-1)
    merged = (g_attn * (o_attn @ w_branch_attn) + g_lru * (o_lru @ w_branch_lru)
              + g_mla * (o_mla @ w_branch_mla))
    y = merged @ w_out
    return y[:, :lq], y[:, lq:]


def moe_ffn(h, router_w, router_bias, w_gate, w_up, w_down):
    shape = h.shape
    t = h.reshape(-1, shape[-1])
    scores = jax.nn.sigmoid(jnp.matmul(t, router_w, preferred_element_type=jnp.float32))
    sel = scores + router_bias.astype(jnp.float32)
    grp = sel.reshape(-1, N_GROUPS, EXPERTS_PER_GROUP)
    group_score = jnp.sum(lax.top_k(grp, GROUP_SCORE_TOPK)[0], axis=-1)
    _, g_idx = lax.top_k(group_score, TOPK_GROUPS)
    group_mask = jnp.sum(jax.nn.one_hot(g_idx, N_GROUPS, dtype=jnp.float32), axis=1)
    expert_mask = jnp.repeat(group_mask, EXPERTS_PER_GROUP, axis=-1)
    masked = jnp.where(expert_mask > 0, sel, -jnp.inf)
    _, e_idx = lax.top_k(masked, TOP_K)
    w = jnp.take_along_axis(scores, e_idx, axis=-1)
    w = ROUTED_SCALE * w / jnp.sum(w, axis=-1, keepdims=True)
    combine = jnp.sum(jax.nn.one_hot(e_idx, N_EXPERTS, dtype=jnp.float32) * w[..., None], axis=1).astype(h.dtype)
    gate = jnp.einsum('nd,edf->nef', t, w_gate)
    up = jnp.einsum('nd,edf->nef', t, w_up)
    act = jax.nn.silu(gate) * up * combine[..., None]
    y = jnp.einsum('nef,efd->nd', act, w_down)
    return y.reshape(shape)


def setup_inputs(seed: int = 0) -> dict:
    key = jax.random.key(seed)
    ks = iter(jax.random.split(key, 40))
    d = D_MODEL

    def nrm(shape, scale):
        return jax.random.normal(next(ks), shape, jnp.float32) * scale

    x = nrm((BATCH, SEQ, d), 1.0)
    c = nrm((BATCH, d), 1.0)
    ctx = nrm((BATCH, CTX_LEN, d), 1.0)
    c_ctx = nrm((d,), 1.0)
    w_mod = nrm((DEPTH, d, N_MOD * d), 0.5 * d ** -0.5)
    b_mod = nrm((DEPTH, N_MOD * d), 0.02)
    norm_mix = 1.0 + nrm((DEPTH, d), 0.05)
    norm_ffn = 1.0 + nrm((DEPTH, d), 0.05)
    w_in = nrm((DEPTH, d, IN_WIDTH), d ** -0.5)
    gqa_q_norm = 1.0 + nrm((DEPTH, GQA_HEAD_DIM), 0.05)
    gqa_k_norm = 1.0 + nrm((DEPTH, GQA_HEAD_DIM), 0.05)
    conv_w = nrm((DEPTH, CONV_WIDTH, LRU_WIDTH), CONV_WIDTH ** -0.5)
    conv_b = nrm((DEPTH, LRU_WIDTH), 0.02)
    lru_w_a = nrm((DEPTH, 2, LRU_BLOCKS, LRU_BLOCK_W, LRU_BLOCK_W), LRU_BLOCK_W ** -0.5)
    lru_b_a = nrm((DEPTH, 2, LRU_WIDTH), 0.02)
    lru_w_i = nrm((DEPTH, 2, LRU_BLOCKS, LRU_BLOCK_W, LRU_BLOCK_W), LRU_BLOCK_W ** -0.5)
    lru_b_i = nrm((DEPTH, 2, LRU_WIDTH), 0.02)
    a8 = jax.random.uniform(next(ks), (DEPTH, 2, LRU_WIDTH), jnp.float32, minval=0.9, maxval=0.999)
    s = a8 ** 0.125
    lru_lam = jnp.log(s) - jnp.log1p(-s)
    mla_q_a_norm = 1.0 + nrm((DEPTH, MLA_Q_LORA), 0.05)
    mla_w_qb = nrm((DEPTH, MLA_Q_LORA, MLA_HEADS * (MLA_NOPE + MLA_ROPE)), MLA_Q_LORA ** -0.5)
    mla_kv_a_norm = 1.0 + nrm((DEPTH, MLA_KV_LORA), 0.05)
    mla_w_kvb = nrm((DEPTH, MLA_KV_LORA, MLA_HEADS * (MLA_NOPE + MLA_V)), MLA_KV_LORA ** -0.5)
    w_branch_attn = nrm((DEPTH, GQA_WIDTH, d), GQA_WIDTH ** -0.5)
    w_branch_lru = nrm((DEPTH, LRU_WIDTH, d), LRU_WIDTH ** -0.5)
    w_branch_mla = nrm((DEPTH, MLA_WIDTH, d), MLA_WIDTH ** -0.5)
    w_out = nrm((DEPTH, d, d), d ** -0.5)
    router_w = nrm((d, N_EXPERTS), d ** -0.5)
    router_bias = nrm((N_EXPERTS,), 0.01)
    moe_w_gate = nrm((DEPTH, N_EXPERTS, d, EXPERT_FF), d ** -0.5)
    moe_w_up = nrm((DEPTH, N_EXPERTS, d, EXPERT_FF), d ** -0.5)
    moe_w_down = nrm((DEPTH, N_EXPERTS, EXPERT_FF, d), EXPERT_FF ** -0.5)
    final_norm = 1.0 + nrm((d,), 0.05)
    return {'x': x, 'c': c, 'ctx': ctx, 'c_ctx': c_ctx, 'w_mod': w_mod, 'b_mod': b_mod,
            'norm_mix': norm_mix, 'norm_ffn': norm_ffn, 'w_in': w_in,
            'gqa_q_norm': gqa_q_norm, 'gqa_k_norm': gqa_k_norm, 'conv_w': conv_w, 'conv_b': conv_b,
            'lru_w_a': lru_w_a, 'lru_b_a': lru_b_a, 'lru_w_i': lru_w_i, 'lru_b_i': lru_b_i, 'lru_lam': lru_lam,
            'mla_q_a_norm': mla_q_a_norm, 'mla_w_qb': mla_w_qb, 'mla_kv_a_norm': mla_kv_a_norm,
            'mla_w_kvb': mla_w_kvb, 'w_branch_attn': w_branch_attn, 'w_branch_lru': w_branch_lru,
            'w_branch_mla': w_branch_mla, 'w_out': w_out, 'router_w': router_w, 'router_bias': router_bias,
            'moe_w_gate': moe_w_gate, 'moe_w_up': moe_w_up, 'moe_w_down': moe_w_down, 'final_norm': final_norm}


def reference(x, c, ctx, c_ctx, w_mod, b_mod, norm_mix, norm_ffn, w_in, gqa_q_norm, gqa_k_norm, conv_w, conv_b,
              lru_w_a, lru_b_a, lru_w_i, lru_b_i, lru_lam, mla_q_a_norm, mla_w_qb, mla_kv_a_norm, mla_w_kvb,
              w_branch_attn, w_branch_lru, w_branch_mla, w_out, router_w, router_bias,
              moe_w_gate, moe_w_up, moe_w_down, final_norm):
    rows = x.shape[1] // GRID_W
    rope_gqa = axial_rope_tables(rows, GQA_HEAD_DIM)
    rope_mla = axial_rope_tables(rows, MLA_ROPE)
    act_c = jax.nn.silu(c)[:, None, :]
    act_cc = jax.nn.silu(c_ctx)[None, None, :]
    n_ctx = ctx.shape[1]
    xc, xl = ctx, x
    for layer in range(DEPTH):
        need_ctx = layer < DEPTH - 1
        mod_l = jnp.split(act_c @ w_mod[layer] + b_mod[layer], N_MOD, axis=-1)
        mod_c = jnp.split(act_cc @ w_mod[layer] + b_mod[layer], N_MOD, axis=-1)
        hc = modulate(rms_norm(xc, norm_mix[layer]), mod_c[0], mod_c[1])
        hl = modulate(rms_norm(xl, norm_mix[layer]), mod_l[0], mod_l[1])
        yc, yl = token_mixer(hc, hl, need_ctx, rope_gqa, rope_mla, w_in[layer], gqa_q_norm[layer],
                             gqa_k_norm[layer], conv_w[layer], conv_b[layer], lru_w_a[layer], lru_b_a[layer],
                             lru_w_i[layer], lru_b_i[layer], lru_lam[layer], mla_q_a_norm[layer],
                             mla_w_qb[layer], mla_kv_a_norm[layer], mla_w_kvb[layer], w_branch_attn[layer],
                             w_branch_lru[layer], w_branch_mla[layer], w_out[layer])
        xl = xl + mod_l[2] * yl
        hl = modulate(rms_norm(xl, norm_ffn[layer]), mod_l[3], mod_l[4])
        if need_ctx:
            xc = xc + mod_c[2] * yc
            hc = modulate(rms_norm(xc, norm_ffn[layer]), mod_c[3], mod_c[4])
            f = moe_ffn(jnp.concatenate([hc, hl], axis=1), router_w, router_bias,
                        moe_w_gate[layer], moe_w_up[layer], moe_w_down[layer])
            xc = xc + mod_c[5] * f[:, :n_ctx]
            xl = xl + mod_l[5] * f[:, n_ctx:]
        else:
            xl = xl + mod_l[5] * moe_ffn(hl, router_w, router_bias, moe_w_gate[layer], moe_w_up[layer],
                                         moe_w_down[layer])
    return rms_norm(xl, final_norm)
```

```python
import numpy as np
from contextlib import ExitStack
import concourse.bass as bass
import concourse.mybir as mybir
from concourse.bass_utils import run_bass_kernel_spmd
F32 = mybir.dt.float32
BF16 = mybir.dt.bfloat16
AF = mybir.ActivationFunctionType
ALU = mybir.AluOpType
AX = mybir.AxisListType
D = 1024
KC = 8
NCTX = 256
SEQ = 2048
T = NCTX + SEQ
NT = T // 128
DEPTH = 2
BLOCKS = [(0, 256), (256, 512), (768, 512), (1280, 512), (1792, 512)]
IN_WIDTH = 5536
C_Q, C_K, C_V, C_ZX, C_ZY, C_CQ, C_CKV, C_KR, C_G = 0, 512, 640, 768, 1280, 1792, 2176, 2432, 2464
EPS = 1e-6
NE = 16
import os
NLOADS = int(os.environ.get("K_NLOADS", "1"))

class Trk:
    __slots__ = ("name", "w", "r", "sem")
    def __init__(self, name=""):
        self.name = name
        self.w = None
        self.r = {}
        self.sem = None

class FW:
    def __init__(self, nc, stack):
        self.nc = nc
        self.stack = stack
        self.engs = ("pe", "act", "dve", "pool", "sp")
        self.prog = {e: [] for e in self.engs}
        self.sem = {}
        self.cnt = {}
        for e in ("pe", "act", "dve", "pool"):
            self.sem[e] = stack.enter_context(nc.semaphore("s_" + e))
            self.cnt[e] = 0
        self.known = {e: {} for e in self.engs}
        self.dsems = []
        self.ninst = 0
        self.nwait = 0
    def sb(self, name, shape, dt):
        return self.stack.enter_context(self.nc.sbuf_tensor(name, list(shape), dt))
    def ps(self, name, shape, dt=F32):
        return self.stack.enter_context(self.nc.psum_tensor(name, list(shape), dt))
    def _wait(self, e, tok):
        if tok is None:
            return
        key, semh, val = tok
        if key == e and e == "pe":
            return
        kn = self.known[e]
        if kn.get(key, 0) >= val:
            return
        self.prog[e].append(lambda eng, s=semh, v=val: eng.wait_ge(s, v))
        kn[key] = val
        self.nwait += 1
    def _deps(self, e, R, W):
        for t in R:
            self._wait(e, t.w)
        for t in W:
            self._wait(e, t.w)
            for tok in t.r.values():
                self._wait(e, tok)
    @staticmethod
    def _commit(tok, R, W):
        for t in R:
            t.r[tok[0]] = tok
        for t in W:
            t.w = tok
            t.r = {}
    def op(self, e, fn, R=(), W=()):
        self._deps(e, R, W)
        self.cnt[e] += 1
        semh = self.sem[e]
        self.prog[e].append(lambda eng, f=fn, s=semh: f(eng).then_inc(s, 1))
        self._commit((e, semh, self.cnt[e]), R, W)
        self.ninst += 1
    def mm(self, out, lhsT, rhs, start=True, stop=True, R=(), W=()):
        self.op("pe", lambda eng: eng.matmul(out, lhsT, rhs, start=start, stop=stop), R, W)
    def tr(self, out, in_, ident, R=(), W=()):
        self.op("pe", lambda eng: eng.transpose(out, in_, ident), R, W)
    def act(self, out, in_, func, bias=None, scale=None, R=(), W=()):
        kw = {}
        if bias is not None:
            kw["bias"] = bias
        if scale is not None:
            kw["scale"] = scale
        self.op("act", lambda eng: eng.activation(out, in_, func, **kw), R, W)
    def copy(self, e, out, in_, R=(), W=()):
        if e == "act":
            self.op("act", lambda eng: eng.activation(out, in_, AF.Copy), R, W)
        else:
            self.op(e, lambda eng: eng.tensor_copy(out, in_), R, W)
    def tt(self, e, out, in0, in1, op, R=(), W=()):
        self.op(e, lambda eng: eng.tensor_tensor(out, in0, in1, op), R, W)
    def ts(self, e, out, in0, s1, s2, op0, op1=None, R=(), W=()):
        if op1 is None:
            self.op(e, lambda eng: eng.tensor_scalar(out, in0, s1, None, op0), R, W)
        else:
            self.op(e, lambda eng: eng.tensor_scalar(out, in0, s1, s2, op0, op1), R, W)
    def stt(self, e, out, in0, scalar, in1, op0, op1, R=(), W=()):
        self.op(e, lambda eng: eng.scalar_tensor_tensor(out, in0, scalar, in1, op0, op1), R, W)
    def memset(self, e, ap, val, W=()):
        self.op(e, lambda eng: eng.memset(ap, val), (), W)
    def recip(self, out, in_, R=(), W=()):
        self.op("dve", lambda eng: eng.reciprocal(out, in_), R, W)
    def reduce(self, out, in_, op, R=(), W=()):
        self.op("dve", lambda eng: eng.tensor_reduce(out, in_, AX.X, op), R, W)
    def scan(self, out, d0, d1, init, R=(), W=()):
        self.op("dve", lambda eng: eng.tensor_tensor_scan(out, d0, d1, init, ALU.mult, ALU.add), R, W)
    def dma(self, q, out, in_, R=(), W=(), key=None, **kw):
        if key is None:
            key = W[0]
        if key.sem is None:
            n = len(self.dsems)
            key.sem = [self.stack.enter_context(self.nc.semaphore("d%d" % n)), 0, "d%d" % n]
            self.dsems.append(key.sem)
        semh, c, kname = key.sem
        self._deps(q, R, W)
        if c > 0:
            self._wait(q, (kname, semh, c))
        self.prog[q].append(lambda eng, o=out, i=in_, s=semh, k=kw: eng.dma_start(out=o, in_=i, **k).then_inc(s, 16))
        key.sem[1] = c + 16
        self._commit((kname, semh, c + 16), R, W)
    def wait_all(self, e, trks):
        for t in trks:
            self._wait(e, t.w)
            for tok in t.r.values():
                self._wait(e, tok)
    def barrier(self):
        for e in self.engs:
            for c in ("pe", "act", "dve", "pool"):
                if self.cnt[c] > 0 and not (c == e == "pe"):
                    self._wait(e, (c, self.sem[c], self.cnt[c]))
            for semh, cval, kname in self.dsems:
                if cval > 0:
                    self._wait(e, (kname, semh, cval))
    def emit(self):
        prog = self.prog
        with self.nc.Block() as block:
            @block.sync
            def _(eng):
                for f in prog["sp"]:
                    f(eng)
            @block.tensor
            def _(eng):
                for f in prog["pe"]:
                    f(eng)
            @block.scalar
            def _(eng):
                for f in prog["act"]:
                    f(eng)
            @block.vector
            def _(eng):
                for f in prog["dve"]:
                    f(eng)
            @block.gpsimd
            def _(eng):
                for f in prog["pool"]:
                    f(eng)

class Arena:
    def __init__(self, tensor, words):
        self.t = tensor
        self.cap = words
        self.top = 0
    def mark(self):
        return self.top
    def release(self, m):
        self.top = m
    def alloc(self, free_shape, dt, parts=128):
        n = int(np.prod(free_shape))
        words = n if dt == F32 else (n + 1) // 2
        assert self.top + words <= self.cap, ("arena overflow", self.top, words, self.cap)
        ap = self.t[0:parts, self.top:self.top + words]
        self.top += words
        self.peak = max(getattr(self, 'peak', 0), self.top)
        if dt != F32:
            ap = ap.bitcast(dt)[:, 0:n]
        if len(free_shape) == 2:
            ap = ap.rearrange("p (a b) -> p a b", a=free_shape[0], b=free_shape[1])
        elif len(free_shape) == 3:
            ap = ap.rearrange("p (a b c) -> p a b c", a=free_shape[0], b=free_shape[1], c=free_shape[2])
        return ap

class Ring:
    def __init__(self, items):
        self.items = items
        self.i = 0
    def next(self):
        it = self.items[self.i]
        self.i = (self.i + 1) % len(self.items)
        return it

def _pv_layout():
    off = {}
    n = 0
    def add(name, w):
        nonlocal n
        off[name] = n
        n += w
    for l in range(DEPTH):
        add(("norm_mix", l), 8)
        add(("norm_ffn", l), 8)
        add(("gq", l), 1)
        add(("gk", l), 1)
        add(("conv_w", l), 16)
        add(("conv_b", l), 4)
        add(("lru_b_a", l), 8)
        add(("lru_b_i", l), 8)
        add(("lru_lam", l), 8)
        add(("gcq", l), 3)
        add(("gckv", l), 2)
        add(("b_mod", l), 48)
    add("final_norm", 8)
    add("eps", 1)
    return off, n

PV_OFF, NPV = _pv_layout()

def _fm(v, k):
    return np.ascontiguousarray(np.asarray(v, np.float32).reshape(k, 128).T)

def _rope_tables():
    s = np.arange(SEQ)
    row = (s // 64).astype(np.float32)
    col = (s % 64).astype(np.float32)
    def tables(dim):
        quarter = dim // 4
        inv = (np.float32(10000.0) ** (-np.arange(quarter, dtype=np.float32) / np.float32(quarter))).astype(np.float32)
        ar = row[:, None] * inv[None, :]
        ac = col[:, None] * inv[None, :]
        cos = np.ones((dim, T), np.float32)
        sin = np.zeros((dim, T), np.float32)
        half = dim // 2
        for d in range(dim):
            ang = ar if d < half else ac
            j = d % quarter
            first = (d % half) < quarter
            cos[d, NCTX:] = np.cos(ang[:, j])
            sin[d, NCTX:] = (-1.0 if first else 1.0) * np.sin(ang[:, j])
        return cos, sin
    cg, sg = tables(64)
    ropeG = np.stack([np.concatenate([cg, cg], 0), np.concatenate([sg, sg], 0)], 0)
    cm, sm = tables(32)
    cosM = np.ones((96, T), np.float32)
    sinM = np.zeros((96, T), np.float32)
    cosM[64:] = cm
    sinM[64:] = sm
    ropeM = np.stack([cosM, sinM], 0)
    def perm(dim):
        quarter = dim // 4
        half = dim // 2
        P = np.zeros((dim, dim), np.float32)
        for m in range(dim):
            first = (m % half) < quarter
            P[m + quarter if first else m - quarter, m] = 1.0
        return P
    pg = np.zeros((128, 128), np.float32)
    pg[0:64, 0:64] = perm(64)
    pg[64:128, 64:128] = perm(64)
    pm = np.zeros((96, 96), np.float32)
    pm[64:96, 64:96] = perm(32)
    return ropeG.astype(np.float32), ropeM.astype(np.float32), pg, pm

def _prepare(inp):
    f32 = np.float32
    shared = {}
    qperm = []
    for j in range(4):
        qperm += list(range(j * 64, j * 64 + 64)) + list(range((4 + j) * 64, (4 + j) * 64 + 64))
    cols = np.array(qperm + list(range(512, IN_WIDTH)))
    shared["w_in"] = np.ascontiguousarray(np.asarray(inp["w_in"], f32)[:, :, cols])
    shared["w_mod"] = np.ascontiguousarray(np.asarray(inp["w_mod"], f32))
    shared["w_qb"] = np.ascontiguousarray(np.asarray(inp["mla_w_qb"], f32))
    kvb = np.asarray(inp["mla_w_kvb"], f32).reshape(DEPTH, 256, 8, 128)
    shared["w_kvb"] = np.ascontiguousarray(
        np.concatenate([kvb[:, :, :, 0:64].reshape(DEPTH, 256, 512), kvb[:, :, :, 64:128].reshape(DEPTH, 256, 512)], -1))
    shared["w_ba"] = np.ascontiguousarray(np.asarray(inp["w_branch_attn"], f32))
    shared["w_bl"] = np.ascontiguousarray(np.asarray(inp["w_branch_lru"], f32))
    shared["w_bm"] = np.ascontiguousarray(np.asarray(inp["w_branch_mla"], f32))
    shared["w_out"] = np.ascontiguousarray(np.asarray(inp["w_out"], f32))
    shared["router_w"] = np.ascontiguousarray(np.asarray(inp["router_w"], f32))
    shared["rbias"] = np.ascontiguousarray(np.broadcast_to(np.asarray(inp["router_bias"], f32)[None, :], (128, NE)))
    shared["wg"] = np.ascontiguousarray(np.asarray(inp["moe_w_gate"], f32))
    shared["wu"] = np.ascontiguousarray(np.asarray(inp["moe_w_up"], f32))
    shared["wd"] = np.ascontiguousarray(np.asarray(inp["moe_w_down"], f32))
    lw = np.zeros((DEPTH, 2, 2, 4, 128, 128), f32)
    for gi, nm in enumerate(("lru_w_a", "lru_w_i")):
        w = np.asarray(inp[nm], f32)
        for c in range(4):
            lw[:, :, gi, c, 0:64, 0:64] = w[:, :, 2 * c]
            lw[:, :, gi, c, 64:128, 64:128] = w[:, :, 2 * c + 1]
    shared["lruW"] = lw
    ropeG, ropeM, pg, pm = _rope_tables()
    shared["ropeG"] = ropeG
    shared["ropeM"] = ropeM
    shared["permG"] = pg
    shared["permM"] = pm
    shared["ident"] = np.eye(128, dtype=f32)
    bo = np.zeros((128, 128), f32)
    bo[0:64, 0:64] = 1.0
    bo[64:, 64:] = 1.0
    shared["blockones"] = bo
    sel = np.zeros((NE, NE, 128), f32)
    for e in range(NE):
        sel[e, e, :] = 1.0
    shared["selE"] = sel.reshape(NE, NE * 128)
    pv = np.zeros((128, NPV), f32)
    def put(key, arr):
        o = PV_OFF[key]
        pv[:, o:o + arr.shape[1]] = arr
    for l in range(DEPTH):
        put(("norm_mix", l), _fm(inp["norm_mix"][l], 8))
        put(("norm_ffn", l), _fm(inp["norm_ffn"][l], 8))
        put(("gq", l), np.tile(np.asarray(inp["gqa_q_norm"][l], f32), 2)[:, None])
        put(("gk", l), np.tile(np.asarray(inp["gqa_k_norm"][l], f32), 2)[:, None])
        cw = np.asarray(inp["conv_w"][l], f32)
        put(("conv_w", l), np.concatenate([_fm(cw[j], 4) for j in range(4)], 1))
        put(("conv_b", l), _fm(inp["conv_b"][l], 4))
        for nm in ("lru_b_a", "lru_b_i", "lru_lam"):
            a = np.asarray(inp[nm][l], f32)
            put((nm, l), np.concatenate([_fm(a[0], 4), _fm(a[1], 4)], 1))
        put(("gcq", l), _fm(inp["mla_q_a_norm"][l], 3))
        put(("gckv", l), _fm(inp["mla_kv_a_norm"][l], 2))
        put(("b_mod", l), _fm(inp["b_mod"][l], 48))
    put("final_norm", _fm(inp["final_norm"], 8))
    pv[:, PV_OFF["eps"]] = EPS
    shared["pv"] = pv
    per_core = []
    x = np.asarray(inp["x"], f32)
    ctx = np.asarray(inp["ctx"], f32)
    c = np.asarray(inp["c"], f32)
    cc = np.asarray(inp["c_ctx"], f32)
    for b in range(x.shape[0]):
        xin = np.ascontiguousarray(np.concatenate([ctx[b], x[b]], 0))
        cv = np.stack([_fm(c[b], 8), _fm(cc, 8)], -1).reshape(128, 16)
        per_core.append({"xin": xin, "cvec": np.ascontiguousarray(cv)})
    return shared, per_core

SHARED_SHAPES = {
    "w_in": [DEPTH, D, IN_WIDTH], "w_mod": [DEPTH, D, 6 * D], "w_qb": [DEPTH, 384, 768], "w_kvb": [DEPTH, 256, 1024],
    "w_ba": [DEPTH, 512, D], "w_bl": [DEPTH, 512, D], "w_bm": [DEPTH, 512, D], "w_out": [DEPTH, D, D],
    "router_w": [D, NE], "rbias": [128, NE], "wg": [DEPTH, NE, D, 512], "wu": [DEPTH, NE, D, 512],
    "wd": [DEPTH, NE, 512, D], "lruW": [DEPTH, 2, 2, 4, 128, 128], "ropeG": [2, 128, T], "ropeM": [2, 96, T],
    "permG": [128, 128], "permM": [96, 96], "ident": [128, 128], "blockones": [128, 128], "selE": [NE, NE * 128],
    "pv": [128, NPV], "xin": [T, D], "cvec": [128, 16],
}

class _Stop(Exception):
    pass
def build_nc(depth=DEPTH, stop=None, dumps=()):
    nc = bass.Bass("TRN2", target_bir_lowering=False)
    dr = {k: nc.dram_tensor(k, v, F32, kind="ExternalInput").ap() for k, v in SHARED_SHAPES.items()}
    out_d = nc.dram_tensor("out", [SEQ, D], F32, kind="ExternalOutput").ap()
    xs_d = nc.dram_tensor("xs_scratch", [128, KC, T], F32, kind="Internal").ap()
    with ExitStack() as st:
        fw = FW(nc, st)
        ARW = 53200
        ar = Arena(fw.sb("arena", [128, ARW], F32), ARW)
        psb = [fw.ps("ps%d" % i, [128, 512], F32) for i in range(8)]
        pst = [Trk("ps%d" % i) for i in range(8)]
        ring = Ring([(psb[i], pst[i]) for i in range(6)])
        accr = Ring([(psb[i], pst[i]) for i in (6, 7)])
        pv = ar.alloc([NPV], F32); t_pv = Trk("pv")
        ident = ar.alloc([128], F32); t_c = Trk("consts")
        ones32 = ar.alloc([128], F32)
        bones = ar.alloc([128], F32)
        sel64 = ar.alloc([64], F32)
        permG = ar.alloc([128], BF16)
        permM = ar.alloc([96], BF16, parts=96)
        cvec = ar.alloc([16], F32)
        actc = ar.alloc([16], F32)
        rbias = ar.alloc([NE], F32)
        rw = ar.alloc([KC, NE], F32)
        modsb = ar.alloc([96], F32); t_mod = Trk("mod")
        der = ar.alloc([6, 16], F32); t_der = Trk("der")
        cneg = ar.alloc([8], F32); t_cneg = Trk("cneg")
        hT = ar.alloc([KC, T], BF16)
        t_h = [Trk("h%d" % b) for b in range(5)]
        xs_t = [Trk("xs%d" % b) for b in range(5)]
        t_out = Trk("out")
        eps_ap = pv[:, PV_OFF["eps"]:PV_OFF["eps"] + 1]
        fw.dma("sp", pv, dr["pv"], W=[t_pv])
        fw.dma("sp", ident, dr["ident"], W=[t_c])
        fw.dma("sp", bones, dr["blockones"], W=[t_c])
        fw.dma("pool", permG, dr["permG"], W=[t_c])
        fw.dma("pool", permM, dr["permM"], W=[t_c])
        fw.dma("sp", cvec, dr["cvec"], W=[t_c])
        fw.dma("sp", rbias, dr["rbias"], W=[t_c])
        fw.dma("sp", rw, dr["router_w"].rearrange("(k p) e -> p k e", p=128), W=[t_c])
        fw.memset("dve", ones32, 1.0, W=[t_c])
        fw.memset("dve", sel64, 0.0, W=[t_c])
        fw.memset("dve", sel64[64:65, :], 1.0, W=[t_c])
        fw.act(actc, cvec, AF.Silu, R=[t_c], W=[t_c])
        base_mark = ar.mark()
        def pvc(key, j=0, n=1):
            o = PV_OFF[key] + j
            return pv[:, o:o + n]
        def blk_s(b):
            return 1 if b == 0 else 0
        def phase_load():
            m = ar.mark()
            xt = [ar.alloc([D], F32) for _ in range(2)]; t_xt = [Trk() for _ in range(2)]
            xo = [ar.alloc([KC, 128], F32) for _ in range(2)]; t_xo = [Trk() for _ in range(2)]
            for i in range(NT):
                bi = next(b for b, (t0, nb) in enumerate(BLOCKS) if t0 <= i * 128 < t0 + nb)
                s = i % 2
                fw.dma("sp", xt[s], dr["xin"][i * 128:(i + 1) * 128, :], W=[t_xt[s]])
                for hf in range(2):
                    pb, pt = ring.next()
                    for kk in range(4):
                        k = hf * 4 + kk
                        fw.tr(pb[:, kk * 128:(kk + 1) * 128], xt[s][:, k * 128:(k + 1) * 128], ident,
                              R=[t_xt[s], t_c], W=[pt])
                    fw.copy("act" if hf == 0 else "dve", xo[s][:, hf * 4:(hf + 1) * 4, :],
                            pb[:, :].rearrange("p (a b) -> p a b", a=4), R=[pt], W=[t_xo[s]])
                fw.dma("sp", xs_d[:, :, i * 128:(i + 1) * 128], xo[s], R=[t_xo[s]], W=[xs_t[bi]])
            fw.barrier()
            ar.release(m)
        def phase_mod(l):
            m = ar.mark()
            wm = [ar.alloc([KC, 512], F32) for _ in range(2)]; t_wm = [Trk() for _ in range(2)]
            pb, pt = accr.next()
            wsrc = dr["w_mod"][l].rearrange("(k p) n -> p k n", p=128)
            for g in range(12):
                s = g % 2
                fw.dma("sp", wm[s], wsrc[:, :, g * 512:(g + 1) * 512], W=[t_wm[s]])
                for jj in range(4):
                    j = g * 4 + jj
                    for k in range(KC):
                        fw.mm(pb[:, 2 * j:2 * j + 2], wm[s][:, k, jj * 128:(jj + 1) * 128], actc[:, 2 * k:2 * k + 2],
                              start=(k == 0), stop=(k == KC - 1), R=[t_wm[s], t_c], W=[pt])
            bm = pvc(("b_mod", l), 0, 48)
            for s in range(2):
                fw.tt("dve", modsb[:, s:96:2], pb[:, s:96:2], bm, ALU.add, R=[pt, t_pv], W=[t_mod])
            def modv(mi):
                return modsb[:, mi * 16:(mi + 1) * 16]
            for (di, mi_scale, gkey) in ((0, 1, ("norm_mix", l)), (3, 4, ("norm_ffn", l))):
                for s in range(2):
                    fw.stt("dve", der[:, di, s:16:2], modv(mi_scale)[:, s:16:2], 1.0, pvc(gkey, 0, 8), ALU.add, ALU.mult,
                           R=[t_mod, t_pv], W=[t_der])
            for (di, mi) in ((1, 0), (2, 2), (4, 3), (5, 5)):
                fw.copy("dve", der[:, di, :], modv(mi), R=[t_mod], W=[t_der])
            tmp = ar.alloc([6, 8], F32); t_tmp = Trk()
            lam = pvc(("lru_lam", l), 0, 8)
            e_, l_, p_, mk, a_, b_ = (tmp[:, i, :] for i in range(6))
            fw.act(e_, lam, AF.Exp, scale=-1.0, R=[t_pv], W=[t_tmp])
            fw.act(l_, e_, AF.Ln, bias=1.0, R=[t_tmp], W=[t_tmp])
            fw.ts("dve", p_, e_, -0.2, 0.25, ALU.mult, ALU.add, R=[t_tmp], W=[t_tmp])
            for cst in (1.0 / 3.0, 0.5, 1.0):
                fw.tt("dve", p_, p_, e_, ALU.mult, R=[t_tmp], W=[t_tmp])
                fw.ts("dve", p_, p_, -1.0, cst, ALU.mult, ALU.add, R=[t_tmp], W=[t_tmp])
            fw.tt("dve", p_, p_, e_, ALU.mult, R=[t_tmp], W=[t_tmp])
            fw.ts("dve", mk, e_, 0.1, None, ALU.is_lt, R=[t_tmp], W=[t_tmp])
            fw.tt("dve", a_, p_, l_, ALU.subtract, R=[t_tmp], W=[t_tmp])
            fw.tt("dve", a_, a_, mk, ALU.mult, R=[t_tmp], W=[t_tmp])
            fw.tt("dve", a_, a_, l_, ALU.add, R=[t_tmp], W=[t_tmp])
            fw.ts("dve", cneg, a_, -8.0, None, ALU.mult, R=[t_tmp], W=[t_cneg])
            fw.barrier()
            ar.release(m)
        def rms_rstd(ssq_ps, pt, n, inv_n, rstd, t_rstd, parts=128):
            fw.act(rstd[0:parts, 0:n], ssq_ps[0:parts, 0:n], AF.Ln, bias=eps_ap[0:parts, :], scale=inv_n,
                   R=[pt, t_pv], W=[t_rstd])
            fw.act(rstd[0:parts, 0:n], rstd[0:parts, 0:n], AF.Exp, scale=-0.5, R=[t_rstd], W=[t_rstd])
        def phase_norm(l, which, blocks, combT=None, t_comb=None):
            m = ar.mark()
            di = 0 if which == 1 else 3
            xb = [ar.alloc([KC, 512], F32) for _ in range(2)]; t_xb = [Trk() for _ in range(2)]
            sq = ar.alloc([KC, 512], F32); t_sq = Trk()
            rstd = ar.alloc([512], F32); t_rstd = Trk()
            tmp = [ar.alloc([512], F32) for _ in range(2)]; t_tmp = [Trk() for _ in range(2)]
            if which == 2:
                h32 = ar.alloc([KC, 512], F32); t_h32 = Trk()
                rt = ar.alloc([8, NE], F32); t_rt = Trk()
            for bi in blocks:
                t0, nb = BLOCKS[bi]
                s = blk_s(bi)
                bs = bi % 2
                fw.dma("sp", xb[bs][:, :, 0:nb], xs_d[:, :, t0:t0 + nb], R=[xs_t[bi]], W=[t_xb[bs]], key=t_xb[bs])
                fw.act(sq[:, :, 0:nb], xb[bs][:, :, 0:nb], AF.Square, R=[t_xb[bs]], W=[t_sq])
                pb, pt = ring.next()
                for k in range(KC):
                    fw.mm(pb[:, 0:nb], ones32, sq[:, k, 0:nb], start=(k == 0), stop=(k == KC - 1), R=[t_sq, t_c], W=[pt])
                rms_rstd(pb, pt, nb, 1.0 / D, rstd, t_rstd)
                for k in range(KC):
                    ts_ = k % 2
                    fw.stt("dve", tmp[ts_][:, 0:nb], xb[bs][:, k, 0:nb], der[:, di, 2 * k + s:2 * k + s + 1], rstd[:, 0:nb],
                           ALU.mult, ALU.mult, R=[t_xb[bs], t_der, t_rstd], W=[t_tmp[ts_]])
                    sh = der[:, di + 1, 2 * k + s:2 * k + s + 1]
                    if which == 1:
                        fw.act(hT[:, k, t0:t0 + nb], tmp[ts_][:, 0:nb], AF.Identity, bias=sh, R=[t_tmp[ts_], t_der], W=[t_h[bi]])
                    else:
                        fw.act(h32[:, k, 0:nb], tmp[ts_][:, 0:nb], AF.Identity, bias=sh, R=[t_tmp[ts_], t_der], W=[t_h32])
                        fw.copy("pool", hT[:, k, t0:t0 + nb], h32[:, k, 0:nb], R=[t_h32], W=[t_h[bi]])
                if which == 2:
                    for ti in range(nb // 128):
                        pb, pt = ring.next()
                        for k in range(KC):
                            fw.mm(pb[:, 0:NE], h32[:, k, ti * 128:(ti + 1) * 128], rw[:, k, :], start=(k == 0),
                                  stop=(k == KC - 1), R=[t_h32, t_c], W=[pt])
                        sc, sel, tm, sel2, m1, m2, wsel, comb = (rt[:, i, :] for i in range(8))
                        R_ = [t_rt]
                        fw.act(sc, pb[:, 0:NE], AF.Sigmoid, R=[pt], W=[t_rt])
                        fw.tt("dve", sel, sc, rbias, ALU.add, R=[t_rt, t_c], W=[t_rt])
                        g4 = lambda a: a.rearrange("p (g j) -> p g j", g=4)
                        fw.reduce(m1[:, 0:4], g4(sel), ALU.max, R=R_, W=[t_rt])
                        for g in range(4):
                            fw.ts("dve", tm[:, 4 * g:4 * g + 4], sel[:, 4 * g:4 * g + 4], m1[:, g:g + 1], -1e9,
                                  ALU.is_equal, ALU.mult, R=R_, W=[t_rt])
                        fw.tt("dve", sel2, sel, tm, ALU.add, R=R_, W=[t_rt])
                        fw.reduce(m2[:, 0:4], g4(sel2), ALU.max, R=R_, W=[t_rt])
                        fw.tt("dve", m1[:, 4:8], m1[:, 0:4], m2[:, 0:4], ALU.add, R=R_, W=[t_rt])
                        fw.reduce(m1[:, 8:9], m1[:, 4:8], ALU.max, R=R_, W=[t_rt])
                        fw.ts("dve", m1[:, 12:16], m1[:, 4:8], m1[:, 8:9], None, ALU.is_equal, R=R_, W=[t_rt])
                        for g in range(4):
                            fw.ts("dve", tm[:, 4 * g:4 * g + 4], sel[:, 4 * g:4 * g + 4], m2[:, g:g + 1], m1[:, 12 + g:13 + g],
                                  ALU.is_ge, ALU.mult, R=R_, W=[t_rt])
                        fw.tt("dve", wsel, tm, sc, ALU.mult, R=R_, W=[t_rt])
                        fw.reduce(m2[:, 8:9], wsel, ALU.add, R=R_, W=[t_rt])
                        fw.recip(m2[:, 9:10], m2[:, 8:9], R=R_, W=[t_rt])
                        fw.ts("dve", comb, wsel, m2[:, 9:10], None, ALU.mult, R=R_, W=[t_rt])
                        pb2, pt2 = ring.next()
                        fw.tr(pb2[0:NE, 0:128], comb, ident, R=[t_rt, t_c], W=[pt2])
                        c0 = t0 + ti * 128
                        fw.copy("act", combT[0:NE, c0:c0 + 128], pb2[0:NE, 0:128], R=[pt2], W=[t_comb])
            fw.barrier()
            ar.release(m)
        WTR = {}
        def wt(name):
            if name not in WTR:
                WTR[name] = Trk(name)
            return WTR[name]
        pend_loads = []
        def load_w(dst, src, trk, R=()):
            if len(pend_loads) >= NLOADS:
                fw._wait("pool", pend_loads.pop(0))
            fw.dma("pool", dst, src, R=list(R), W=[trk])
            pend_loads.append(trk.w)
        def win(l, c0, c1):
            return dr["w_in"][l].rearrange("(k p) n -> p k n", p=128)[:, :, c0:c1]
        def phase_lru(l, o_lru, t_olru):
            m = ar.mark()
            wz = ar.alloc([KC, 1024], BF16); t_wz = wt("wz0"); t_wz1 = wt("wz1")
            load_w(wz[:, :, 0:512], win(l, C_ZX, C_ZX + 512), t_wz)
            load_w(wz[:, :, 512:1024], win(l, C_ZY, C_ZY + 512), t_wz1)
            lw = ar.alloc([16, 128], BF16); t_lw = wt("lw")
            load_w(lw, dr["lruW"][l].rearrange("d g c p m -> p (d g c) m"), t_lw)
            ZP = T + 6
            zx = ar.alloc([ZP], F32); t_zx = Trk()
            u = ar.alloc([T], F32); t_u = Trk()
            ub = ar.alloc([T], BF16); t_ub = Trk()
            gy = ar.alloc([T], F32); t_gy = Trk()
            a_ = ar.alloc([T], F32); t_a = Trk()
            b_ = ar.alloc([T], F32); t_b = Trk()
            hf = ar.alloc([T], F32); t_hf = Trk()
            hb = ar.alloc([T], F32); t_hb = Trk()
            tq = [ar.alloc([512], F32) for _ in range(4)]; t_tq = [Trk() for _ in range(4)]
            fw.memset("pool", zx, 0.0, W=[t_zx])
            for c in range(4):
                for bi, (t0, nb) in enumerate(BLOCKS):
                    pb, pt = ring.next()
                    for k in range(KC):
                        fw.mm(pb[:, 0:nb], wz[:, k, c * 128:(c + 1) * 128], hT[:, k, t0:t0 + nb], start=(k == 0),
                              stop=(k == KC - 1), R=[t_wz, t_h[bi]], W=[pt])
                    zo = 1 if bi == 0 else t0 + 4
                    fw.copy("act", zx[:, zo:zo + nb], pb[:, 0:nb], R=[pt], W=[t_zx])
                    pb, pt = ring.next()
                    for k in range(KC):
                        fw.mm(pb[:, 0:nb], wz[:, k, 512 + c * 128:512 + (c + 1) * 128], hT[:, k, t0:t0 + nb], start=(k == 0),
                              stop=(k == KC - 1), R=[t_wz1, t_h[bi]], W=[pt])
                    fw.act(gy[:, t0:t0 + nb], pb[:, 0:nb], AF.Gelu_apprx_tanh, R=[pt], W=[t_gy])
                for (d0, n, base) in ((0, NCTX, 1), (NCTX, SEQ, 260)):
                    for j in range(4):
                        wj = pvc(("conv_w", l), j * 4 + c)
                        src = zx[:, base - 1 + j:base - 1 + j + n]
                        if j == 0:
                            fw.ts("dve", u[:, d0:d0 + n], src, wj, pvc(("conv_b", l), c), ALU.mult, ALU.add,
                                  R=[t_zx, t_pv], W=[t_u])
                        else:
                            fw.stt("dve", u[:, d0:d0 + n], src, wj, u[:, d0:d0 + n], ALU.mult, ALU.add,
                                   R=[t_zx, t_pv, t_u], W=[t_u])
                fw.copy("act", ub, u, R=[t_u], W=[t_ub])
                for dr_ in range(2):
                    for bi, (t0, nb) in enumerate(BLOCKS):
                        r_, i_, s_, iu = tq
                        tr_, ti_, ts2, tiu = t_tq
                        pb, pt = ring.next()
                        fw.mm(pb[:, 0:nb], lw[:, (dr_ * 2 + 0) * 4 + c, :], ub[:, t0:t0 + nb], R=[t_lw, t_ub], W=[pt])
                        fw.act(r_[:, 0:nb], pb[:, 0:nb], AF.Sigmoid, bias=pvc(("lru_b_a", l), dr_ * 4 + c), R=[pt, t_pv], W=[tr_])
                        fw.act(a_[:, t0:t0 + nb], r_[:, 0:nb], AF.Exp, scale=cneg[:, dr_ * 4 + c:dr_ * 4 + c + 1],
                               R=[tr_, t_cneg], W=[t_a])
                        pb, pt = ring.next()
                        fw.mm(pb[:, 0:nb], lw[:, (dr_ * 2 + 1) * 4 + c, :], ub[:, t0:t0 + nb], R=[t_lw, t_ub], W=[pt])
                        fw.act(i_[:, 0:nb], pb[:, 0:nb], AF.Sigmoid, bias=pvc(("lru_b_i", l), dr_ * 4 + c), R=[pt, t_pv], W=[ti_])
                        fw.tt("dve", iu[:, 0:nb], i_[:, 0:nb], u[:, t0:t0 + nb], ALU.mult, R=[ti_, t_u], W=[tiu])
                        fw.act(s_[:, 0:nb], a_[:, t0:t0 + nb], AF.Square, R=[t_a], W=[ts2])
                        fw.act(s_[:, 0:nb], s_[:, 0:nb], AF.Sqrt, bias=1.0, scale=-1.0, R=[ts2], W=[ts2])
                        fw.tt("dve", b_[:, t0:t0 + nb], s_[:, 0:nb], iu[:, 0:nb], ALU.mult, R=[ts2, tiu], W=[t_b])
                    if dr_ == 0:
                        fw.scan(hf, a_, b_, 0.0, R=[t_a, t_b], W=[t_hf])
                    else:
                        fw.scan(hb[:, 0:NCTX][:, ::-1], a_[:, 0:NCTX][:, ::-1], b_[:, 0:NCTX][:, ::-1], 0.0,
                                R=[t_a, t_b], W=[t_hb])
                        fw.scan(hb[:, NCTX:T][:, ::-1], a_[:, NCTX:T][:, ::-1], b_[:, NCTX:T][:, ::-1], hb[:, 0:1],
                                R=[t_a, t_b, t_hb], W=[t_hb])
                fw.tt("dve", hf, hf, hb, ALU.add, R=[t_hf, t_hb], W=[t_hf])
                fw.tt("dve", o_lru[:, c, :], hf, gy, ALU.mult, R=[t_hf, t_gy], W=[t_olru])
            fw.barrier()
            ar.release(m)
        def norm_rope(pb, pt, nb, t0, gain, dst, dst_t, wk):
            (sq, t_sq), (rstd, t_rstd), (kn, t_kn), (cs, t_cs), (t1, t_t1), (t2, t_t2) = wk
            fw.act(sq[:, 0:nb], pb[:, 0:nb], AF.Square, R=[pt], W=[t_sq])
            pb2, pt2 = ring.next()
            fw.mm(pb2[:, 0:nb], bones, sq[:, 0:nb], R=[t_sq, t_c], W=[pt2])
            rms_rstd(pb2, pt2, nb, 1.0 / 64.0, rstd, t_rstd)
            fw.stt("dve", kn[:, 0:nb], pb[:, 0:nb], gain, rstd[:, 0:nb], ALU.mult, ALU.mult, R=[pt, t_pv, t_rstd], W=[t_kn])
            pb3, pt3 = ring.next()
            fw.mm(pb3[:, 0:nb], permG, kn[:, 0:nb], R=[t_kn, t_c], W=[pt3])
            fw.dma("sp", cs[:, :, 0:nb], dr["ropeG"][:, :, t0:t0 + nb].rearrange("c p t -> p c t"), W=[t_cs])
            fw.tt("dve", t1[:, 0:nb], kn[:, 0:nb], cs[:, 0, 0:nb], ALU.mult, R=[t_kn, t_cs], W=[t_t1])
            fw.tt("dve", t2[:, 0:nb], pb3[:, 0:nb], cs[:, 1, 0:nb], ALU.mult, R=[pt3, t_cs], W=[t_t2])
            if isinstance(dst, list):
                for (dap, p0, p1) in dst:
                    fw.tt("pool", dap, t1[p0:p1, 0:nb], t2[p0:p1, 0:nb], ALU.add, R=[t_t1, t_t2], W=[dst_t])
            else:
                fw.tt("pool", dst, t1[:, 0:nb], t2[:, 0:nb], ALU.add, R=[t_t1, t_t2], W=[dst_t])
        def rope_m(src, t_src, nb, t0, dst, dst_t, wk, dst_parts=(0, 96)):
            (cs, t_cs), (t1, t_t1), (t2, t_t2) = wk
            pb3, pt3 = ring.next()
            fw.mm(pb3[0:96, 0:nb], permM, src[0:96, 0:nb], R=[t_src, t_c], W=[pt3])
            fw.dma("sp", cs[0:96, :, 0:nb], dr["ropeM"][:, :, t0:t0 + nb].rearrange("c p t -> p c t"), W=[t_cs])
            fw.tt("dve", t1[0:96, 0:nb], src[0:96, 0:nb], cs[0:96, 0, 0:nb], ALU.mult, R=[t_src, t_cs], W=[t_t1])
            fw.tt("dve", t2[0:96, 0:nb], pb3[0:96, 0:nb], cs[0:96, 1, 0:nb], ALU.mult, R=[pt3, t_cs], W=[t_t2])
            p0, p1 = dst_parts
            fw.tt("pool", dst, t1[p0:p1, 0:nb], t2[p0:p1, 0:nb], ALU.add, R=[t_t1, t_t2], W=[dst_t])
        def mk_work():
            sq = ar.alloc([3, 512], F32); rstd = ar.alloc([512], F32); kn = ar.alloc([512], BF16)
            cs = ar.alloc([2, 512], F32)
            t_cs = Trk(); t_sq = Trk()
            return dict(sq=(sq, t_sq), rstd=(rstd, Trk()), kn=(kn, Trk()), cs=(cs, t_cs), t1=(sq[:, 1, :], t_sq), t2=(sq[:, 2, :], t_sq),
                        csm=(cs, t_cs))
        def phase_kv(l, kv):
            m = ar.mark()
            kTg, Vg, kTm, Vm, t_kTg, t_Vg, t_kTm, t_Vm = kv
            wk_ = ar.alloc([KC, 128], BF16); wv_ = ar.alloc([KC, 128], BF16); wc_ = ar.alloc([KC, 256], BF16)
            wr_ = ar.alloc([KC, 96], BF16); wkvb = ar.alloc([2, 1024], BF16)
            t_wk, t_wv, t_wc, t_wr, t_wkvb = wt("wk"), wt("wv"), wt("wc"), wt("wr"), wt("wkvb")
            fw.memset("pool", wr_, 0.0, W=[t_wr])
            load_w(wk_, win(l, C_K, C_K + 128), t_wk)
            load_w(wv_, win(l, C_V, C_V + 128), t_wv)
            load_w(wc_, win(l, C_CKV, C_CKV + 256), t_wc)
            load_w(wr_[:, :, 64:96], win(l, C_KR, C_KR + 32), t_wr)
            load_w(wkvb, dr["w_kvb"][l].rearrange("(j p) n -> p j n", p=128), t_wkvb)
            fw.memset("pool", Vg[:, :, :, 64:65], 1.0, W=[t_Vg])
            fw.memset("pool", Vm[:, :, :, 64:65], 1.0, W=[t_Vm])
            W_ = mk_work()
            ckvT = ar.alloc([2, 512], BF16); t_ckv = Trk()
            krs = ar.alloc([512], BF16); t_krs = Trk()
            krr = ar.alloc([512], BF16); t_krr = Trk()
            sq, t_sq = W_["sq"]; rstd, t_rstd = W_["rstd"]
            for bi, (t0, nb) in enumerate(BLOCKS):
                pb, pt = ring.next()
                for k in range(KC):
                    fw.mm(pb[:, 0:nb], wk_[:, k, :], hT[:, k, t0:t0 + nb], start=(k == 0), stop=(k == KC - 1),
                          R=[t_wk, t_h[bi]], W=[pt])
                norm_rope(pb, pt, nb, t0, pvc(("gk", l)), kTg[:, t0:t0 + nb], t_kTg,
                          ((sq[:, 0, :], t_sq), W_["rstd"], W_["kn"], W_["cs"], W_["t1"], W_["t2"]))
                for ti in range(nb // 128):
                    tt_ = t0 // 128 + ti
                    pb, pt = ring.next()
                    for k in range(KC):
                        fw.mm(pb[:, 0:128], hT[:, k, tt_ * 128:(tt_ + 1) * 128], wv_[:, k, :], start=(k == 0), stop=(k == KC - 1),
                              R=[t_wv, t_h[bi]], W=[pt])
                    fw.copy("act", Vg[:, tt_, :, 0:64], pb[:, 0:128].rearrange("p (h d) -> p h d", h=2), R=[pt], W=[t_Vg])
                pbs = [ring.next() for _ in range(2)]
                for j in range(2):
                    for k in range(KC):
                        fw.mm(pbs[j][0][:, 0:nb], wc_[:, k, j * 128:(j + 1) * 128], hT[:, k, t0:t0 + nb], start=(k == 0),
                              stop=(k == KC - 1), R=[t_wc, t_h[bi]], W=[pbs[j][1]])
                    fw.act(sq[:, j, 0:nb], pbs[j][0][:, 0:nb], AF.Square, R=[pbs[j][1]], W=[t_sq])
                pb2, pt2 = ring.next()
                for j in range(2):
                    fw.mm(pb2[:, 0:nb], ones32, sq[:, j, 0:nb], start=(j == 0), stop=(j == 1), R=[t_sq, t_c], W=[pt2])
                rms_rstd(pb2, pt2, nb, 1.0 / 256.0, rstd, t_rstd)
                for j in range(2):
                    fw.stt("dve", ckvT[:, j, 0:nb], pbs[j][0][:, 0:nb], pvc(("gckv", l), j), rstd[:, 0:nb], ALU.mult, ALU.mult,
                           R=[pbs[j][1], t_pv, t_rstd], W=[t_ckv])
                for h in range(8):
                    pb, pt = ring.next()
                    for j in range(2):
                        fw.mm(pb[0:64, 0:nb], wkvb[:, j, h * 64:(h + 1) * 64], ckvT[:, j, 0:nb], start=(j == 0), stop=(j == 1),
                              R=[t_wkvb, t_ckv], W=[pt])
                    fw.copy("act" if h % 2 else "dve", kTm[0:64, h, t0:t0 + nb], pb[0:64, 0:nb], R=[pt], W=[t_kTm])
                for ti in range(nb // 128):
                    tt_ = t0 // 128 + ti
                    pb, pt = ring.next()
                    for j in range(2):
                        fw.mm(pb[:, 0:512], ckvT[:, j, ti * 128:(ti + 1) * 128], wkvb[:, j, 512:1024], start=(j == 0), stop=(j == 1),
                              R=[t_wkvb, t_ckv], W=[pt])
                    fw.copy("act" if ti % 2 else "dve", Vm[:, tt_, :, 0:64], pb[:, 0:512].rearrange("p (h d) -> p h d", h=8),
                            R=[pt], W=[t_Vm])
                pb, pt = ring.next()
                for k in range(KC):
                    fw.mm(pb[0:96, 0:nb], wr_[:, k, :], hT[:, k, t0:t0 + nb], start=(k == 0), stop=(k == KC - 1),
                          R=[t_wr, t_h[bi]], W=[pt])
                fw.copy("act", krs[0:96, 0:nb], pb[0:96, 0:nb], R=[pt], W=[t_krs])
                rope_m(krs, t_krs, nb, t0, krr[64:96, 0:nb], t_krr, (W_["csm"], W_["t1"], W_["t2"]), dst_parts=(64, 96))
                for h in range(8):
                    fw.copy("pool" if h % 2 else "dve", kTm[64:96, h, t0:t0 + nb], krr[64:96, 0:nb], R=[t_krr], W=[t_kTm])
            fw.barrier()
            ar.release(m)
        def phase_attn(l, blocks, kv, o_attn, t_oattn, o_mla, t_omla):
            m = ar.mark()
            kTg, Vg, kTm, Vm, t_kTg, t_Vg, t_kTm, t_Vm = kv
            wq = ar.alloc([KC, 512], BF16); wcq = ar.alloc([KC, 384], BF16); wqb = ar.alloc([3, 768], BF16)
            t_wq, t_wcq, t_wqb = wt("wq"), wt("wcq"), wt("wqb")
            load_w(wq, win(l, C_Q, C_Q + 512), t_wq)
            load_w(wcq, win(l, C_CQ, C_CQ + 384), t_wcq)
            load_w(wqb, dr["w_qb"][l].rearrange("(j p) n -> p j n", p=128), t_wqb)
            W_ = mk_work()
            sq, t_sq = W_["sq"]; rstd, t_rstd = W_["rstd"]
            qTg = ar.alloc([8, 512], BF16); t_qTg = [Trk() for _ in range(4)]
            for j in range(4):
                fw.memset("pool", qTg[:, 2 * j:2 * j + 2, :], 0.0, W=[t_qTg[j]])
            cqT = ar.alloc([3, 512], BF16); t_cq = Trk()
            qraw = ar.alloc([512], BF16); t_qraw = Trk()
            qm = [ar.alloc([512], BF16) for _ in range(2)]; t_qm = [Trk() for _ in range(2)]
            pT = [ar.alloc([512], BF16) for _ in range(2)]; t_pT = [Trk() for _ in range(2)]
            osb = ar.alloc([512], F32); t_osb = Trk()
            fw.memset("pool", osb, 0.0, W=[t_osb])
            LA = 2
            for bi in blocks:
                t0, nb = BLOCKS[bi]
                ktiles = list(range(2)) if bi == 0 else list(range(NT))
                nk = len(ktiles)
                for j in range(4):
                    pb, pt = ring.next()
                    for k in range(KC):
                        fw.mm(pb[:, 0:nb], wq[:, k, j * 128:(j + 1) * 128], hT[:, k, t0:t0 + nb], start=(k == 0), stop=(k == KC - 1),
                              R=[t_wq, t_h[bi]], W=[pt])
                    norm_rope(pb, pt, nb, t0, pvc(("gq", l)),
                              [(qTg[0:64, 2 * j, 0:nb], 0, 64), (qTg[64:128, 2 * j + 1, 0:nb], 64, 128)], t_qTg[j],
                              ((sq[:, 0, :], t_sq), W_["rstd"], W_["kn"], W_["cs"], W_["t1"], W_["t2"]))
                pbs = [ring.next() for _ in range(3)]
                for j in range(3):
                    for k in range(KC):
                        fw.mm(pbs[j][0][:, 0:nb], wcq[:, k, j * 128:(j + 1) * 128], hT[:, k, t0:t0 + nb], start=(k == 0),
                              stop=(k == KC - 1), R=[t_wcq, t_h[bi]], W=[pbs[j][1]])
                    fw.act(sq[:, j, 0:nb], pbs[j][0][:, 0:nb], AF.Square, R=[pbs[j][1]], W=[t_sq])
                pb2, pt2 = ring.next()
                for j in range(3):
                    fw.mm(pb2[:, 0:nb], ones32, sq[:, j, 0:nb], start=(j == 0), stop=(j == 2), R=[t_sq, t_c], W=[pt2])
                rms_rstd(pb2, pt2, nb, 1.0 / 384.0, rstd, t_rstd)
                for j in range(3):
                    fw.stt("dve", cqT[:, j, 0:nb], pbs[j][0][:, 0:nb], pvc(("gcq", l), j), rstd[:, 0:nb], ALU.mult, ALU.mult,
                           R=[pbs[j][1], t_pv, t_rstd], W=[t_cq])
                jobs = []
                for j in range(4):
                    for half in range(2):
                        hd = j + 4 * half
                        p0 = 64 * half
                        jobs.append(dict(
                            mla=None,
                            k_of=(lambda kt: kTg[:, kt * 128:(kt + 1) * 128]),
                            q_ap=qTg[:, 2 * j + half, 0:nb],
                            v_of=(lambda kt, half=half: Vg[:, kt, half, 0:65]),
                            scale=0.125,
                            dst=o_attn[64 * (hd % 2):64 * (hd % 2) + 64, hd // 2, t0:t0 + nb], dst_t=t_oattn,
                            Rk=[t_kTg], Rq=[t_qTg[j]], Rv=[t_Vg]))
                for h in range(8):
                    s = h % 2
                    jobs.append(dict(
                        mla=h,
                        k_of=(lambda kt, h=h: kTm[0:96, h, kt * 128:(kt + 1) * 128]),
                        q_ap=qm[s][0:96, 0:nb],
                        v_of=(lambda kt, h=h: Vm[:, kt, h, 0:65]),
                        scale=96.0 ** -0.5,
                        dst=o_mla[64 * (h % 2):64 * (h % 2) + 64, h // 2, t0:t0 + nb], dst_t=t_omla,
                        Rk=[t_kTm], Rq=[t_qm[s]], Rv=[t_Vm]))

                def prep_a(job):
                    h = job["mla"]
                    if h is None:
                        return
                    pb, pt = ring.next()
                    for j in range(3):
                        fw.mm(pb[0:96, 0:nb], wqb[:, j, h * 96:(h + 1) * 96], cqT[:, j, 0:nb], start=(j == 0), stop=(j == 2),
                              R=[t_wqb, t_cq], W=[pt])
                    fw.copy("dve", qraw[0:96, 0:nb], pb[0:96, 0:nb], R=[pt], W=[t_qraw])

                def prep_b(job):
                    h = job["mla"]
                    if h is None:
                        return
                    s = h % 2
                    rope_m(qraw, t_qraw, nb, t0, qm[s][0:96, 0:nb], t_qm[s], (W_["csm"], W_["t1"], W_["t2"]))

                def emit_S(job, kt):
                    pb, pt = ring.next()
                    fw.mm(pb[:, 0:nb], job["k_of"](kt), job["q_ap"], R=job["Rk"] + job["Rq"], W=[pt])
                    return pb, pt

                def finish_a(job, acc, acct):
                    fw.copy("dve", osb[0:65, 0:nb], acc[0:65, 0:nb], R=[acct], W=[t_osb])
                    fw.recip(osb[64:65, 0:nb], osb[64:65, 0:nb], R=[t_osb], W=[t_osb])

                def finish_b(job):
                    pb, pt = ring.next()
                    fw.mm(pb[0:64, 0:nb], sel64, osb[:, 0:nb], R=[t_osb, t_c], W=[pt])
                    fw.tt("dve", job["dst"], osb[0:64, 0:nb], pb[0:64, 0:nb], ALU.mult, R=[t_osb, pt], W=[job["dst_t"]])

                flat = [(ji, ki) for ji in range(len(jobs)) for ki in range(nk)]
                done_a = set(); done_b = set()

                def ensure_prep(jx, upto_b=True):
                    if jx not in done_a:
                        prep_a(jobs[jx]); done_a.add(jx)
                    if upto_b and jx not in done_b:
                        prep_b(jobs[jx]); done_b.add(jx)

                inflight = []
                for i0 in range(min(LA, len(flat))):
                    ji, ki = flat[i0]
                    ensure_prep(ji)
                    inflight.append(emit_S(jobs[ji], ktiles[ki]))
                pend_fin = None
                acc = acct = None
                kb = min(8, nk - 1)
                for i, (ji, ki) in enumerate(flat):
                    job = jobs[ji]
                    if ki == 0:
                        acc, acct = accr.next()
                        if ji + 1 < len(jobs):
                            ensure_prep(ji + 1, upto_b=False)
                    if ki == kb and ji + 1 < len(jobs):
                        ensure_prep(ji + 1)
                    if i + LA < len(flat):
                        ji2, ki2 = flat[i + LA]
                        ensure_prep(ji2)
                        inflight.append(emit_S(jobs[ji2], ktiles[ki2]))
                    pb, pt = inflight.pop(0)
                    s = i % 2
                    fw.act(pT[s][:, 0:nb], pb[:, 0:nb], AF.Exp, scale=job["scale"], R=[pt], W=[t_pT[s]])
                    fw.mm(acc[0:65, 0:nb], job["v_of"](ktiles[ki]), pT[s][:, 0:nb], start=(ki == 0), stop=(ki == nk - 1),
                          R=job["Rv"] + [t_pT[s]], W=[acct])
                    if ki == min(10, nk - 1) and pend_fin is not None:
                        finish_b(pend_fin)
                        pend_fin = None
                    if ki == nk - 1:
                        if pend_fin is not None:
                            finish_b(pend_fin)
                        finish_a(job, acc, acct)
                        pend_fin = job
                if pend_fin is not None:
                    finish_b(pend_fin)
            fw.barrier()
            ar.release(m)
        def phase_merge(l, blocks, outs, merged, t_mg):
            m = ar.mark()
            wg = ar.alloc([3, KC, 512], BF16); wb = ar.alloc([3, 4, 512], BF16)
            sg = [ar.alloc([512], F32) for _ in range(2)]; t_sg = [Trk() for _ in range(2)]
            mm_ = ar.alloc([512], F32); t_mm = Trk()
            tt2 = ar.alloc([512], F32); t_tt2 = Trk()
            wnames = ("w_ba", "w_bl", "w_bm")
            t_wg = [wt("wg%d" % i) for i in range(3)]; t_wb = [wt("wb%d" % i) for i in range(3)]
            for half in range(2):
                for br in range(3):
                    c0 = C_G + br * 1024 + half * 512
                    load_w(wg[:, br, :, :], win(l, c0, c0 + 512), t_wg[br])
                    load_w(wb[:, br, :, :], dr[wnames[br]][l].rearrange("(j p) n -> p j n", p=128)[:, :, half * 512:(half + 1) * 512], t_wb[br])
                for bi in blocks:
                    t0, nb = BLOCKS[bi]
                    for ff in range(4):
                        f = half * 4 + ff
                        for br in range(3):
                            o_br, t_obr = outs[br]
                            pg_, ptg = ring.next()
                            for k in range(KC):
                                fw.mm(pg_[:, 0:nb], wg[:, br, k, ff * 128:(ff + 1) * 128], hT[:, k, t0:t0 + nb], start=(k == 0),
                                      stop=(k == KC - 1), R=[t_wg[br], t_h[bi]], W=[ptg])
                            s = br % 2
                            fw.act(sg[s][:, 0:nb], pg_[:, 0:nb], AF.Sigmoid, R=[ptg], W=[t_sg[s]])
                            pb, pt = ring.next()
                            for j in range(4):
                                fw.mm(pb[:, 0:nb], wb[:, br, j, ff * 128:(ff + 1) * 128], o_br[:, j, t0:t0 + nb], start=(j == 0),
                                      stop=(j == 3), R=[t_wb[br], t_obr], W=[pt])
                            if br == 0:
                                fw.tt("dve", mm_[:, 0:nb], sg[s][:, 0:nb], pb[:, 0:nb], ALU.mult, R=[t_sg[s], pt], W=[t_mm])
                            else:
                                fw.tt("dve", tt2[:, 0:nb], sg[s][:, 0:nb], pb[:, 0:nb], ALU.mult, R=[t_sg[s], pt], W=[t_tt2])
                                if br == 1:
                                    fw.tt("pool", mm_[:, 0:nb], mm_[:, 0:nb], tt2[:, 0:nb], ALU.add, R=[t_mm, t_tt2], W=[t_mm])
                                else:
                                    fw.tt("pool", merged[:, f, t0:t0 + nb], mm_[:, 0:nb], tt2[:, 0:nb], ALU.add,
                                          R=[t_mm, t_tt2], W=[t_mg[bi]])
            fw.barrier()
            ar.release(m)
        def phase_wout(l, blocks, merged, t_mg):
            m = ar.mark()
            wo = ar.alloc([KC, D], BF16); t_wo = [wt("wo0"), wt("wo1")]
            wsrc = dr["w_out"][l].rearrange("(k p) n -> p k n", p=128)
            load_w(wo[:, :, 0:512], wsrc[:, :, 0:512], t_wo[0])
            load_w(wo[:, :, 512:1024], wsrc[:, :, 512:1024], t_wo[1])
            xb = [ar.alloc([KC, 512], F32) for _ in range(2)]; t_xb = [Trk() for _ in range(2)]
            for bi in blocks:
                t0, nb = BLOCKS[bi]
                s = blk_s(bi)
                bs = bi % 2
                fw.dma("sp", xb[bs][:, :, 0:nb], xs_d[:, :, t0:t0 + nb], R=[xs_t[bi]], W=[t_xb[bs]], key=t_xb[bs])
                for f in range(KC):
                    pb, pt = ring.next()
                    for k in range(KC):
                        fw.mm(pb[:, 0:nb], wo[:, k, f * 128:(f + 1) * 128], merged[:, k, t0:t0 + nb], start=(k == 0),
                              stop=(k == KC - 1), R=[t_wo[f // 4], t_mg[bi]], W=[pt])
                    fw.stt("dve", xb[bs][:, f, 0:nb], pb[:, 0:nb], der[:, 2, 2 * f + s:2 * f + s + 1], xb[bs][:, f, 0:nb],
                           ALU.mult, ALU.add, R=[pt, t_der, t_xb[bs]], W=[t_xb[bs]])
                fw.dma("sp", xs_d[:, :, t0:t0 + nb], xb[bs][:, :, 0:nb], R=[t_xb[bs]], W=[xs_t[bi]], key=t_xb[bs])
            fw.barrier()
            ar.release(m)
        def phase_moe(l, blocks, combT, t_comb, yacc, t_y):
            m = ar.mark()
            selE = ar.alloc([NE, 128], F32, parts=NE); t_sel = Trk()
            fw.dma("sp", selE, dr["selE"].rearrange("k (e m) -> k e m", e=NE), W=[t_sel])
            wgu = [ar.alloc([2, KC, 512], BF16) for _ in range(2)]
            wdn = [ar.alloc([4, D], BF16) for _ in range(2)]
            t_we = [[wt("we%d_%d" % (s_, i)) for i in range(4)] for s_ in range(2)]
            cb = ar.alloc([512], F32); t_cb = Trk()
            ss = [ar.alloc([512], F32) for _ in range(2)]; t_ss = [Trk() for _ in range(2)]
            tu = [ar.alloc([512], F32) for _ in range(2)]; t_tu = [Trk() for _ in range(2)]
            actT = [ar.alloc([4, 512], BF16) for _ in range(2)]; t_act = [Trk() for _ in range(2)]
            def load_e(e):
                s = e % 2
                load_w(wgu[s][:, 0, :, :], dr["wg"][l, e].rearrange("(k p) n -> p k n", p=128), t_we[s][0])
                load_w(wgu[s][:, 1, :, :], dr["wu"][l, e].rearrange("(k p) n -> p k n", p=128), t_we[s][1])
                wsrc = dr["wd"][l, e].rearrange("(j p) n -> p j n", p=128)
                load_w(wdn[s][:, :, 0:512], wsrc[:, :, 0:512], t_we[s][2])
                load_w(wdn[s][:, :, 512:1024], wsrc[:, :, 512:1024], t_we[s][3])
            def emit_gu(e, bi, idx):
                s = e % 2
                t0, nb = BLOCKS[bi]
                pb, pt = ring.next()
                fw.mm(pb[:, 0:nb], selE[0:NE, e, :], combT[0:NE, t0:t0 + nb], R=[t_sel, t_comb], W=[pt])
                fw.copy("act", cb[:, 0:nb], pb[:, 0:nb], R=[pt], W=[t_cb])
                a_s = idx % 2
                for ff in range(4):
                    pg_, ptg = ring.next()
                    for k in range(KC):
                        fw.mm(pg_[:, 0:nb], wgu[s][:, 0, k, ff * 128:(ff + 1) * 128], hT[:, k, t0:t0 + nb], start=(k == 0),
                              stop=(k == KC - 1), R=[t_we[s][0], t_h[bi]], W=[ptg])
                    pu_, ptu = ring.next()
                    for k in range(KC):
                        fw.mm(pu_[:, 0:nb], wgu[s][:, 1, k, ff * 128:(ff + 1) * 128], hT[:, k, t0:t0 + nb], start=(k == 0),
                              stop=(k == KC - 1), R=[t_we[s][1], t_h[bi]], W=[ptu])
                    q = ff % 2
                    fw.act(ss[q][:, 0:nb], pg_[:, 0:nb], AF.Silu, R=[ptg], W=[t_ss[q]])
                    fw.tt("dve", tu[q][:, 0:nb], ss[q][:, 0:nb], pu_[:, 0:nb], ALU.mult, R=[t_ss[q], ptu], W=[t_tu[q]])
                    fw.tt("pool", actT[a_s][:, ff, 0:nb], tu[q][:, 0:nb], cb[:, 0:nb], ALU.mult, R=[t_tu[q], t_cb], W=[t_act[a_s]])

            def emit_down(e, bi, idx):
                s = e % 2
                t0, nb = BLOCKS[bi]
                a_s = idx % 2
                for f in range(KC):
                    pb, pt = ring.next()
                    for j in range(4):
                        fw.mm(pb[:, 0:nb], wdn[s][:, j, f * 128:(f + 1) * 128], actT[a_s][:, j, 0:nb], start=(j == 0),
                              stop=(j == 3), R=[t_we[s][2 + f // 4], t_act[a_s]], W=[pt])
                    if e == 0:
                        fw.copy("act", yacc[:, f, t0:t0 + nb], pb[:, 0:nb], R=[pt], W=[t_y[bi]])
                    else:
                        fw.tt("dve", yacc[:, f, t0:t0 + nb], yacc[:, f, t0:t0 + nb], pb[:, 0:nb], ALU.add,
                              R=[pt, t_y[bi]], W=[t_y[bi]])

            load_e(0)
            items = [(e, bi) for e in range(NE) for bi in blocks]
            prev = None
            for idx, (e, bi) in enumerate(items):
                emit_gu(e, bi, idx)
                if prev is not None:
                    emit_down(*prev)
                if bi == blocks[0] and e + 1 < NE:
                    load_e(e + 1)
                prev = (e, bi, idx)
            emit_down(*prev)
            fw.barrier()
            ar.release(m)
        def phase_ffn_res(l, blocks, yacc, t_y, last):
            m = ar.mark()
            xb = [ar.alloc([KC, 512], F32) for _ in range(2)]; t_xb = [Trk() for _ in range(2)]
            if last:
                sq = ar.alloc([KC, 512], F32); t_sq = Trk()
                rstd = ar.alloc([512], F32); t_rstd = Trk()
                ot = [ar.alloc([D], F32) for _ in range(2)]; t_ot = [Trk() for _ in range(2)]
            oi = 0
            for bi in blocks:
                t0, nb = BLOCKS[bi]
                s = blk_s(bi)
                bs = bi % 2
                fw.dma("sp", xb[bs][:, :, 0:nb], xs_d[:, :, t0:t0 + nb], R=[xs_t[bi]], W=[t_xb[bs]], key=t_xb[bs])
                for f in range(KC):
                    fw.stt("dve", xb[bs][:, f, 0:nb], yacc[:, f, t0:t0 + nb], der[:, 5, 2 * f + s:2 * f + s + 1], xb[bs][:, f, 0:nb],
                           ALU.mult, ALU.add, R=[t_y[bi], t_der, t_xb[bs]], W=[t_xb[bs]])
                if not last:
                    fw.dma("sp", xs_d[:, :, t0:t0 + nb], xb[bs][:, :, 0:nb], R=[t_xb[bs]], W=[xs_t[bi]], key=t_xb[bs])
                    continue
                fw.act(sq[:, :, 0:nb], xb[bs][:, :, 0:nb], AF.Square, R=[t_xb[bs]], W=[t_sq])
                pb, pt = ring.next()
                for k in range(KC):
                    fw.mm(pb[:, 0:nb], ones32, sq[:, k, 0:nb], start=(k == 0), stop=(k == KC - 1), R=[t_sq, t_c], W=[pt])
                rms_rstd(pb, pt, nb, 1.0 / D, rstd, t_rstd)
                for k in range(KC):
                    fw.stt("dve", xb[bs][:, k, 0:nb], xb[bs][:, k, 0:nb], pvc("final_norm", k), rstd[:, 0:nb], ALU.mult, ALU.mult,
                           R=[t_xb[bs], t_pv, t_rstd], W=[t_xb[bs]])
                for ti in range(nb // 128):
                    os_ = oi % 2
                    oi += 1
                    for hf in range(2):
                        pb, pt = ring.next()
                        for kk in range(4):
                            k = hf * 4 + kk
                            fw.tr(pb[:, kk * 128:(kk + 1) * 128], xb[bs][:, k, ti * 128:(ti + 1) * 128], ident,
                                  R=[t_xb[bs], t_c], W=[pt])
                        fw.copy("act" if hf == 0 else "dve", ot[os_][:, hf * 512:(hf + 1) * 512], pb[:, 0:512], R=[pt], W=[t_ot[os_]])
                    r0 = t0 - NCTX + ti * 128
                    fw.dma("sp", out_d[r0:r0 + 128, :], ot[os_], R=[t_ot[os_]], W=[t_out], key=t_ot[os_])
            fw.barrier()
            ar.release(m)
        def ck(name, bufs):
            if stop != name:
                return
            fw.barrier()
            for key, ap in bufs.items():
                if key not in dumps:
                    continue
                d = nc.dram_tensor("dbg_" + key, list(ap.shape), F32, kind="ExternalOutput").ap()
                fw.dma("pool", d, ap, W=[Trk()])
            raise _Stop()
        ALLB = [0, 1, 2, 3, 4]
        LATB = [1, 2, 3, 4]
        try:
            phase_load()
            ck("load", dict(xs=xs_d))
            for l in range(depth):
                last = (l == DEPTH - 1)
                qblocks = LATB if last else ALLB
                phase_mod(l)
                ck("mod%d" % l, dict(mod=modsb, der=der, cneg=cneg))
                phase_norm(l, 1, ALLB)
                ck("norm%d" % l, dict(hT=hT))
                lm = ar.mark()
                o_lru = ar.alloc([4, T], BF16); t_olru = Trk()
                phase_lru(l, o_lru, t_olru)
                ck("lru%d" % l, dict(o_lru=o_lru))
                om = ar.mark()
                o_attn = ar.alloc([4, T], BF16); t_oattn = Trk()
                o_mla = ar.alloc([4, T], BF16); t_omla = Trk()
                kvm = ar.mark()
                kTg = ar.alloc([T], BF16); Vg = ar.alloc([NT, 2, 65], BF16)
                kTm = ar.alloc([8, T], BF16, parts=96); Vm = ar.alloc([NT, 8, 65], BF16)
                kv = (kTg, Vg, kTm, Vm, Trk(), Trk(), Trk(), Trk())
                phase_kv(l, kv)
                ck("kv%d" % l, dict(kTg=kTg, kTm=kTm, Vg=Vg, Vm=Vm))
                phase_attn(l, qblocks, kv, o_attn, t_oattn, o_mla, t_omla)
                ck("attn%d" % l, dict(o_attn=o_attn, o_mla=o_mla))
                ar.release(kvm)
                merged = ar.alloc([KC, T], BF16); t_mg = [Trk() for _ in range(5)]
                phase_merge(l, qblocks, ((o_attn, t_oattn), (o_lru, t_olru), (o_mla, t_omla)), merged, t_mg)
                ck("merge%d" % l, dict(merged=merged))
                phase_wout(l, qblocks, merged, t_mg)
                ck("wout%d" % l, dict(xs=xs_d))
                ar.release(lm)
                combT = ar.alloc([T], F32, parts=NE); t_comb = Trk()
                phase_norm(l, 2, qblocks, combT, t_comb)
                ck("normf%d" % l, dict(hT=hT, combT=combT))
                yacc = ar.alloc([KC, T], F32); t_y = [Trk() for _ in range(5)]
                phase_moe(l, qblocks, combT, t_comb, yacc, t_y)
                ck("moe%d" % l, dict(yacc=yacc))
                phase_ffn_res(l, qblocks, yacc, t_y, last)
                ck("res%d" % l, dict(xs=xs_d))
                ar.release(lm)
        except _Stop:
            pass
        fw.wait_all("sp", [t_out] + xs_t)
        fw.barrier()
        fw.emit()
        build_nc.stats = (fw.ninst, fw.nwait, len(fw.dsems), ar.peak)
    return nc

_CACHE = {}

def kernel(**inputs):
    shared, per_core = _prepare(inputs)
    if "nc" not in _CACHE:
        _CACHE["nc"] = build_nc()
    nc = _CACHE["nc"]
    in_maps = []
    for pc in per_core:
        d = dict(shared)
        d.update(pc)
        in_maps.append(d)
    res = run_bass_kernel_spmd(nc, in_maps, core_ids=list(range(len(in_maps))))
    out = np.stack([np.asarray(r["out"], np.float32) for r in res.results], 0)
    return out
```

```python
import numpy as np
from contextlib import ExitStack
import concourse.bass as bass
import concourse.mybir as mybir
from concourse.bass_utils import run_bass_kernel_spmd
F32 = mybir.dt.float32
BF16 = mybir.dt.bfloat16
AF = mybir.ActivationFunctionType
ALU = mybir.AluOpType
AX = mybir.AxisListType
D = 1024
KC = 8
NCTX = 256
SEQ = 2048
T = NCTX + SEQ
NT = T // 128
DEPTH = 2
BLOCKS = [(0, 256), (256, 512), (768, 512), (1280, 512), (1792, 512)]
IN_WIDTH = 5536
C_Q, C_K, C_V, C_ZX, C_ZY, C_CQ, C_CKV, C_KR, C_G = 0, 512, 640, 768, 1280, 1792, 2176, 2432, 2464
EPS = 1e-6
NE = 16
import os
NLOADS = int(os.environ.get("K_NLOADS", "1"))

class Trk:
    __slots__ = ("name", "w", "r", "sem")
    def __init__(self, name=""):
        self.name = name
        self.w = None
        self.r = {}
        self.sem = None

class FW:
    def __init__(self, nc, stack):
        self.nc = nc
        self.stack = stack
        self.engs = ("pe", "act", "dve", "pool", "sp")
        self.prog = {e: [] for e in self.engs}
        self.sem = {}
        self.cnt = {}
        for e in ("pe", "act", "dve", "pool"):
            self.sem[e] = stack.enter_context(nc.semaphore("s_" + e))
            self.cnt[e] = 0
        self.known = {e: {} for e in self.engs}
        self.dsems = []
        self.ninst = 0
        self.nwait = 0
    def sb(self, name, shape, dt):
        return self.stack.enter_context(self.nc.sbuf_tensor(name, list(shape), dt))
    def ps(self, name, shape, dt=F32):
        return self.stack.enter_context(self.nc.psum_tensor(name, list(shape), dt))
    def _wait(self, e, tok):
        if tok is None:
            return
        key, semh, val = tok
        if key == e and e == "pe":
            return
        kn = self.known[e]
        if kn.get(key, 0) >= val:
            return
        self.prog[e].append(lambda eng, s=semh, v=val: eng.wait_ge(s, v))
        kn[key] = val
        self.nwait += 1
    def _deps(self, e, R, W):
        for t in R:
            self._wait(e, t.w)
        for t in W:
            self._wait(e, t.w)
            for tok in t.r.values():
                self._wait(e, tok)
    @staticmethod
    def _commit(tok, R, W):
        for t in R:
            t.r[tok[0]] = tok
        for t in W:
            t.w = tok
            t.r = {}
    def op(self, e, fn, R=(), W=()):
        self._deps(e, R, W)
        self.cnt[e] += 1
        semh = self.sem[e]
        self.prog[e].append(lambda eng, f=fn, s=semh: f(eng).then_inc(s, 1))
        self._commit((e, semh, self.cnt[e]), R, W)
        self.ninst += 1
    def mm(self, out, lhsT, rhs, start=True, stop=True, R=(), W=()):
        self.op("pe", lambda eng: eng.matmul(out, lhsT, rhs, start=start, stop=stop), R, W)
    def tr(self, out, in_, ident, R=(), W=()):
        self.op("pe", lambda eng: eng.transpose(out, in_, ident), R, W)
    def act(self, out, in_, func, bias=None, scale=None, R=(), W=()):
        kw = {}
        if bias is not None:
            kw["bias"] = bias
        if scale is not None:
            kw["scale"] = scale
        self.op("act", lambda eng: eng.activation(out, in_, func, **kw), R, W)
    def copy(self, e, out, in_, R=(), W=()):
        if e == "act":
            self.op("act", lambda eng: eng.activation(out, in_, AF.Copy), R, W)
        else:
            self.op(e, lambda eng: eng.tensor_copy(out, in_), R, W)
    def tt(self, e, out, in0, in1, op, R=(), W=()):
        self.op(e, lambda eng: eng.tensor_tensor(out, in0, in1, op), R, W)
    def ts(self, e, out, in0, s1, s2, op0, op1=None, R=(), W=()):
        if op1 is None:
            self.op(e, lambda eng: eng.tensor_scalar(out, in0, s1, None, op0), R, W)
        else:
            self.op(e, lambda eng: eng.tensor_scalar(out, in0, s1, s2, op0, op1), R, W)
    def stt(self, e, out, in0, scalar, in1, op0, op1, R=(), W=()):
        self.op(e, lambda eng: eng.scalar_tensor_tensor(out, in0, scalar, in1, op0, op1), R, W)
    def memset(self, e, ap, val, W=()):
        self.op(e, lambda eng: eng.memset(ap, val), (), W)
    def recip(self, out, in_, R=(), W=()):
        self.op("dve", lambda eng: eng.reciprocal(out, in_), R, W)
    def reduce(self, out, in_, op, R=(), W=()):
        self.op("dve", lambda eng: eng.tensor_reduce(out, in_, AX.X, op), R, W)
    def scan(self, out, d0, d1, init, R=(), W=()):
        self.op("dve", lambda eng: eng.tensor_tensor_scan(out, d0, d1, init, ALU.mult, ALU.add), R, W)
    def dma(self, q, out, in_, R=(), W=(), key=None, **kw):
        if key is None:
            key = W[0]
        if key.sem is None:
            n = len(self.dsems)
            key.sem = [self.stack.enter_context(self.nc.semaphore("d%d" % n)), 0, "d%d" % n]
            self.dsems.append(key.sem)
        semh, c, kname = key.sem
        self._deps(q, R, W)
        if c > 0:
            self._wait(q, (kname, semh, c))
        self.prog[q].append(lambda eng, o=out, i=in_, s=semh, k=kw: eng.dma_start(out=o, in_=i, **k).then_inc(s, 16))
        key.sem[1] = c + 16
        self._commit((kname, semh, c + 16), R, W)
    def wait_all(self, e, trks):
        for t in trks:
            self._wait(e, t.w)
            for tok in t.r.values():
                self._wait(e, tok)
    def barrier(self):
        for e in self.engs:
            for c in ("pe", "act", "dve", "pool"):
                if self.cnt[c] > 0 and not (c == e == "pe"):
                    self._wait(e, (c, self.sem[c], self.cnt[c]))
            for semh, cval, kname in self.dsems:
                if cval > 0:
                    self._wait(e, (kname, semh, cval))
    def emit(self):
        prog = self.prog
        with self.nc.Block() as block:
            @block.sync
            def _(eng):
                for f in prog["sp"]:
                    f(eng)
            @block.tensor
            def _(eng):
                for f in prog["pe"]:
                    f(eng)
            @block.scalar
            def _(eng):
                for f in prog["act"]:
                    f(eng)
            @block.vector
            def _(eng):
                for f in prog["dve"]:
                    f(eng)
            @block.gpsimd
            def _(eng):
                for f in prog["pool"]:
                    f(eng)

class Arena:
    def __init__(self, tensor, words):
        self.t = tensor
        self.cap = words
        self.top = 0
    def mark(self):
        return self.top
    def release(self, m):
        self.top = m
    def alloc(self, free_shape, dt, parts=128):
        n = int(np.prod(free_shape))
        words = n if dt == F32 else (n + 1) // 2
        assert self.top + words <= self.cap, ("arena overflow", self.top, words, self.cap)
        ap = self.t[0:parts, self.top:self.top + words]
        self.top += words
        self.peak = max(getattr(self, 'peak', 0), self.top)
        if dt != F32:
            ap = ap.bitcast(dt)[:, 0:n]
        if len(free_shape) == 2:
            ap = ap.rearrange("p (a b) -> p a b", a=free_shape[0], b=free_shape[1])
        elif len(free_shape) == 3:
            ap = ap.rearrange("p (a b c) -> p a b c", a=free_shape[0], b=free_shape[1], c=free_shape[2])
        return ap

class Ring:
    def __init__(self, items):
        self.items = items
        self.i = 0
    def next(self):
        it = self.items[self.i]
        self.i = (self.i + 1) % len(self.items)
        return it

def _pv_layout():
    off = {}
    n = 0
    def add(name, w):
        nonlocal n
        off[name] = n
        n += w
    for l in range(DEPTH):
        add(("norm_mix", l), 8)
        add(("norm_ffn", l), 8)
        add(("gq", l), 1)
        add(("gk", l), 1)
        add(("conv_w", l), 16)
        add(("conv_b", l), 4)
        add(("lru_b_a", l), 8)
        add(("lru_b_i", l), 8)
        add(("lru_lam", l), 8)
        add(("gcq", l), 3)
        add(("gckv", l), 2)
        add(("b_mod", l), 48)
    add("final_norm", 8)
    add("eps", 1)
    return off, n

PV_OFF, NPV = _pv_layout()

def _fm(v, k):
    return np.ascontiguousarray(np.asarray(v, np.float32).reshape(k, 128).T)

def _rope_tables():
    s = np.arange(SEQ)
    row = (s // 64).astype(np.float32)
    col = (s % 64).astype(np.float32)
    def tables(dim):
        quarter = dim // 4
        inv = (np.float32(10000.0) ** (-np.arange(quarter, dtype=np.float32) / np.float32(quarter))).astype(np.float32)
        ar = row[:, None] * inv[None, :]
        ac = col[:, None] * inv[None, :]
        cos = np.ones((dim, T), np.float32)
        sin = np.zeros((dim, T), np.float32)
        half = dim // 2
        for d in range(dim):
            ang = ar if d < half else ac
            j = d % quarter
            first = (d % half) < quarter
            cos[d, NCTX:] = np.cos(ang[:, j])
            sin[d, NCTX:] = (-1.0 if first else 1.0) * np.sin(ang[:, j])
        return cos, sin
    cg, sg = tables(64)
    ropeG = np.stack([np.concatenate([cg, cg], 0), np.concatenate([sg, sg], 0)], 0)
    cm, sm = tables(32)
    cosM = np.ones((96, T), np.float32)
    sinM = np.zeros((96, T), np.float32)
    cosM[64:] = cm
    sinM[64:] = sm
    ropeM = np.stack([cosM, sinM], 0)
    def perm(dim):
        quarter = dim // 4
        half = dim // 2
        P = np.zeros((dim, dim), np.float32)
        for m in range(dim):
            first = (m % half) < quarter
            P[m + quarter if first else m - quarter, m] = 1.0
        return P
    pg = np.zeros((128, 128), np.float32)
    pg[0:64, 0:64] = perm(64)
    pg[64:128, 64:128] = perm(64)
    pm = np.zeros((96, 96), np.float32)
    pm[64:96, 64:96] = perm(32)
    return ropeG.astype(np.float32), ropeM.astype(np.float32), pg, pm

def _prepare(inp):
    f32 = np.float32
    shared = {}
    qperm = []
    for j in range(4):
        qperm += list(range(j * 64, j * 64 + 64)) + list(range((4 + j) * 64, (4 + j) * 64 + 64))
    cols = np.array(qperm + list(range(512, IN_WIDTH)))
    shared["w_in"] = np.ascontiguousarray(np.asarray(inp["w_in"], f32)[:, :, cols])
    shared["w_mod"] = np.ascontiguousarray(np.asarray(inp["w_mod"], f32))
    shared["w_qb"] = np.ascontiguousarray(np.asarray(inp["mla_w_qb"], f32))
    kvb = np.asarray(inp["mla_w_kvb"], f32).reshape(DEPTH, 256, 8, 128)
    shared["w_kvb"] = np.ascontiguousarray(
        np.concatenate([kvb[:, :, :, 0:64].reshape(DEPTH, 256, 512), kvb[:, :, :, 64:128].reshape(DEPTH, 256, 512)], -1))
    shared["w_ba"] = np.ascontiguousarray(np.asarray(inp["w_branch_attn"], f32))
    shared["w_bl"] = np.ascontiguousarray(np.asarray(inp["w_branch_lru"], f32))
    shared["w_bm"] = np.ascontiguousarray(np.asarray(inp["w_branch_mla"], f32))
    shared["w_out"] = np.ascontiguousarray(np.asarray(inp["w_out"], f32))
    shared["router_w"] = np.ascontiguousarray(np.asarray(inp["router_w"], f32))
    shared["rbias"] = np.ascontiguousarray(np.broadcast_to(np.asarray(inp["router_bias"], f32)[None, :], (128, NE)))
    shared["wgu"] = np.ascontiguousarray(np.concatenate([np.asarray(inp["moe_w_gate"], f32), np.asarray(inp["moe_w_up"], f32)], -1))
    shared["wd"] = np.ascontiguousarray(np.asarray(inp["moe_w_down"], f32))
    lw = np.zeros((DEPTH, 2, 2, 4, 128, 128), f32)
    for gi, nm in enumerate(("lru_w_a", "lru_w_i")):
        w = np.asarray(inp[nm], f32)
        for c in range(4):
            lw[:, :, gi, c, 0:64, 0:64] = w[:, :, 2 * c]
            lw[:, :, gi, c, 64:128, 64:128] = w[:, :, 2 * c + 1]
    shared["lruW"] = lw
    ropeG, ropeM, pg, pm = _rope_tables()
    shared["ropeG"] = ropeG
    shared["ropeM"] = ropeM
    shared["permG"] = pg
    shared["permM"] = pm
    shared["ident"] = np.eye(128, dtype=f32)
    bo = np.zeros((128, 128), f32)
    bo[0:64, 0:64] = 1.0
    bo[64:, 64:] = 1.0
    shared["blockones"] = bo
    sel = np.zeros((NE, NE, 128), f32)
    for e in range(NE):
        sel[e, e, :] = 1.0
    shared["selE"] = sel.reshape(NE, NE * 128)
    pv = np.zeros((128, NPV), f32)
    def put(key, arr):
        o = PV_OFF[key]
        pv[:, o:o + arr.shape[1]] = arr
    for l in range(DEPTH):
        put(("norm_mix", l), _fm(inp["norm_mix"][l], 8))
        put(("norm_ffn", l), _fm(inp["norm_ffn"][l], 8))
        put(("gq", l), np.tile(np.asarray(inp["gqa_q_norm"][l], f32), 2)[:, None])
        put(("gk", l), np.tile(np.asarray(inp["gqa_k_norm"][l], f32), 2)[:, None])
        cw = np.asarray(inp["conv_w"][l], f32)
        put(("conv_w", l), np.concatenate([_fm(cw[j], 4) for j in range(4)], 1))
        put(("conv_b", l), _fm(inp["conv_b"][l], 4))
        for nm in ("lru_b_a", "lru_b_i", "lru_lam"):
            a = np.asarray(inp[nm][l], f32)
            put((nm, l), np.concatenate([_fm(a[0], 4), _fm(a[1], 4)], 1))
        put(("gcq", l), _fm(inp["mla_q_a_norm"][l], 3))
        put(("gckv", l), _fm(inp["mla_kv_a_norm"][l], 2))
        put(("b_mod", l), _fm(inp["b_mod"][l], 48))
    put("final_norm", _fm(inp["final_norm"], 8))
    pv[:, PV_OFF["eps"]] = EPS
    shared["pv"] = pv
    per_core = []
    x = np.asarray(inp["x"], f32)
    ctx = np.asarray(inp["ctx"], f32)
    c = np.asarray(inp["c"], f32)
    cc = np.asarray(inp["c_ctx"], f32)
    for b in range(x.shape[0]):
        xin = np.ascontiguousarray(np.concatenate([ctx[b], x[b]], 0))
        cv = np.stack([_fm(c[b], 8), _fm(cc, 8)], -1).reshape(128, 16)
        per_core.append({"xin": xin, "cvec": np.ascontiguousarray(cv)})
    return shared, per_core

SHARED_SHAPES = {
    "w_in": [DEPTH, D, IN_WIDTH], "w_mod": [DEPTH, D, 6 * D], "w_qb": [DEPTH, 384, 768], "w_kvb": [DEPTH, 256, 1024],
    "w_ba": [DEPTH, 512, D], "w_bl": [DEPTH, 512, D], "w_bm": [DEPTH, 512, D], "w_out": [DEPTH, D, D],
    "router_w": [D, NE], "rbias": [128, NE], "wgu": [DEPTH, NE, D, 1024],
    "wd": [DEPTH, NE, 512, D], "lruW": [DEPTH, 2, 2, 4, 128, 128], "ropeG": [2, 128, T], "ropeM": [2, 96, T],
    "permG": [128, 128], "permM": [96, 96], "ident": [128, 128], "blockones": [128, 128], "selE": [NE, NE * 128],
    "pv": [128, NPV], "xin": [T, D], "cvec": [128, 16],
}

class _Stop(Exception):
    pass
def build_nc(depth=DEPTH, stop=None, dumps=()):
    nc = bass.Bass("TRN2", target_bir_lowering=False)
    dr = {k: nc.dram_tensor(k, v, F32, kind="ExternalInput").ap() for k, v in SHARED_SHAPES.items()}
    out_d = nc.dram_tensor("out", [SEQ, D], F32, kind="ExternalOutput").ap()
    xs_d = nc.dram_tensor("xs_scratch", [128, KC, T], F32, kind="Internal").ap()
    with ExitStack() as st:
        fw = FW(nc, st)
        ARW = 53200
        ar = Arena(fw.sb("arena", [128, ARW], F32), ARW)
        psb = [fw.ps("ps%d" % i, [128, 512], F32) for i in range(8)]
        pst = [Trk("ps%d" % i) for i in range(8)]
        ring = Ring([(psb[i], pst[i]) for i in range(6)])
        accr = Ring([(psb[i], pst[i]) for i in (6, 7)])
        pv = ar.alloc([NPV], F32); t_pv = Trk("pv")
        ident = ar.alloc([128], F32); t_c = Trk("consts")
        ones32 = ar.alloc([128], F32)
        bones = ar.alloc([128], F32)
        sel64 = ar.alloc([64], F32)
        permG = ar.alloc([128], BF16)
        permM = ar.alloc([96], BF16, parts=96)
        cvec = ar.alloc([16], F32)
        actc = ar.alloc([16], F32)
        rbias = ar.alloc([NE], F32)
        rw = ar.alloc([KC, NE], F32)
        modsb = ar.alloc([96], F32); t_mod = Trk("mod")
        der = ar.alloc([6, 16], F32); t_der = Trk("der")
        cneg = ar.alloc([8], F32); t_cneg = Trk("cneg")
        cneg2 = ar.alloc([8], F32)
        hT = ar.alloc([KC, T], BF16)
        t_h = [Trk("h%d" % b) for b in range(5)]
        xs_t = [Trk("xs%d" % b) for b in range(5)]
        t_out = Trk("out")
        eps_ap = pv[:, PV_OFF["eps"]:PV_OFF["eps"] + 1]
        fw.dma("sp", pv, dr["pv"], W=[t_pv])
        fw.dma("sp", ident, dr["ident"], W=[t_c])
        fw.dma("sp", bones, dr["blockones"], W=[t_c])
        fw.dma("pool", permG, dr["permG"], W=[t_c])
        fw.dma("pool", permM, dr["permM"], W=[t_c])
        fw.dma("sp", cvec, dr["cvec"], W=[t_c])
        fw.dma("sp", rbias, dr["rbias"], W=[t_c])
        fw.dma("sp", rw, dr["router_w"].rearrange("(k p) e -> p k e", p=128), W=[t_c])
        fw.memset("dve", ones32, 1.0, W=[t_c])
        fw.memset("dve", sel64, 0.0, W=[t_c])
        fw.memset("dve", sel64[64:65, :], 1.0, W=[t_c])
        fw.act(actc, cvec, AF.Silu, R=[t_c], W=[t_c])
        base_mark = ar.mark()
        def pvc(key, j=0, n=1):
            o = PV_OFF[key] + j
            return pv[:, o:o + n]
        def blk_s(b):
            return 1 if b == 0 else 0
        def phase_load():
            m = ar.mark()
            xt = [ar.alloc([D], F32) for _ in range(2)]; t_xt = [Trk() for _ in range(2)]
            xo = [ar.alloc([KC, 128], F32) for _ in range(2)]; t_xo = [Trk() for _ in range(2)]
            for i in range(NT):
                bi = next(b for b, (t0, nb) in enumerate(BLOCKS) if t0 <= i * 128 < t0 + nb)
                s = i % 2
                fw.dma("sp", xt[s], dr["xin"][i * 128:(i + 1) * 128, :], W=[t_xt[s]])
                for hf in range(2):
                    pb, pt = ring.next()
                    for kk in range(4):
                        k = hf * 4 + kk
                        fw.tr(pb[:, kk * 128:(kk + 1) * 128], xt[s][:, k * 128:(k + 1) * 128], ident,
                              R=[t_xt[s], t_c], W=[pt])
                    fw.copy("act" if hf == 0 else "dve", xo[s][:, hf * 4:(hf + 1) * 4, :],
                            pb[:, :].rearrange("p (a b) -> p a b", a=4), R=[pt], W=[t_xo[s]])
                fw.dma("sp", xs_d[:, :, i * 128:(i + 1) * 128], xo[s], R=[t_xo[s]], W=[xs_t[bi]])
            fw.barrier()
            ar.release(m)
        def phase_mod(l):
            m = ar.mark()
            wm = [ar.alloc([KC, 512], F32) for _ in range(2)]; t_wm = [Trk() for _ in range(2)]
            pb, pt = accr.next()
            wsrc = dr["w_mod"][l].rearrange("(k p) n -> p k n", p=128)
            for g in range(12):
                s = g % 2
                fw.dma("sp", wm[s], wsrc[:, :, g * 512:(g + 1) * 512], W=[t_wm[s]])
                for jj in range(4):
                    j = g * 4 + jj
                    for k in range(KC):
                        fw.mm(pb[:, 2 * j:2 * j + 2], wm[s][:, k, jj * 128:(jj + 1) * 128], actc[:, 2 * k:2 * k + 2],
                              start=(k == 0), stop=(k == KC - 1), R=[t_wm[s], t_c], W=[pt])
            bm = pvc(("b_mod", l), 0, 48)
            for s in range(2):
                fw.tt("dve", modsb[:, s:96:2], pb[:, s:96:2], bm, ALU.add, R=[pt, t_pv], W=[t_mod])
            def modv(mi):
                return modsb[:, mi * 16:(mi + 1) * 16]
            for (di, mi_scale, gkey) in ((0, 1, ("norm_mix", l)), (3, 4, ("norm_ffn", l))):
                for s in range(2):
                    fw.stt("dve", der[:, di, s:16:2], modv(mi_scale)[:, s:16:2], 1.0, pvc(gkey, 0, 8), ALU.add, ALU.mult,
                           R=[t_mod, t_pv], W=[t_der])
            for (di, mi) in ((1, 0), (2, 2), (4, 3), (5, 5)):
                fw.copy("dve", der[:, di, :], modv(mi), R=[t_mod], W=[t_der])
            tmp = ar.alloc([6, 8], F32); t_tmp = Trk()
            lam = pvc(("lru_lam", l), 0, 8)
            e_, l_, p_, mk, a_, b_ = (tmp[:, i, :] for i in range(6))
            fw.act(e_, lam, AF.Exp, scale=-1.0, R=[t_pv], W=[t_tmp])
            fw.act(l_, e_, AF.Ln, bias=1.0, R=[t_tmp], W=[t_tmp])
            fw.ts("dve", p_, e_, -0.2, 0.25, ALU.mult, ALU.add, R=[t_tmp], W=[t_tmp])
            for cst in (1.0 / 3.0, 0.5, 1.0):
                fw.tt("dve", p_, p_, e_, ALU.mult, R=[t_tmp], W=[t_tmp])
                fw.ts("dve", p_, p_, -1.0, cst, ALU.mult, ALU.add, R=[t_tmp], W=[t_tmp])
            fw.tt("dve", p_, p_, e_, ALU.mult, R=[t_tmp], W=[t_tmp])
            fw.ts("dve", mk, e_, 0.1, None, ALU.is_lt, R=[t_tmp], W=[t_tmp])
            fw.tt("dve", a_, p_, l_, ALU.subtract, R=[t_tmp], W=[t_tmp])
            fw.tt("dve", a_, a_, mk, ALU.mult, R=[t_tmp], W=[t_tmp])
            fw.tt("dve", a_, a_, l_, ALU.add, R=[t_tmp], W=[t_tmp])
            fw.ts("dve", cneg, a_, -8.0, None, ALU.mult, R=[t_tmp], W=[t_cneg])
            fw.ts("dve", cneg2, a_, -16.0, None, ALU.mult, R=[t_tmp, t_cneg], W=[t_cneg])
            fw.barrier()
            ar.release(m)
        def rms_rstd(ssq_ps, pt, n, inv_n, rstd, t_rstd, parts=128):
            fw.act(rstd[0:parts, 0:n], ssq_ps[0:parts, 0:n], AF.Ln, bias=eps_ap[0:parts, :], scale=inv_n,
                   R=[pt, t_pv], W=[t_rstd])
            fw.act(rstd[0:parts, 0:n], rstd[0:parts, 0:n], AF.Exp, scale=-0.5, R=[t_rstd], W=[t_rstd])
        def phase_norm(l, which, blocks, combT=None, t_comb=None):
            m = ar.mark()
            di = 0 if which == 1 else 3
            xb = [ar.alloc([KC, 512], F32) for _ in range(2)]; t_xb = [Trk() for _ in range(2)]
            sq = ar.alloc([KC, 512], F32); t_sq = Trk()
            rstd = ar.alloc([512], F32); t_rstd = Trk()
            tmp = [ar.alloc([512], F32) for _ in range(2)]; t_tmp = [Trk() for _ in range(2)]
            if which == 2:
                h32 = ar.alloc([KC, 512], F32); t_h32 = Trk()
                rts = [ar.alloc([8, NE], F32) for _ in range(4)]; t_rts = [Trk() for _ in range(4)]
            for bi in blocks:
                t0, nb = BLOCKS[bi]
                s = blk_s(bi)
                bs = bi % 2
                fw.dma("sp", xb[bs][:, :, 0:nb], xs_d[:, :, t0:t0 + nb], R=[xs_t[bi]], W=[t_xb[bs]], key=t_xb[bs])
                fw.act(sq[:, :, 0:nb], xb[bs][:, :, 0:nb], AF.Square, R=[t_xb[bs]], W=[t_sq])
                pb, pt = ring.next()
                for k in range(KC):
                    fw.mm(pb[:, 0:nb], ones32, sq[:, k, 0:nb], start=(k == 0), stop=(k == KC - 1), R=[t_sq, t_c], W=[pt])
                rms_rstd(pb, pt, nb, 1.0 / D, rstd, t_rstd)
                for k in range(KC):
                    ts_ = k % 2
                    fw.stt("dve", tmp[ts_][:, 0:nb], xb[bs][:, k, 0:nb], der[:, di, 2 * k + s:2 * k + s + 1], rstd[:, 0:nb],
                           ALU.mult, ALU.mult, R=[t_xb[bs], t_der, t_rstd], W=[t_tmp[ts_]])
                    sh = der[:, di + 1, 2 * k + s:2 * k + s + 1]
                    if which == 1:
                        fw.act(hT[:, k, t0:t0 + nb], tmp[ts_][:, 0:nb], AF.Identity, bias=sh, R=[t_tmp[ts_], t_der], W=[t_h[bi]])
                    else:
                        fw.act(h32[:, k, 0:nb], tmp[ts_][:, 0:nb], AF.Identity, bias=sh, R=[t_tmp[ts_], t_der], W=[t_h32])
                        fw.copy("pool", hT[:, k, t0:t0 + nb], h32[:, k, 0:nb], R=[t_h32], W=[t_h[bi]])
                if which == 2:
                    ntl = nb // 128
                    chains = []
                    for ti in range(ntl):
                        pb, pt = ring.next()
                        for k in range(KC):
                            fw.mm(pb[:, 0:NE], h32[:, k, ti * 128:(ti + 1) * 128], rw[:, k, :], start=(k == 0),
                                  stop=(k == KC - 1), R=[t_h32, t_c], W=[pt])
                        rt_ = rts[ti]; trt = t_rts[ti]
                        sc, sel, tm, sel2, m1, m2, wsel, comb = (rt_[:, i, :] for i in range(8))
                        R_ = [trt]
                        g4 = lambda a: a.rearrange("p (g j) -> p g j", g=4)
                        ops = []
                        A = ops.append
                        A(lambda sc=sc, pb=pb, pt=pt, trt=trt: fw.act(sc, pb[:, 0:NE], AF.Sigmoid, R=[pt], W=[trt]))
                        A(lambda sel=sel, sc=sc, trt=trt: fw.tt("dve", sel, sc, rbias, ALU.add, R=[trt, t_c], W=[trt]))
                        A(lambda m1=m1, sel=sel, trt=trt, g4=g4: fw.reduce(m1[:, 0:4], g4(sel), ALU.max, R=[trt], W=[trt]))
                        for g in range(4):
                            A(lambda g=g, tm=tm, sel=sel, m1=m1, trt=trt: fw.ts("dve", tm[:, 4 * g:4 * g + 4], sel[:, 4 * g:4 * g + 4],
                                                                             m1[:, g:g + 1], -1e9, ALU.is_equal, ALU.mult, R=[trt], W=[trt]))
                        A(lambda sel2=sel2, sel=sel, tm=tm, trt=trt: fw.tt("dve", sel2, sel, tm, ALU.add, R=[trt], W=[trt]))
                        A(lambda m2=m2, sel2=sel2, trt=trt, g4=g4: fw.reduce(m2[:, 0:4], g4(sel2), ALU.max, R=[trt], W=[trt]))
                        A(lambda m1=m1, m2=m2, trt=trt: fw.tt("dve", m1[:, 4:8], m1[:, 0:4], m2[:, 0:4], ALU.add, R=[trt], W=[trt]))
                        A(lambda m1=m1, trt=trt: fw.reduce(m1[:, 8:9], m1[:, 4:8], ALU.max, R=[trt], W=[trt]))
                        A(lambda m1=m1, trt=trt: fw.ts("dve", m1[:, 12:16], m1[:, 4:8], m1[:, 8:9], None, ALU.is_equal, R=[trt], W=[trt]))
                        for g in range(4):
                            A(lambda g=g, tm=tm, sel=sel, m1=m1, m2=m2, trt=trt: fw.ts("dve", tm[:, 4 * g:4 * g + 4], sel[:, 4 * g:4 * g + 4],
                                                                                    m2[:, g:g + 1], m1[:, 12 + g:13 + g], ALU.is_ge, ALU.mult,
                                                                                    R=[trt], W=[trt]))
                        A(lambda wsel=wsel, tm=tm, sc=sc, trt=trt: fw.tt("dve", wsel, tm, sc, ALU.mult, R=[trt], W=[trt]))
                        A(lambda m2=m2, wsel=wsel, trt=trt: fw.reduce(m2[:, 8:9], wsel, ALU.add, R=[trt], W=[trt]))
                        A(lambda m2=m2, trt=trt: fw.recip(m2[:, 9:10], m2[:, 8:9], R=[trt], W=[trt]))
                        A(lambda comb=comb, wsel=wsel, m2=m2, trt=trt: fw.ts("dve", comb, wsel, m2[:, 9:10], None, ALU.mult, R=[trt], W=[trt]))
                        chains.append((ops, comb, trt, ti))
                    for k in range(max(len(c[0]) for c in chains)):
                        for ops, _, _, _ in chains:
                            if k < len(ops):
                                ops[k]()
                    for ops, comb, trt, ti in chains:
                        pb2, pt2 = ring.next()
                        fw.tr(pb2[0:NE, 0:128], comb, ident, R=[trt, t_c], W=[pt2])
                        c0 = t0 + ti * 128
                        fw.copy("act", combT[0:NE, c0:c0 + 128], pb2[0:NE, 0:128], R=[pt2], W=[t_comb])
            fw.barrier()
            ar.release(m)
        WTR = {}
        def wt(name):
            if name not in WTR:
                WTR[name] = Trk(name)
            return WTR[name]
        pend_loads = []
        def load_w(dst, src, trk, R=()):
            if len(pend_loads) >= NLOADS:
                fw._wait("pool", pend_loads.pop(0))
            fw.dma("pool", dst, src, R=list(R), W=[trk])
            pend_loads.append(trk.w)
        def win(l, c0, c1):
            return dr["w_in"][l].rearrange("(k p) n -> p k n", p=128)[:, :, c0:c1]
        def phase_lru(l, o_lru, t_olru):
            m = ar.mark()
            wz = ar.alloc([KC, 1024], BF16); t_wz = wt("wz0"); t_wz1 = wt("wz1")
            load_w(wz[:, :, 0:512], win(l, C_ZX, C_ZX + 512), t_wz)
            load_w(wz[:, :, 512:1024], win(l, C_ZY, C_ZY + 512), t_wz1)
            lw = ar.alloc([16, 128], BF16); t_lw = wt("lw")
            load_w(lw, dr["lruW"][l].rearrange("d g c p m -> p (d g c) m"), t_lw)
            ZP = T + 6
            zx = ar.alloc([ZP], F32); t_zx = Trk()
            u = ar.alloc([T], F32); t_u = Trk()
            ub = ar.alloc([T], BF16); t_ub = Trk()
            gy = ar.alloc([T], F32); t_gy = Trk()
            a_ = ar.alloc([T], F32); t_a = Trk()
            b_ = ar.alloc([T], F32); t_b = Trk()
            hf = ar.alloc([T], F32); t_hf = Trk()
            hb = ar.alloc([T], F32); t_hb = Trk()
            tq = [ar.alloc([512], F32) for _ in range(4)]; t_tq = [Trk() for _ in range(4)]
            fw.memset("pool", zx, 0.0, W=[t_zx])
            for c in range(4):
                for bi, (t0, nb) in enumerate(BLOCKS):
                    pb, pt = ring.next()
                    for k in range(KC):
                        fw.mm(pb[:, 0:nb], wz[:, k, c * 128:(c + 1) * 128], hT[:, k, t0:t0 + nb], start=(k == 0),
                              stop=(k == KC - 1), R=[t_wz, t_h[bi]], W=[pt])
                    zo = 1 if bi == 0 else t0 + 4
                    fw.copy("act", zx[:, zo:zo + nb], pb[:, 0:nb], R=[pt], W=[t_zx])
                for bi, (t0, nb) in enumerate(BLOCKS):
                    pb, pt = ring.next()
                    for k in range(KC):
                        fw.mm(pb[:, 0:nb], wz[:, k, 512 + c * 128:512 + (c + 1) * 128], hT[:, k, t0:t0 + nb], start=(k == 0),
                              stop=(k == KC - 1), R=[t_wz1, t_h[bi]], W=[pt])
                    fw.act(gy[:, t0:t0 + nb], pb[:, 0:nb], AF.Gelu_apprx_tanh, R=[pt], W=[t_gy])
                for (d0, n, base) in ((0, NCTX, 1), (NCTX, SEQ, 260)):
                    for j in range(4):
                        wj = pvc(("conv_w", l), j * 4 + c)
                        src = zx[:, base - 1 + j:base - 1 + j + n]
                        if j == 0:
                            fw.ts("dve", u[:, d0:d0 + n], src, wj, pvc(("conv_b", l), c), ALU.mult, ALU.add,
                                  R=[t_zx, t_pv], W=[t_u])
                        else:
                            fw.stt("dve", u[:, d0:d0 + n], src, wj, u[:, d0:d0 + n], ALU.mult, ALU.add,
                                   R=[t_zx, t_pv, t_u], W=[t_u])
                fw.copy("act", ub, u, R=[t_u], W=[t_ub])
                for dr_ in range(2):
                    gi = dr_ * 4 + c
                    for bi, (t0, nb) in enumerate(BLOCKS):
                        pb, pt = ring.next()
                        fw.mm(pb[:, 0:nb], lw[:, (dr_ * 2 + 0) * 4 + c, :], ub[:, t0:t0 + nb], R=[t_lw, t_ub], W=[pt])
                        fw.act(a_[:, t0:t0 + nb], pb[:, 0:nb], AF.Sigmoid, bias=pvc(("lru_b_a", l), gi), R=[pt, t_pv], W=[t_a])
                    for bi, (t0, nb) in enumerate(BLOCKS):
                        pb, pt = ring.next()
                        fw.mm(pb[:, 0:nb], lw[:, (dr_ * 2 + 1) * 4 + c, :], ub[:, t0:t0 + nb], R=[t_lw, t_ub], W=[pt])
                        fw.act(b_[:, t0:t0 + nb], pb[:, 0:nb], AF.Sigmoid, bias=pvc(("lru_b_i", l), gi), R=[pt, t_pv], W=[t_b])
                    for bi, (t0, nb) in enumerate(BLOCKS):
                        fw.act(hb[:, t0:t0 + nb], a_[:, t0:t0 + nb], AF.Exp, scale=cneg2[:, gi:gi + 1], R=[t_a, t_cneg], W=[t_hb])
                    for bi, (t0, nb) in enumerate(BLOCKS):
                        fw.act(a_[:, t0:t0 + nb], a_[:, t0:t0 + nb], AF.Exp, scale=cneg[:, gi:gi + 1], R=[t_a, t_cneg], W=[t_a])
                    fw.tt("dve", b_, b_, u, ALU.mult, R=[t_b, t_u], W=[t_b])
                    for bi, (t0, nb) in enumerate(BLOCKS):
                        fw.act(hb[:, t0:t0 + nb], hb[:, t0:t0 + nb], AF.Sqrt, bias=1.0, scale=-1.0, R=[t_hb], W=[t_hb])
                    fw.tt("dve", b_, b_, hb, ALU.mult, R=[t_b, t_hb], W=[t_b])
                    if dr_ == 0:
                        fw.scan(hf, a_, b_, 0.0, R=[t_a, t_b], W=[t_hf])
                    else:
                        fw.scan(hb[:, 0:NCTX][:, ::-1], a_[:, 0:NCTX][:, ::-1], b_[:, 0:NCTX][:, ::-1], 0.0,
                                R=[t_a, t_b], W=[t_hb])
                        fw.scan(hb[:, NCTX:T][:, ::-1], a_[:, NCTX:T][:, ::-1], b_[:, NCTX:T][:, ::-1], hb[:, 0:1],
                                R=[t_a, t_b, t_hb], W=[t_hb])
                fw.tt("dve", hf, hf, hb, ALU.add, R=[t_hf, t_hb], W=[t_hf])
                fw.tt("dve", o_lru[:, c, :], hf, gy, ALU.mult, R=[t_hf, t_gy], W=[t_olru])
            fw.barrier()
            ar.release(m)
        def norm_rope(pb, pt, nb, t0, gain, dst, dst_t, wk):
            (sq, t_sq), (rstd, t_rstd), (kn, t_kn), (cs, t_cs), (t1, t_t1), (t2, t_t2) = wk
            fw.act(sq[:, 0:nb], pb[:, 0:nb], AF.Square, R=[pt], W=[t_sq])
            pb2, pt2 = ring.next()
            fw.mm(pb2[:, 0:nb], bones, sq[:, 0:nb], R=[t_sq, t_c], W=[pt2])
            rms_rstd(pb2, pt2, nb, 1.0 / 64.0, rstd, t_rstd)
            fw.stt("dve", kn[:, 0:nb], pb[:, 0:nb], gain, rstd[:, 0:nb], ALU.mult, ALU.mult, R=[pt, t_pv, t_rstd], W=[t_kn])
            pb3, pt3 = ring.next()
            fw.mm(pb3[:, 0:nb], permG, kn[:, 0:nb], R=[t_kn, t_c], W=[pt3])
            fw.dma("sp", cs[:, :, 0:nb], dr["ropeG"][:, :, t0:t0 + nb].rearrange("c p t -> p c t"), W=[t_cs])
            fw.tt("dve", t1[:, 0:nb], kn[:, 0:nb], cs[:, 0, 0:nb], ALU.mult, R=[t_kn, t_cs], W=[t_t1])
            fw.tt("dve", t2[:, 0:nb], pb3[:, 0:nb], cs[:, 1, 0:nb], ALU.mult, R=[pt3, t_cs], W=[t_t2])
            if isinstance(dst, list):
                for (dap, p0, p1) in dst:
                    fw.tt("pool", dap, t1[p0:p1, 0:nb], t2[p0:p1, 0:nb], ALU.add, R=[t_t1, t_t2], W=[dst_t])
            else:
                fw.tt("pool", dst, t1[:, 0:nb], t2[:, 0:nb], ALU.add, R=[t_t1, t_t2], W=[dst_t])
        def rope_m(src, t_src, nb, t0, dst, dst_t, wk, dst_parts=(0, 96)):
            (cs, t_cs), (t1, t_t1), (t2, t_t2) = wk
            pb3, pt3 = ring.next()
            fw.mm(pb3[0:96, 0:nb], permM, src[0:96, 0:nb], R=[t_src, t_c], W=[pt3])
            fw.dma("sp", cs[0:96, :, 0:nb], dr["ropeM"][:, :, t0:t0 + nb].rearrange("c p t -> p c t"), W=[t_cs])
            fw.tt("dve", t1[0:96, 0:nb], src[0:96, 0:nb], cs[0:96, 0, 0:nb], ALU.mult, R=[t_src, t_cs], W=[t_t1])
            fw.tt("dve", t2[0:96, 0:nb], pb3[0:96, 0:nb], cs[0:96, 1, 0:nb], ALU.mult, R=[pt3, t_cs], W=[t_t2])
            p0, p1 = dst_parts
            fw.tt("pool", dst, t1[p0:p1, 0:nb], t2[p0:p1, 0:nb], ALU.add, R=[t_t1, t_t2], W=[dst_t])
        def mk_work():
            sq = ar.alloc([3, 512], F32); rstd = ar.alloc([512], F32); kn = ar.alloc([512], BF16)
            cs = ar.alloc([2, 512], F32)
            t_cs = Trk(); t_sq = Trk()
            return dict(sq=(sq, t_sq), rstd=(rstd, Trk()), kn=(kn, Trk()), cs=(cs, t_cs), t1=(sq[:, 1, :], t_sq), t2=(sq[:, 2, :], t_sq),
                        csm=(cs, t_cs))
        def phase_kv(l, kv):
            m = ar.mark()
            kTg, Vg, kTm, Vm, t_kTg, t_Vg, t_kTm, t_Vm = kv
            wk_ = ar.alloc([KC, 128], BF16); wv_ = ar.alloc([KC, 128], BF16); wc_ = ar.alloc([KC, 256], BF16)
            wr_ = ar.alloc([KC, 96], BF16); wkvb = ar.alloc([2, 1024], BF16)
            t_wk, t_wv, t_wc, t_wr, t_wkvb = wt("wk"), wt("wv"), wt("wc"), wt("wr"), wt("wkvb")
            fw.memset("pool", wr_, 0.0, W=[t_wr])
            load_w(wk_, win(l, C_K, C_K + 128), t_wk)
            load_w(wv_, win(l, C_V, C_V + 128), t_wv)
            load_w(wc_, win(l, C_CKV, C_CKV + 256), t_wc)
            load_w(wr_[:, :, 64:96], win(l, C_KR, C_KR + 32), t_wr)
            load_w(wkvb, dr["w_kvb"][l].rearrange("(j p) n -> p j n", p=128), t_wkvb)
            fw.memset("pool", Vg[:, :, :, 64:65], 1.0, W=[t_Vg])
            fw.memset("pool", Vm[:, :, :, 64:65], 1.0, W=[t_Vm])
            W_ = mk_work()
            ckvT = ar.alloc([2, 512], BF16); t_ckv = Trk()
            krs = ar.alloc([512], BF16); t_krs = Trk()
            krr = ar.alloc([512], BF16); t_krr = Trk()
            sq, t_sq = W_["sq"]; rstd, t_rstd = W_["rstd"]
            for bi, (t0, nb) in enumerate(BLOCKS):
                pb, pt = ring.next()
                for k in range(KC):
                    fw.mm(pb[:, 0:nb], wk_[:, k, :], hT[:, k, t0:t0 + nb], start=(k == 0), stop=(k == KC - 1),
                          R=[t_wk, t_h[bi]], W=[pt])
                norm_rope(pb, pt, nb, t0, pvc(("gk", l)), kTg[:, t0:t0 + nb], t_kTg,
                          ((sq[:, 0, :], t_sq), W_["rstd"], W_["kn"], W_["cs"], W_["t1"], W_["t2"]))
                for ti in range(nb // 128):
                    tt_ = t0 // 128 + ti
                    pb, pt = ring.next()
                    for k in range(KC):
                        fw.mm(pb[:, 0:128], hT[:, k, tt_ * 128:(tt_ + 1) * 128], wv_[:, k, :], start=(k == 0), stop=(k == KC - 1),
                              R=[t_wv, t_h[bi]], W=[pt])
                    fw.copy("act", Vg[:, tt_, :, 0:64], pb[:, 0:128].rearrange("p (h d) -> p h d", h=2), R=[pt], W=[t_Vg])
                pbs = [ring.next() for _ in range(2)]
                for j in range(2):
                    for k in range(KC):
                        fw.mm(pbs[j][0][:, 0:nb], wc_[:, k, j * 128:(j + 1) * 128], hT[:, k, t0:t0 + nb], start=(k == 0),
                              stop=(k == KC - 1), R=[t_wc, t_h[bi]], W=[pbs[j][1]])
                    fw.act(sq[:, j, 0:nb], pbs[j][0][:, 0:nb], AF.Square, R=[pbs[j][1]], W=[t_sq])
                pb2, pt2 = ring.next()
                for j in range(2):
                    fw.mm(pb2[:, 0:nb], ones32, sq[:, j, 0:nb], start=(j == 0), stop=(j == 1), R=[t_sq, t_c], W=[pt2])
                rms_rstd(pb2, pt2, nb, 1.0 / 256.0, rstd, t_rstd)
                for j in range(2):
                    fw.stt("dve", ckvT[:, j, 0:nb], pbs[j][0][:, 0:nb], pvc(("gckv", l), j), rstd[:, 0:nb], ALU.mult, ALU.mult,
                           R=[pbs[j][1], t_pv, t_rstd], W=[t_ckv])
                for h in range(8):
                    pb, pt = ring.next()
                    for j in range(2):
                        fw.mm(pb[0:64, 0:nb], wkvb[:, j, h * 64:(h + 1) * 64], ckvT[:, j, 0:nb], start=(j == 0), stop=(j == 1),
                              R=[t_wkvb, t_ckv], W=[pt])
                    fw.copy("act" if h % 2 else "dve", kTm[0:64, h, t0:t0 + nb], pb[0:64, 0:nb], R=[pt], W=[t_kTm])
                for ti in range(nb // 128):
                    tt_ = t0 // 128 + ti
                    pb, pt = ring.next()
                    for j in range(2):
                        fw.mm(pb[:, 0:512], ckvT[:, j, ti * 128:(ti + 1) * 128], wkvb[:, j, 512:1024], start=(j == 0), stop=(j == 1),
                              R=[t_wkvb, t_ckv], W=[pt])
                    fw.copy("act" if ti % 2 else "dve", Vm[:, tt_, :, 0:64], pb[:, 0:512].rearrange("p (h d) -> p h d", h=8),
                            R=[pt], W=[t_Vm])
                pb, pt = ring.next()
                for k in range(KC):
                    fw.mm(pb[0:96, 0:nb], wr_[:, k, :], hT[:, k, t0:t0 + nb], start=(k == 0), stop=(k == KC - 1),
                          R=[t_wr, t_h[bi]], W=[pt])
                fw.copy("act", krs[0:96, 0:nb], pb[0:96, 0:nb], R=[pt], W=[t_krs])
                rope_m(krs, t_krs, nb, t0, krr[64:96, 0:nb], t_krr, (W_["csm"], W_["t1"], W_["t2"]), dst_parts=(64, 96))
                for h in range(8):
                    fw.copy("pool" if h % 2 else "dve", kTm[64:96, h, t0:t0 + nb], krr[64:96, 0:nb], R=[t_krr], W=[t_kTm])
            fw.barrier()
            ar.release(m)
        def phase_attn(l, blocks, kv, o_attn, t_oattn, o_mla, t_omla):
            m = ar.mark()
            kTg, Vg, kTm, Vm, t_kTg, t_Vg, t_kTm, t_Vm = kv
            wq = ar.alloc([KC, 512], BF16); wcq = ar.alloc([KC, 384], BF16); wqb = ar.alloc([3, 768], BF16)
            t_wq, t_wcq, t_wqb = wt("wq"), wt("wcq"), wt("wqb")
            load_w(wq, win(l, C_Q, C_Q + 512), t_wq)
            load_w(wcq, win(l, C_CQ, C_CQ + 384), t_wcq)
            load_w(wqb, dr["w_qb"][l].rearrange("(j p) n -> p j n", p=128), t_wqb)
            W_ = mk_work()
            sq, t_sq = W_["sq"]; rstd, t_rstd = W_["rstd"]
            qTg = ar.alloc([8, 512], BF16); t_qTg = [Trk() for _ in range(4)]
            for j in range(4):
                fw.memset("pool", qTg[:, 2 * j:2 * j + 2, :], 0.0, W=[t_qTg[j]])
            cqT = ar.alloc([3, 512], BF16); t_cq = Trk()
            qraw = ar.alloc([512], BF16); t_qraw = Trk()
            qm = [ar.alloc([512], BF16) for _ in range(2)]; t_qm = [Trk() for _ in range(2)]
            pT = [ar.alloc([512], BF16) for _ in range(2)]; t_pT = [Trk() for _ in range(2)]
            osb = ar.alloc([512], F32); t_osb = Trk()
            fw.memset("pool", osb, 0.0, W=[t_osb])
            LA = 2
            for bi in blocks:
                t0, nb = BLOCKS[bi]
                ktiles = list(range(2)) if bi == 0 else list(range(NT))
                nk = len(ktiles)
                for j in range(4):
                    pb, pt = ring.next()
                    for k in range(KC):
                        fw.mm(pb[:, 0:nb], wq[:, k, j * 128:(j + 1) * 128], hT[:, k, t0:t0 + nb], start=(k == 0), stop=(k == KC - 1),
                              R=[t_wq, t_h[bi]], W=[pt])
                    norm_rope(pb, pt, nb, t0, pvc(("gq", l)),
                              [(qTg[0:64, 2 * j, 0:nb], 0, 64), (qTg[64:128, 2 * j + 1, 0:nb], 64, 128)], t_qTg[j],
                              ((sq[:, 0, :], t_sq), W_["rstd"], W_["kn"], W_["cs"], W_["t1"], W_["t2"]))
                pbs = [ring.next() for _ in range(3)]
                for j in range(3):
                    for k in range(KC):
                        fw.mm(pbs[j][0][:, 0:nb], wcq[:, k, j * 128:(j + 1) * 128], hT[:, k, t0:t0 + nb], start=(k == 0),
                              stop=(k == KC - 1), R=[t_wcq, t_h[bi]], W=[pbs[j][1]])
                    fw.act(sq[:, j, 0:nb], pbs[j][0][:, 0:nb], AF.Square, R=[pbs[j][1]], W=[t_sq])
                pb2, pt2 = ring.next()
                for j in range(3):
                    fw.mm(pb2[:, 0:nb], ones32, sq[:, j, 0:nb], start=(j == 0), stop=(j == 2), R=[t_sq, t_c], W=[pt2])
                rms_rstd(pb2, pt2, nb, 1.0 / 384.0, rstd, t_rstd)
                for j in range(3):
                    fw.stt("dve", cqT[:, j, 0:nb], pbs[j][0][:, 0:nb], pvc(("gcq", l), j), rstd[:, 0:nb], ALU.mult, ALU.mult,
                           R=[pbs[j][1], t_pv, t_rstd], W=[t_cq])
                jobs = []
                for j in range(4):
                    for half in range(2):
                        hd = j + 4 * half
                        p0 = 64 * half
                        jobs.append(dict(
                            mla=None,
                            k_of=(lambda kt: kTg[:, kt * 128:(kt + 1) * 128]),
                            q_ap=qTg[:, 2 * j + half, 0:nb],
                            v_of=(lambda kt, half=half: Vg[:, kt, half, 0:65]),
                            scale=0.125,
                            dst=o_attn[64 * (hd % 2):64 * (hd % 2) + 64, hd // 2, t0:t0 + nb], dst_t=t_oattn,
                            Rk=[t_kTg], Rq=[t_qTg[j]], Rv=[t_Vg]))
                for h in range(8):
                    s = h % 2
                    jobs.append(dict(
                        mla=h,
                        k_of=(lambda kt, h=h: kTm[0:96, h, kt * 128:(kt + 1) * 128]),
                        q_ap=qm[s][0:96, 0:nb],
                        v_of=(lambda kt, h=h: Vm[:, kt, h, 0:65]),
                        scale=96.0 ** -0.5,
                        dst=o_mla[64 * (h % 2):64 * (h % 2) + 64, h // 2, t0:t0 + nb], dst_t=t_omla,
                        Rk=[t_kTm], Rq=[t_qm[s]], Rv=[t_Vm]))

                def prep_a(job):
                    h = job["mla"]
                    if h is None:
                        return
                    pb, pt = ring.next()
                    for j in range(3):
                        fw.mm(pb[0:96, 0:nb], wqb[:, j, h * 96:(h + 1) * 96], cqT[:, j, 0:nb], start=(j == 0), stop=(j == 2),
                              R=[t_wqb, t_cq], W=[pt])
                    fw.copy("dve", qraw[0:96, 0:nb], pb[0:96, 0:nb], R=[pt], W=[t_qraw])

                def prep_b(job):
                    h = job["mla"]
                    if h is None:
                        return
                    s = h % 2
                    rope_m(qraw, t_qraw, nb, t0, qm[s][0:96, 0:nb], t_qm[s], (W_["csm"], W_["t1"], W_["t2"]))

                def emit_S(job, kt):
                    pb, pt = ring.next()
                    fw.mm(pb[:, 0:nb], job["k_of"](kt), job["q_ap"], R=job["Rk"] + job["Rq"], W=[pt])
                    return pb, pt

                def finish_a(job, acc, acct):
                    fw.copy("dve", osb[0:65, 0:nb], acc[0:65, 0:nb], R=[acct], W=[t_osb])
                    fw.recip(osb[64:65, 0:nb], osb[64:65, 0:nb], R=[t_osb], W=[t_osb])

                def finish_b(job):
                    pb, pt = ring.next()
                    fw.mm(pb[0:64, 0:nb], sel64, osb[:, 0:nb], R=[t_osb, t_c], W=[pt])
                    fw.tt("dve", job["dst"], osb[0:64, 0:nb], pb[0:64, 0:nb], ALU.mult, R=[t_osb, pt], W=[job["dst_t"]])

                flat = [(ji, ki) for ji in range(len(jobs)) for ki in range(nk)]
                done_a = set(); done_b = set()

                def ensure_prep(jx, upto_b=True):
                    if jx not in done_a:
                        prep_a(jobs[jx]); done_a.add(jx)
                    if upto_b and jx not in done_b:
                        prep_b(jobs[jx]); done_b.add(jx)

                inflight = []
                for i0 in range(min(LA, len(flat))):
                    ji, ki = flat[i0]
                    ensure_prep(ji)
                    inflight.append(emit_S(jobs[ji], ktiles[ki]))
                pend_fin = None
                acc = acct = None
                kb = min(8, nk - 1)
                for i, (ji, ki) in enumerate(flat):
                    job = jobs[ji]
                    if ki == 0:
                        acc, acct = accr.next()
                        if ji + 1 < len(jobs):
                            ensure_prep(ji + 1, upto_b=False)
                    if ki == kb and ji + 1 < len(jobs):
                        ensure_prep(ji + 1)
                    if i + LA < len(flat):
                        ji2, ki2 = flat[i + LA]
                        ensure_prep(ji2)
                        inflight.append(emit_S(jobs[ji2], ktiles[ki2]))
                    pb, pt = inflight.pop(0)
                    s = i % 2
                    fw.act(pT[s][:, 0:nb], pb[:, 0:nb], AF.Exp, scale=job["scale"], R=[pt], W=[t_pT[s]])
                    fw.mm(acc[0:65, 0:nb], job["v_of"](ktiles[ki]), pT[s][:, 0:nb], start=(ki == 0), stop=(ki == nk - 1),
                          R=job["Rv"] + [t_pT[s]], W=[acct])
                    if ki == min(10, nk - 1) and pend_fin is not None:
                        finish_b(pend_fin)
                        pend_fin = None
                    if ki == nk - 1:
                        if pend_fin is not None:
                            finish_b(pend_fin)
                        finish_a(job, acc, acct)
                        pend_fin = job
                if pend_fin is not None:
                    finish_b(pend_fin)
            fw.barrier()
            ar.release(m)
        def phase_merge(l, blocks, outs, merged, t_mg):
            m = ar.mark()
            wg = ar.alloc([3, KC, 512], BF16); wb = ar.alloc([3, 4, 512], BF16)
            sg = [ar.alloc([512], F32) for _ in range(2)]; t_sg = [Trk() for _ in range(2)]
            mm_ = ar.alloc([512], F32); t_mm = Trk()
            tt2 = ar.alloc([512], F32); t_tt2 = Trk()
            wnames = ("w_ba", "w_bl", "w_bm")
            t_wg = [wt("wg%d" % i) for i in range(3)]; t_wb = [wt("wb%d" % i) for i in range(3)]
            for half in range(2):
                for br in range(3):
                    c0 = C_G + br * 1024 + half * 512
                    load_w(wg[:, br, :, :], win(l, c0, c0 + 512), t_wg[br])
                    load_w(wb[:, br, :, :], dr[wnames[br]][l].rearrange("(j p) n -> p j n", p=128)[:, :, half * 512:(half + 1) * 512], t_wb[br])
                for bi in blocks:
                    t0, nb = BLOCKS[bi]
                    for ff in range(4):
                        f = half * 4 + ff
                        for br in range(3):
                            o_br, t_obr = outs[br]
                            pg_, ptg = ring.next()
                            for k in range(KC):
                                fw.mm(pg_[:, 0:nb], wg[:, br, k, ff * 128:(ff + 1) * 128], hT[:, k, t0:t0 + nb], start=(k == 0),
                                      stop=(k == KC - 1), R=[t_wg[br], t_h[bi]], W=[ptg])
                            s = br % 2
                            fw.act(sg[s][:, 0:nb], pg_[:, 0:nb], AF.Sigmoid, R=[ptg], W=[t_sg[s]])
                            pb, pt = ring.next()
                            for j in range(4):
                                fw.mm(pb[:, 0:nb], wb[:, br, j, ff * 128:(ff + 1) * 128], o_br[:, j, t0:t0 + nb], start=(j == 0),
                                      stop=(j == 3), R=[t_wb[br], t_obr], W=[pt])
                            if br == 0:
                                fw.tt("dve", mm_[:, 0:nb], sg[s][:, 0:nb], pb[:, 0:nb], ALU.mult, R=[t_sg[s], pt], W=[t_mm])
                            else:
                                fw.tt("dve", tt2[:, 0:nb], sg[s][:, 0:nb], pb[:, 0:nb], ALU.mult, R=[t_sg[s], pt], W=[t_tt2])
                                if br == 1:
                                    fw.tt("pool", mm_[:, 0:nb], mm_[:, 0:nb], tt2[:, 0:nb], ALU.add, R=[t_mm, t_tt2], W=[t_mm])
                                else:
                                    fw.tt("pool", merged[:, f, t0:t0 + nb], mm_[:, 0:nb], tt2[:, 0:nb], ALU.add,
                                          R=[t_mm, t_tt2], W=[t_mg[bi]])
            fw.barrier()
            ar.release(m)
        def phase_wout(l, blocks, merged, t_mg):
            m = ar.mark()
            wo = ar.alloc([KC, D], BF16); t_wo = [wt("wo0"), wt("wo1")]
            wsrc = dr["w_out"][l].rearrange("(k p) n -> p k n", p=128)
            load_w(wo[:, :, 0:512], wsrc[:, :, 0:512], t_wo[0])
            load_w(wo[:, :, 512:1024], wsrc[:, :, 512:1024], t_wo[1])
            xb = [ar.alloc([KC, 512], F32) for _ in range(2)]; t_xb = [Trk() for _ in range(2)]
            for bi in blocks:
                t0, nb = BLOCKS[bi]
                s = blk_s(bi)
                bs = bi % 2
                fw.dma("sp", xb[bs][:, :, 0:nb], xs_d[:, :, t0:t0 + nb], R=[xs_t[bi]], W=[t_xb[bs]], key=t_xb[bs])
                for f in range(KC):
                    pb, pt = ring.next()
                    for k in range(KC):
                        fw.mm(pb[:, 0:nb], wo[:, k, f * 128:(f + 1) * 128], merged[:, k, t0:t0 + nb], start=(k == 0),
                              stop=(k == KC - 1), R=[t_wo[f // 4], t_mg[bi]], W=[pt])
                    fw.stt("dve", xb[bs][:, f, 0:nb], pb[:, 0:nb], der[:, 2, 2 * f + s:2 * f + s + 1], xb[bs][:, f, 0:nb],
                           ALU.mult, ALU.add, R=[pt, t_der, t_xb[bs]], W=[t_xb[bs]])
                fw.dma("sp", xs_d[:, :, t0:t0 + nb], xb[bs][:, :, 0:nb], R=[t_xb[bs]], W=[xs_t[bi]], key=t_xb[bs])
            fw.barrier()
            ar.release(m)
        def phase_moe(l, blocks, combT, t_comb, yacc, t_y):
            m = ar.mark()
            selE = ar.alloc([NE, 128], F32, parts=NE); t_sel = Trk()
            fw.dma("sp", selE, dr["selE"].rearrange("k (e m) -> k e m", e=NE), W=[t_sel])
            wgu = [ar.alloc([KC, 1024], BF16) for _ in range(2)]
            wdn = [ar.alloc([4, D], BF16) for _ in range(2)]
            t_we = [[wt("we%d_%d" % (s_, i)) for i in range(2)] for s_ in range(2)]
            cb = ar.alloc([512], F32); t_cb = Trk()
            ss = [ar.alloc([512], F32) for _ in range(2)]; t_ss = [Trk() for _ in range(2)]
            tu = [ar.alloc([512], F32) for _ in range(2)]; t_tu = [Trk() for _ in range(2)]
            actT = [ar.alloc([4, 512], BF16) for _ in range(2)]; t_act = [Trk() for _ in range(2)]
            def load_e(e):
                s = e % 2
                load_w(wgu[s], dr["wgu"][l, e].rearrange("(k p) n -> p k n", p=128), t_we[s][0])
                load_w(wdn[s], dr["wd"][l, e].rearrange("(j p) n -> p j n", p=128), t_we[s][1])
            def emit_gu(e, bi, idx):
                s = e % 2
                t0, nb = BLOCKS[bi]
                pb, pt = ring.next()
                fw.mm(pb[:, 0:nb], selE[0:NE, e, :], combT[0:NE, t0:t0 + nb], R=[t_sel, t_comb], W=[pt])
                fw.copy("act", cb[:, 0:nb], pb[:, 0:nb], R=[pt], W=[t_cb])
                a_s = idx % 2
                for ff in range(4):
                    pg_, ptg = ring.next()
                    for k in range(KC):
                        fw.mm(pg_[:, 0:nb], wgu[s][:, k, ff * 128:(ff + 1) * 128], hT[:, k, t0:t0 + nb], start=(k == 0),
                              stop=(k == KC - 1), R=[t_we[s][0], t_h[bi]], W=[ptg])
                    pu_, ptu = ring.next()
                    for k in range(KC):
                        fw.mm(pu_[:, 0:nb], wgu[s][:, k, 512 + ff * 128:512 + (ff + 1) * 128], hT[:, k, t0:t0 + nb], start=(k == 0),
                              stop=(k == KC - 1), R=[t_we[s][0], t_h[bi]], W=[ptu])
                    q = ff % 2
                    fw.act(ss[q][:, 0:nb], pg_[:, 0:nb], AF.Silu, R=[ptg], W=[t_ss[q]])
                    fw.tt("dve", tu[q][:, 0:nb], ss[q][:, 0:nb], pu_[:, 0:nb], ALU.mult, R=[t_ss[q], ptu], W=[t_tu[q]])
                    fw.tt("pool", actT[a_s][:, ff, 0:nb], tu[q][:, 0:nb], cb[:, 0:nb], ALU.mult, R=[t_tu[q], t_cb], W=[t_act[a_s]])

            def emit_down(e, bi, idx):
                s = e % 2
                t0, nb = BLOCKS[bi]
                a_s = idx % 2
                for f in range(KC):
                    pb, pt = ring.next()
                    for j in range(4):
                        fw.mm(pb[:, 0:nb], wdn[s][:, j, f * 128:(f + 1) * 128], actT[a_s][:, j, 0:nb], start=(j == 0),
                              stop=(j == 3), R=[t_we[s][1], t_act[a_s]], W=[pt])
                    if e == 0:
                        fw.copy("act", yacc[:, f, t0:t0 + nb], pb[:, 0:nb], R=[pt], W=[t_y[bi]])
                    else:
                        fw.tt("dve", yacc[:, f, t0:t0 + nb], yacc[:, f, t0:t0 + nb], pb[:, 0:nb], ALU.add,
                              R=[pt, t_y[bi]], W=[t_y[bi]])

            load_e(0)
            items = [(e, bi) for e in range(NE) for bi in blocks]
            prev = None
            for idx, (e, bi) in enumerate(items):
                emit_gu(e, bi, idx)
                if prev is not None:
                    emit_down(*prev)
                if bi == blocks[0] and e + 1 < NE:
                    load_e(e + 1)
                prev = (e, bi, idx)
            emit_down(*prev)
            fw.barrier()
            ar.release(m)
        def phase_ffn_res(l, blocks, yacc, t_y, last):
            m = ar.mark()
            xb = [ar.alloc([KC, 512], F32) for _ in range(2)]; t_xb = [Trk() for _ in range(2)]
            if last:
                sq = ar.alloc([KC, 512], F32); t_sq = Trk()
                rstd = ar.alloc([512], F32); t_rstd = Trk()
                ot = [ar.alloc([D], F32) for _ in range(2)]; t_ot = [Trk() for _ in range(2)]
            oi = 0
            for bi in blocks:
                t0, nb = BLOCKS[bi]
                s = blk_s(bi)
                bs = bi % 2
                fw.dma("sp", xb[bs][:, :, 0:nb], xs_d[:, :, t0:t0 + nb], R=[xs_t[bi]], W=[t_xb[bs]], key=t_xb[bs])
                for f in range(KC):
                    fw.stt("dve", xb[bs][:, f, 0:nb], yacc[:, f, t0:t0 + nb], der[:, 5, 2 * f + s:2 * f + s + 1], xb[bs][:, f, 0:nb],
                           ALU.mult, ALU.add, R=[t_y[bi], t_der, t_xb[bs]], W=[t_xb[bs]])
                if not last:
                    fw.dma("sp", xs_d[:, :, t0:t0 + nb], xb[bs][:, :, 0:nb], R=[t_xb[bs]], W=[xs_t[bi]], key=t_xb[bs])
                    continue
                fw.act(sq[:, :, 0:nb], xb[bs][:, :, 0:nb], AF.Square, R=[t_xb[bs]], W=[t_sq])
                pb, pt = ring.next()
                for k in range(KC):
                    fw.mm(pb[:, 0:nb], ones32, sq[:, k, 0:nb], start=(k == 0), stop=(k == KC - 1), R=[t_sq, t_c], W=[pt])
                rms_rstd(pb, pt, nb, 1.0 / D, rstd, t_rstd)
                for k in range(KC):
                    fw.stt("dve", xb[bs][:, k, 0:nb], xb[bs][:, k, 0:nb], pvc("final_norm", k), rstd[:, 0:nb], ALU.mult, ALU.mult,
                           R=[t_xb[bs], t_pv, t_rstd], W=[t_xb[bs]])
                for ti in range(nb // 128):
                    os_ = oi % 2
                    oi += 1
                    for hf in range(2):
                        pb, pt = ring.next()
                        for kk in range(4):
                            k = hf * 4 + kk
                            fw.tr(pb[:, kk * 128:(kk + 1) * 128], xb[bs][:, k, ti * 128:(ti + 1) * 128], ident,
                                  R=[t_xb[bs], t_c], W=[pt])
                        fw.copy("act" if hf == 0 else "dve", ot[os_][:, hf * 512:(hf + 1) * 512], pb[:, 0:512], R=[pt], W=[t_ot[os_]])
                    r0 = t0 - NCTX + ti * 128
                    fw.dma("sp", out_d[r0:r0 + 128, :], ot[os_], R=[t_ot[os_]], W=[t_out], key=t_ot[os_])
            fw.barrier()
            ar.release(m)
        def ck(name, bufs):
            if stop != name:
                return
            fw.barrier()
            for key, ap in bufs.items():
                if key not in dumps:
                    continue
                d = nc.dram_tensor("dbg_" + key, list(ap.shape), F32, kind="ExternalOutput").ap()
                fw.dma("pool", d, ap, W=[Trk()])
            raise _Stop()
        ALLB = [0, 1, 2, 3, 4]
        LATB = [1, 2, 3, 4]
        try:
            phase_load()
            ck("load", dict(xs=xs_d))
            for l in range(depth):
                last = (l == DEPTH - 1)
                qblocks = LATB if last else ALLB
                phase_mod(l)
                ck("mod%d" % l, dict(mod=modsb, der=der, cneg=cneg))
                phase_norm(l, 1, ALLB)
                ck("norm%d" % l, dict(hT=hT))
                lm = ar.mark()
                o_lru = ar.alloc([4, T], BF16); t_olru = Trk()
                phase_lru(l, o_lru, t_olru)
                ck("lru%d" % l, dict(o_lru=o_lru))
                om = ar.mark()
                o_attn = ar.alloc([4, T], BF16); t_oattn = Trk()
                o_mla = ar.alloc([4, T], BF16); t_omla = Trk()
                kvm = ar.mark()
                kTg = ar.alloc([T], BF16); Vg = ar.alloc([NT, 2, 65], BF16)
                kTm = ar.alloc([8, T], BF16, parts=96); Vm = ar.alloc([NT, 8, 65], BF16)
                kv = (kTg, Vg, kTm, Vm, Trk(), Trk(), Trk(), Trk())
                phase_kv(l, kv)
                ck("kv%d" % l, dict(kTg=kTg, kTm=kTm, Vg=Vg, Vm=Vm))
                phase_attn(l, qblocks, kv, o_attn, t_oattn, o_mla, t_omla)
                ck("attn%d" % l, dict(o_attn=o_attn, o_mla=o_mla))
                ar.release(kvm)
                merged = ar.alloc([KC, T], BF16); t_mg = [Trk() for _ in range(5)]
                phase_merge(l, qblocks, ((o_attn, t_oattn), (o_lru, t_olru), (o_mla, t_omla)), merged, t_mg)
                ck("merge%d" % l, dict(merged=merged))
                phase_wout(l, qblocks, merged, t_mg)
                ck("wout%d" % l, dict(xs=xs_d))
                ar.release(lm)
                combT = ar.alloc([T], F32, parts=NE); t_comb = Trk()
                phase_norm(l, 2, qblocks, combT, t_comb)
                ck("normf%d" % l, dict(hT=hT, combT=combT))
                yacc = ar.alloc([KC, T], F32); t_y = [Trk() for _ in range(5)]
                phase_moe(l, qblocks, combT, t_comb, yacc, t_y)
                ck("moe%d" % l, dict(yacc=yacc))
                phase_ffn_res(l, qblocks, yacc, t_y, last)
                ck("res%d" % l, dict(xs=xs_d))
                ar.release(lm)
        except _Stop:
            pass
        fw.wait_all("sp", [t_out] + xs_t)
        fw.barrier()
        fw.emit()
        build_nc.stats = (fw.ninst, fw.nwait, len(fw.dsems), ar.peak)
    return nc

_CACHE = {}

def kernel(**inputs):
    shared, per_core = _prepare(inputs)
    if "nc" not in _CACHE:
        _CACHE["nc"] = build_nc()
    nc = _CACHE["nc"]
    in_maps = []
    for pc in per_core:
        d = dict(shared)
        d.update(pc)
        in_maps.append(d)
    res = run_bass_kernel_spmd(nc, in_maps, core_ids=list(range(len(in_maps))))
    out = np.stack([np.asarray(r["out"], np.float32) for r in res.results], 0)
    return out
```

```python
import numpy as np
from contextlib import ExitStack
import concourse.bass as bass
import concourse.mybir as mybir
from concourse.bass_utils import run_bass_kernel_spmd
F32 = mybir.dt.float32
BF16 = mybir.dt.bfloat16
AF = mybir.ActivationFunctionType
ALU = mybir.AluOpType
AX = mybir.AxisListType
D = 1024
KC = 8
NCTX = 256
SEQ = 2048
T = NCTX + SEQ
NT = T // 128
DEPTH = 2
BLOCKS = [(0, 256), (256, 512), (768, 512), (1280, 512), (1792, 512)]
IN_WIDTH = 5536
C_Q, C_K, C_V, C_ZX, C_ZY, C_CQ, C_CKV, C_KR, C_G = 0, 512, 640, 768, 1280, 1792, 2176, 2432, 2464
EPS = 1e-6
NE = 16
import os
NLOADS = int(os.environ.get("K_NLOADS", "1"))

class Trk:
    __slots__ = ("name", "w", "r", "sem")
    def __init__(self, name=""):
        self.name = name
        self.w = None
        self.r = {}
        self.sem = None

class FW:
    def __init__(self, nc, stack):
        self.nc = nc
        self.stack = stack
        self.engs = ("pe", "act", "dve", "pool", "sp")
        self.prog = {e: [] for e in self.engs}
        self.sem = {}
        self.cnt = {}
        for e in ("pe", "act", "dve", "pool"):
            self.sem[e] = stack.enter_context(nc.semaphore("s_" + e))
            self.cnt[e] = 0
        self.known = {e: {} for e in self.engs}
        self.dsems = []
        self.ninst = 0
        self.nwait = 0
    def sb(self, name, shape, dt):
        return self.stack.enter_context(self.nc.sbuf_tensor(name, list(shape), dt))
    def ps(self, name, shape, dt=F32):
        return self.stack.enter_context(self.nc.psum_tensor(name, list(shape), dt))
    def _wait(self, e, tok):
        if tok is None:
            return
        key, semh, val = tok
        if key == e and e == "pe":
            return
        kn = self.known[e]
        if kn.get(key, 0) >= val:
            return
        self.prog[e].append(lambda eng, s=semh, v=val: eng.wait_ge(s, v))
        kn[key] = val
        self.nwait += 1
    def _deps(self, e, R, W):
        for t in R:
            self._wait(e, t.w)
        for t in W:
            self._wait(e, t.w)
            for tok in t.r.values():
                self._wait(e, tok)
    @staticmethod
    def _commit(tok, R, W):
        for t in R:
            t.r[tok[0]] = tok
        for t in W:
            t.w = tok
            t.r = {}
    def op(self, e, fn, R=(), W=()):
        self._deps(e, R, W)
        self.cnt[e] += 1
        semh = self.sem[e]
        self.prog[e].append(lambda eng, f=fn, s=semh: f(eng).then_inc(s, 1))
        self._commit((e, semh, self.cnt[e]), R, W)
        self.ninst += 1
    def mm(self, out, lhsT, rhs, start=True, stop=True, R=(), W=()):
        self.op("pe", lambda eng: eng.matmul(out, lhsT, rhs, start=start, stop=stop), R, W)
    def tr(self, out, in_, ident, R=(), W=()):
        self.op("pe", lambda eng: eng.transpose(out, in_, ident), R, W)
    def act(self, out, in_, func, bias=None, scale=None, R=(), W=()):
        kw = {}
        if bias is not None:
            kw["bias"] = bias
        if scale is not None:
            kw["scale"] = scale
        self.op("act", lambda eng: eng.activation(out, in_, func, **kw), R, W)
    def copy(self, e, out, in_, R=(), W=()):
        if e == "act":
            self.op("act", lambda eng: eng.activation(out, in_, AF.Copy), R, W)
        else:
            self.op(e, lambda eng: eng.tensor_copy(out, in_), R, W)
    def tt(self, e, out, in0, in1, op, R=(), W=()):
        self.op(e, lambda eng: eng.tensor_tensor(out, in0, in1, op), R, W)
    def ts(self, e, out, in0, s1, s2, op0, op1=None, R=(), W=()):
        if op1 is None:
            self.op(e, lambda eng: eng.tensor_scalar(out, in0, s1, None, op0), R, W)
        else:
            self.op(e, lambda eng: eng.tensor_scalar(out, in0, s1, s2, op0, op1), R, W)
    def stt(self, e, out, in0, scalar, in1, op0, op1, R=(), W=()):
        self.op(e, lambda eng: eng.scalar_tensor_tensor(out, in0, scalar, in1, op0, op1), R, W)
    def memset(self, e, ap, val, W=()):
        self.op(e, lambda eng: eng.memset(ap, val), (), W)
    def recip(self, out, in_, R=(), W=()):
        self.op("dve", lambda eng: eng.reciprocal(out, in_), R, W)
    def reduce(self, out, in_, op, R=(), W=()):
        self.op("dve", lambda eng: eng.tensor_reduce(out, in_, AX.X, op), R, W)
    def scan(self, out, d0, d1, init, R=(), W=()):
        self.op("dve", lambda eng: eng.tensor_tensor_scan(out, d0, d1, init, ALU.mult, ALU.add), R, W)
    def dma(self, q, out, in_, R=(), W=(), key=None, **kw):
        if key is None:
            key = W[0]
        if key.sem is None:
            n = len(self.dsems)
            key.sem = [self.stack.enter_context(self.nc.semaphore("d%d" % n)), 0, "d%d" % n]
            self.dsems.append(key.sem)
        semh, c, kname = key.sem
        self._deps(q, R, W)
        if c > 0:
            self._wait(q, (kname, semh, c))
        self.prog[q].append(lambda eng, o=out, i=in_, s=semh, k=kw: eng.dma_start(out=o, in_=i, **k).then_inc(s, 16))
        key.sem[1] = c + 16
        self._commit((kname, semh, c + 16), R, W)
    def wait_all(self, e, trks):
        for t in trks:
            self._wait(e, t.w)
            for tok in t.r.values():
                self._wait(e, tok)
    def barrier(self):
        for e in self.engs:
            for c in ("pe", "act", "dve", "pool"):
                if self.cnt[c] > 0 and not (c == e == "pe"):
                    self._wait(e, (c, self.sem[c], self.cnt[c]))
            for semh, cval, kname in self.dsems:
                if cval > 0:
                    self._wait(e, (kname, semh, cval))
    def emit(self):
        prog = self.prog
        with self.nc.Block() as block:
            @block.sync
            def _(eng):
                for f in prog["sp"]:
                    f(eng)
            @block.tensor
            def _(eng):
                for f in prog["pe"]:
                    f(eng)
            @block.scalar
            def _(eng):
                for f in prog["act"]:
                    f(eng)
            @block.vector
            def _(eng):
                for f in prog["dve"]:
                    f(eng)
            @block.gpsimd
            def _(eng):
                for f in prog["pool"]:
                    f(eng)

class Arena:
    def __init__(self, tensor, words):
        self.t = tensor
        self.cap = words
        self.top = 0
    def mark(self):
        return self.top
    def release(self, m):
        self.top = m
    def alloc(self, free_shape, dt, parts=128):
        n = int(np.prod(free_shape))
        words = n if dt == F32 else (n + 1) // 2
        assert self.top + words <= self.cap, ("arena overflow", self.top, words, self.cap)
        ap = self.t[0:parts, self.top:self.top + words]
        self.top += words
        self.peak = max(getattr(self, 'peak', 0), self.top)
        if dt != F32:
            ap = ap.bitcast(dt)[:, 0:n]
        if len(free_shape) == 2:
            ap = ap.rearrange("p (a b) -> p a b", a=free_shape[0], b=free_shape[1])
        elif len(free_shape) == 3:
            ap = ap.rearrange("p (a b c) -> p a b c", a=free_shape[0], b=free_shape[1], c=free_shape[2])
        return ap

class Ring:
    def __init__(self, items):
        self.items = items
        self.i = 0
    def next(self):
        it = self.items[self.i]
        self.i = (self.i + 1) % len(self.items)
        return it

def _pv_layout():
    off = {}
    n = 0
    def add(name, w):
        nonlocal n
        off[name] = n
        n += w
    for l in range(DEPTH):
        add(("norm_mix", l), 8)
        add(("norm_ffn", l), 8)
        add(("gq", l), 1)
        add(("gk", l), 1)
        add(("conv_w", l), 16)
        add(("conv_b", l), 4)
        add(("lru_b_a", l), 8)
        add(("lru_b_i", l), 8)
        add(("lru_lam", l), 8)
        add(("gcq", l), 3)
        add(("gckv", l), 2)
        add(("b_mod", l), 48)
    add("final_norm", 8)
    add("eps", 1)
    return off, n

PV_OFF, NPV = _pv_layout()

def _fm(v, k):
    return np.ascontiguousarray(np.asarray(v, np.float32).reshape(k, 128).T)

def _rope_tables():
    s = np.arange(SEQ)
    row = (s // 64).astype(np.float32)
    col = (s % 64).astype(np.float32)
    def tables(dim):
        quarter = dim // 4
        inv = (np.float32(10000.0) ** (-np.arange(quarter, dtype=np.float32) / np.float32(quarter))).astype(np.float32)
        ar = row[:, None] * inv[None, :]
        ac = col[:, None] * inv[None, :]
        cos = np.ones((dim, T), np.float32)
        sin = np.zeros((dim, T), np.float32)
        half = dim // 2
        for d in range(dim):
            ang = ar if d < half else ac
            j = d % quarter
            first = (d % half) < quarter
            cos[d, NCTX:] = np.cos(ang[:, j])
            sin[d, NCTX:] = (-1.0 if first else 1.0) * np.sin(ang[:, j])
        return cos, sin
    cg, sg = tables(64)
    ropeG = np.stack([np.concatenate([cg, cg], 0), np.concatenate([sg, sg], 0)], 0)
    cm, sm = tables(32)
    cosM = np.ones((96, T), np.float32)
    sinM = np.zeros((96, T), np.float32)
    cosM[64:] = cm
    sinM[64:] = sm
    ropeM = np.stack([cosM, sinM], 0)
    def perm(dim):
        quarter = dim // 4
        half = dim // 2
        P = np.zeros((dim, dim), np.float32)
        for m in range(dim):
            first = (m % half) < quarter
            P[m + quarter if first else m - quarter, m] = 1.0
        return P
    pg = np.zeros((128, 128), np.float32)
    pg[0:64, 0:64] = perm(64)
    pg[64:128, 64:128] = perm(64)
    pm = np.zeros((96, 96), np.float32)
    pm[64:96, 64:96] = perm(32)
    return ropeG.astype(np.float32), ropeM.astype(np.float32), pg, pm

def _prepare(inp):
    f32 = np.float32
    shared = {}
    qperm = []
    for j in range(4):
        qperm += list(range(j * 64, j * 64 + 64)) + list(range((4 + j) * 64, (4 + j) * 64 + 64))
    cols = np.array(qperm + list(range(512, IN_WIDTH)))
    shared["w_in"] = np.ascontiguousarray(np.asarray(inp["w_in"], f32)[:, :, cols])
    shared["w_mod"] = np.ascontiguousarray(np.asarray(inp["w_mod"], f32))
    shared["w_qb"] = np.ascontiguousarray(np.asarray(inp["mla_w_qb"], f32))
    kvb = np.asarray(inp["mla_w_kvb"], f32).reshape(DEPTH, 256, 8, 128)
    shared["w_kvb"] = np.ascontiguousarray(
        np.concatenate([kvb[:, :, :, 0:64].reshape(DEPTH, 256, 512), kvb[:, :, :, 64:128].reshape(DEPTH, 256, 512)], -1))
    shared["w_ba"] = np.ascontiguousarray(np.asarray(inp["w_branch_attn"], f32))
    shared["w_bl"] = np.ascontiguousarray(np.asarray(inp["w_branch_lru"], f32))
    shared["w_bm"] = np.ascontiguousarray(np.asarray(inp["w_branch_mla"], f32))
    shared["w_out"] = np.ascontiguousarray(np.asarray(inp["w_out"], f32))
    shared["router_w"] = np.ascontiguousarray(np.asarray(inp["router_w"], f32))
    shared["rbias"] = np.ascontiguousarray(np.broadcast_to(np.asarray(inp["router_bias"], f32)[None, :], (128, NE)))
    shared["wgu"] = np.ascontiguousarray(np.concatenate([np.asarray(inp["moe_w_gate"], f32), np.asarray(inp["moe_w_up"], f32)], -1))
    shared["wd"] = np.ascontiguousarray(np.asarray(inp["moe_w_down"], f32))
    lw = np.zeros((DEPTH, 2, 2, 4, 128, 128), f32)
    for gi, nm in enumerate(("lru_w_a", "lru_w_i")):
        w = np.asarray(inp[nm], f32)
        for c in range(4):
            lw[:, :, gi, c, 0:64, 0:64] = w[:, :, 2 * c]
            lw[:, :, gi, c, 64:128, 64:128] = w[:, :, 2 * c + 1]
    shared["lruW"] = lw
    ropeG, ropeM, pg, pm = _rope_tables()
    shared["ropeG"] = ropeG
    shared["ropeM"] = ropeM
    shared["permG"] = pg
    shared["permM"] = pm
    shared["ident"] = np.eye(128, dtype=f32)
    bo = np.zeros((128, 128), f32)
    bo[0:64, 0:64] = 1.0
    bo[64:, 64:] = 1.0
    shared["blockones"] = bo
    sel = np.zeros((NE, NE, 128), f32)
    for e in range(NE):
        sel[e, e, :] = 1.0
    shared["selE"] = sel.reshape(NE, NE * 128)
    pv = np.zeros((128, NPV), f32)
    def put(key, arr):
        o = PV_OFF[key]
        pv[:, o:o + arr.shape[1]] = arr
    for l in range(DEPTH):
        put(("norm_mix", l), _fm(inp["norm_mix"][l], 8))
        put(("norm_ffn", l), _fm(inp["norm_ffn"][l], 8))
        put(("gq", l), np.tile(np.asarray(inp["gqa_q_norm"][l], f32), 2)[:, None])
        put(("gk", l), np.tile(np.asarray(inp["gqa_k_norm"][l], f32), 2)[:, None])
        cw = np.asarray(inp["conv_w"][l], f32)
        put(("conv_w", l), np.concatenate([_fm(cw[j], 4) for j in range(4)], 1))
        put(("conv_b", l), _fm(inp["conv_b"][l], 4))
        for nm in ("lru_b_a", "lru_b_i", "lru_lam"):
            a = np.asarray(inp[nm][l], f32)
            put((nm, l), np.concatenate([_fm(a[0], 4), _fm(a[1], 4)], 1))
        put(("gcq", l), _fm(inp["mla_q_a_norm"][l], 3))
        put(("gckv", l), _fm(inp["mla_kv_a_norm"][l], 2))
        put(("b_mod", l), _fm(inp["b_mod"][l], 48))
    put("final_norm", _fm(inp["final_norm"], 8))
    pv[:, PV_OFF["eps"]] = EPS
    shared["pv"] = pv
    per_core = []
    x = np.asarray(inp["x"], f32)
    ctx = np.asarray(inp["ctx"], f32)
    c = np.asarray(inp["c"], f32)
    cc = np.asarray(inp["c_ctx"], f32)
    for b in range(x.shape[0]):
        xin = np.ascontiguousarray(np.concatenate([ctx[b], x[b]], 0))
        cv = np.stack([_fm(c[b], 8), _fm(cc, 8)], -1).reshape(128, 16)
        per_core.append({"xin": xin, "cvec": np.ascontiguousarray(cv)})
    return shared, per_core

SHARED_SHAPES = {
    "w_in": [DEPTH, D, IN_WIDTH], "w_mod": [DEPTH, D, 6 * D], "w_qb": [DEPTH, 384, 768], "w_kvb": [DEPTH, 256, 1024],
    "w_ba": [DEPTH, 512, D], "w_bl": [DEPTH, 512, D], "w_bm": [DEPTH, 512, D], "w_out": [DEPTH, D, D],
    "router_w": [D, NE], "rbias": [128, NE], "wgu": [DEPTH, NE, D, 1024],
    "wd": [DEPTH, NE, 512, D], "lruW": [DEPTH, 2, 2, 4, 128, 128], "ropeG": [2, 128, T], "ropeM": [2, 96, T],
    "permG": [128, 128], "permM": [96, 96], "ident": [128, 128], "blockones": [128, 128], "selE": [NE, NE * 128],
    "pv": [128, NPV], "xin": [T, D], "cvec": [128, 16],
}

class _Stop(Exception):
    pass
def build_nc(depth=DEPTH, stop=None, dumps=()):
    nc = bass.Bass("TRN2", target_bir_lowering=False)
    dr = {k: nc.dram_tensor(k, v, F32, kind="ExternalInput").ap() for k, v in SHARED_SHAPES.items()}
    out_d = nc.dram_tensor("out", [SEQ, D], F32, kind="ExternalOutput").ap()
    xs_d = nc.dram_tensor("xs_scratch", [128, KC, T], F32, kind="Internal").ap()
    with ExitStack() as st:
        fw = FW(nc, st)
        ARW = 53200
        ar = Arena(fw.sb("arena", [128, ARW], F32), ARW)
        psb = [fw.ps("ps%d" % i, [128, 512], F32) for i in range(8)]
        pst = [Trk("ps%d" % i) for i in range(8)]
        ring = Ring([(psb[i], pst[i]) for i in range(6)])
        accr = Ring([(psb[i], pst[i]) for i in (6, 7)])
        pv = ar.alloc([NPV], F32); t_pv = Trk("pv")
        ident = ar.alloc([128], F32); t_c = Trk("consts")
        ones32 = ar.alloc([128], F32)
        bones = ar.alloc([128], F32)
        sel64 = ar.alloc([64], F32)
        permG = ar.alloc([128], BF16)
        permM = ar.alloc([96], BF16, parts=96)
        cvec = ar.alloc([16], F32)
        actc = ar.alloc([16], F32)
        rbias = ar.alloc([NE], F32)
        rw = ar.alloc([KC, NE], F32)
        modsb = ar.alloc([96], F32); t_mod = Trk("mod")
        der = ar.alloc([6, 16], F32); t_der = Trk("der")
        cneg = ar.alloc([8], F32); t_cneg = Trk("cneg")
        cneg2 = ar.alloc([8], F32)
        hT = ar.alloc([KC, T], BF16)
        t_h = [Trk("h%d" % b) for b in range(5)]
        xs_t = [Trk("xs%d" % b) for b in range(5)]
        t_out = Trk("out")
        eps_ap = pv[:, PV_OFF["eps"]:PV_OFF["eps"] + 1]
        fw.dma("sp", pv, dr["pv"], W=[t_pv])
        fw.dma("sp", ident, dr["ident"], W=[t_c])
        fw.dma("sp", bones, dr["blockones"], W=[t_c])
        fw.dma("pool", permG, dr["permG"], W=[t_c])
        fw.dma("pool", permM, dr["permM"], W=[t_c])
        fw.dma("sp", cvec, dr["cvec"], W=[t_c])
        fw.dma("sp", rbias, dr["rbias"], W=[t_c])
        fw.dma("sp", rw, dr["router_w"].rearrange("(k p) e -> p k e", p=128), W=[t_c])
        fw.memset("dve", ones32, 1.0, W=[t_c])
        fw.memset("dve", sel64, 0.0, W=[t_c])
        fw.memset("dve", sel64[64:65, :], 1.0, W=[t_c])
        fw.act(actc, cvec, AF.Silu, R=[t_c], W=[t_c])
        base_mark = ar.mark()
        def pvc(key, j=0, n=1):
            o = PV_OFF[key] + j
            return pv[:, o:o + n]
        def blk_s(b):
            return 1 if b == 0 else 0
        def phase_load():
            m = ar.mark()
            xt = [ar.alloc([D], F32) for _ in range(2)]; t_xt = [Trk() for _ in range(2)]
            xo = [ar.alloc([KC, 128], F32) for _ in range(2)]; t_xo = [Trk() for _ in range(2)]
            for i in range(NT):
                bi = next(b for b, (t0, nb) in enumerate(BLOCKS) if t0 <= i * 128 < t0 + nb)
                s = i % 2
                fw.dma("sp", xt[s], dr["xin"][i * 128:(i + 1) * 128, :], W=[t_xt[s]])
                for hf in range(2):
                    pb, pt = ring.next()
                    for kk in range(4):
                        k = hf * 4 + kk
                        fw.tr(pb[:, kk * 128:(kk + 1) * 128], xt[s][:, k * 128:(k + 1) * 128], ident,
                              R=[t_xt[s], t_c], W=[pt])
                    fw.copy("act" if hf == 0 else "dve", xo[s][:, hf * 4:(hf + 1) * 4, :],
                            pb[:, :].rearrange("p (a b) -> p a b", a=4), R=[pt], W=[t_xo[s]])
                fw.dma("sp", xs_d[:, :, i * 128:(i + 1) * 128], xo[s], R=[t_xo[s]], W=[xs_t[bi]])
            fw.barrier()
            ar.release(m)
        def phase_mod(l):
            m = ar.mark()
            wm = [ar.alloc([KC, 512], F32) for _ in range(2)]; t_wm = [Trk() for _ in range(2)]
            pb, pt = accr.next()
            wsrc = dr["w_mod"][l].rearrange("(k p) n -> p k n", p=128)
            for g in range(12):
                s = g % 2
                fw.dma("sp", wm[s], wsrc[:, :, g * 512:(g + 1) * 512], W=[t_wm[s]])
                for jj in range(4):
                    j = g * 4 + jj
                    for k in range(KC):
                        fw.mm(pb[:, 2 * j:2 * j + 2], wm[s][:, k, jj * 128:(jj + 1) * 128], actc[:, 2 * k:2 * k + 2],
                              start=(k == 0), stop=(k == KC - 1), R=[t_wm[s], t_c], W=[pt])
            bm = pvc(("b_mod", l), 0, 48)
            for s in range(2):
                fw.tt("dve", modsb[:, s:96:2], pb[:, s:96:2], bm, ALU.add, R=[pt, t_pv], W=[t_mod])
            def modv(mi):
                return modsb[:, mi * 16:(mi + 1) * 16]
            for (di, mi_scale, gkey) in ((0, 1, ("norm_mix", l)), (3, 4, ("norm_ffn", l))):
                for s in range(2):
                    fw.stt("dve", der[:, di, s:16:2], modv(mi_scale)[:, s:16:2], 1.0, pvc(gkey, 0, 8), ALU.add, ALU.mult,
                           R=[t_mod, t_pv], W=[t_der])
            for (di, mi) in ((1, 0), (2, 2), (4, 3), (5, 5)):
                fw.copy("dve", der[:, di, :], modv(mi), R=[t_mod], W=[t_der])
            tmp = ar.alloc([6, 8], F32); t_tmp = Trk()
            lam = pvc(("lru_lam", l), 0, 8)
            e_, l_, p_, mk, a_, b_ = (tmp[:, i, :] for i in range(6))
            fw.act(e_, lam, AF.Exp, scale=-1.0, R=[t_pv], W=[t_tmp])
            fw.act(l_, e_, AF.Ln, bias=1.0, R=[t_tmp], W=[t_tmp])
            fw.ts("dve", p_, e_, -0.2, 0.25, ALU.mult, ALU.add, R=[t_tmp], W=[t_tmp])
            for cst in (1.0 / 3.0, 0.5, 1.0):
                fw.tt("dve", p_, p_, e_, ALU.mult, R=[t_tmp], W=[t_tmp])
                fw.ts("dve", p_, p_, -1.0, cst, ALU.mult, ALU.add, R=[t_tmp], W=[t_tmp])
            fw.tt("dve", p_, p_, e_, ALU.mult, R=[t_tmp], W=[t_tmp])
            fw.ts("dve", mk, e_, 0.1, None, ALU.is_lt, R=[t_tmp], W=[t_tmp])
            fw.tt("dve", a_, p_, l_, ALU.subtract, R=[t_tmp], W=[t_tmp])
            fw.tt("dve", a_, a_, mk, ALU.mult, R=[t_tmp], W=[t_tmp])
            fw.tt("dve", a_, a_, l_, ALU.add, R=[t_tmp], W=[t_tmp])
            fw.ts("dve", cneg, a_, -8.0, None, ALU.mult, R=[t_tmp], W=[t_cneg])
            fw.ts("dve", cneg2, a_, -16.0, None, ALU.mult, R=[t_tmp, t_cneg], W=[t_cneg])
            fw.barrier()
            ar.release(m)
        def rms_rstd(ssq_ps, pt, n, inv_n, rstd, t_rstd, parts=128):
            fw.act(rstd[0:parts, 0:n], ssq_ps[0:parts, 0:n], AF.Ln, bias=eps_ap[0:parts, :], scale=inv_n,
                   R=[pt, t_pv], W=[t_rstd])
            fw.act(rstd[0:parts, 0:n], rstd[0:parts, 0:n], AF.Exp, scale=-0.5, R=[t_rstd], W=[t_rstd])
        def phase_norm(l, which, blocks, combT=None, t_comb=None, hooks=None):
            m = ar.mark()
            di = 0 if which == 1 else 3
            xb = [ar.alloc([KC, 512], F32) for _ in range(2)]; t_xb = [Trk() for _ in range(2)]
            sq = ar.alloc([KC, 512], F32); t_sq = Trk()
            rstd = ar.alloc([512], F32); t_rstd = Trk()
            tmp = [ar.alloc([512], F32) for _ in range(2)]; t_tmp = [Trk() for _ in range(2)]
            if which == 2:
                h32 = ar.alloc([KC, 512], F32); t_h32 = Trk()
                rts = [ar.alloc([8, NE], F32) for _ in range(4)]; t_rts = [Trk() for _ in range(4)]
            for bidx, bi in enumerate(blocks):
                if hooks and bidx in hooks:
                    hooks[bidx]()
                t0, nb = BLOCKS[bi]
                s = blk_s(bi)
                bs = bi % 2
                fw.dma("sp", xb[bs][:, :, 0:nb], xs_d[:, :, t0:t0 + nb], R=[xs_t[bi]], W=[t_xb[bs]], key=t_xb[bs])
                fw.act(sq[:, :, 0:nb], xb[bs][:, :, 0:nb], AF.Square, R=[t_xb[bs]], W=[t_sq])
                pb, pt = ring.next()
                for k in range(KC):
                    fw.mm(pb[:, 0:nb], ones32, sq[:, k, 0:nb], start=(k == 0), stop=(k == KC - 1), R=[t_sq, t_c], W=[pt])
                rms_rstd(pb, pt, nb, 1.0 / D, rstd, t_rstd)
                for k in range(KC):
                    ts_ = k % 2
                    fw.stt("dve", tmp[ts_][:, 0:nb], xb[bs][:, k, 0:nb], der[:, di, 2 * k + s:2 * k + s + 1], rstd[:, 0:nb],
                           ALU.mult, ALU.mult, R=[t_xb[bs], t_der, t_rstd], W=[t_tmp[ts_]])
                    sh = der[:, di + 1, 2 * k + s:2 * k + s + 1]
                    if which == 1:
                        fw.act(hT[:, k, t0:t0 + nb], tmp[ts_][:, 0:nb], AF.Identity, bias=sh, R=[t_tmp[ts_], t_der], W=[t_h[bi]])
                    else:
                        fw.act(h32[:, k, 0:nb], tmp[ts_][:, 0:nb], AF.Identity, bias=sh, R=[t_tmp[ts_], t_der], W=[t_h32])
                        fw.copy("pool", hT[:, k, t0:t0 + nb], h32[:, k, 0:nb], R=[t_h32], W=[t_h[bi]])
                if which == 2:
                    ntl = nb // 128
                    chains = []
                    for ti in range(ntl):
                        pb, pt = ring.next()
                        for k in range(KC):
                            fw.mm(pb[:, 0:NE], h32[:, k, ti * 128:(ti + 1) * 128], rw[:, k, :], start=(k == 0),
                                  stop=(k == KC - 1), R=[t_h32, t_c], W=[pt])
                        rt_ = rts[ti]; trt = t_rts[ti]
                        sc, sel, tm, sel2, m1, m2, wsel, comb = (rt_[:, i, :] for i in range(8))
                        R_ = [trt]
                        g4 = lambda a: a.rearrange("p (g j) -> p g j", g=4)
                        ops = []
                        A = ops.append
                        A(lambda sc=sc, pb=pb, pt=pt, trt=trt: fw.act(sc, pb[:, 0:NE], AF.Sigmoid, R=[pt], W=[trt]))
                        A(lambda sel=sel, sc=sc, trt=trt: fw.tt("dve", sel, sc, rbias, ALU.add, R=[trt, t_c], W=[trt]))
                        A(lambda m1=m1, sel=sel, trt=trt, g4=g4: fw.reduce(m1[:, 0:4], g4(sel), ALU.max, R=[trt], W=[trt]))
                        for g in range(4):
                            A(lambda g=g, tm=tm, sel=sel, m1=m1, trt=trt: fw.ts("dve", tm[:, 4 * g:4 * g + 4], sel[:, 4 * g:4 * g + 4],
                                                                             m1[:, g:g + 1], -1e9, ALU.is_equal, ALU.mult, R=[trt], W=[trt]))
                        A(lambda sel2=sel2, sel=sel, tm=tm, trt=trt: fw.tt("dve", sel2, sel, tm, ALU.add, R=[trt], W=[trt]))
                        A(lambda m2=m2, sel2=sel2, trt=trt, g4=g4: fw.reduce(m2[:, 0:4], g4(sel2), ALU.max, R=[trt], W=[trt]))
                        A(lambda m1=m1, m2=m2, trt=trt: fw.tt("dve", m1[:, 4:8], m1[:, 0:4], m2[:, 0:4], ALU.add, R=[trt], W=[trt]))
                        A(lambda m1=m1, trt=trt: fw.reduce(m1[:, 8:9], m1[:, 4:8], ALU.max, R=[trt], W=[trt]))
                        A(lambda m1=m1, trt=trt: fw.ts("dve", m1[:, 12:16], m1[:, 4:8], m1[:, 8:9], None, ALU.is_equal, R=[trt], W=[trt]))
                        for g in range(4):
                            A(lambda g=g, tm=tm, sel=sel, m1=m1, m2=m2, trt=trt: fw.ts("dve", tm[:, 4 * g:4 * g + 4], sel[:, 4 * g:4 * g + 4],
                                                                                    m2[:, g:g + 1], m1[:, 12 + g:13 + g], ALU.is_ge, ALU.mult,
                                                                                    R=[trt], W=[trt]))
                        A(lambda wsel=wsel, tm=tm, sc=sc, trt=trt: fw.tt("dve", wsel, tm, sc, ALU.mult, R=[trt], W=[trt]))
                        A(lambda m2=m2, wsel=wsel, trt=trt: fw.reduce(m2[:, 8:9], wsel, ALU.add, R=[trt], W=[trt]))
                        A(lambda m2=m2, trt=trt: fw.recip(m2[:, 9:10], m2[:, 8:9], R=[trt], W=[trt]))
                        A(lambda comb=comb, wsel=wsel, m2=m2, trt=trt: fw.ts("dve", comb, wsel, m2[:, 9:10], None, ALU.mult, R=[trt], W=[trt]))
                        chains.append((ops, comb, trt, ti))
                    for k in range(max(len(c[0]) for c in chains)):
                        for ops, _, _, _ in chains:
                            if k < len(ops):
                                ops[k]()
                    for ops, comb, trt, ti in chains:
                        pb2, pt2 = ring.next()
                        fw.tr(pb2[0:NE, 0:128], comb, ident, R=[trt, t_c], W=[pt2])
                        c0 = t0 + ti * 128
                        fw.copy("act", combT[0:NE, c0:c0 + 128], pb2[0:NE, 0:128], R=[pt2], W=[t_comb])
            fw.barrier()
            ar.release(m)
        WTR = {}
        def wt(name):
            if name not in WTR:
                WTR[name] = Trk(name)
            return WTR[name]
        pend_loads = []
        def load_w(dst, src, trk, R=()):
            if len(pend_loads) >= NLOADS:
                fw._wait("pool", pend_loads.pop(0))
            fw.dma("pool", dst, src, R=list(R), W=[trk])
            pend_loads.append(trk.w)
        def win(l, c0, c1):
            return dr["w_in"][l].rearrange("(k p) n -> p k n", p=128)[:, :, c0:c1]
        def lru_weights(l):
            wz = ar.alloc([KC, 1024], BF16); t_wz = wt("wz0"); t_wz1 = wt("wz1")
            load_w(wz[:, :, 0:512], win(l, C_ZX, C_ZX + 512), t_wz)
            load_w(wz[:, :, 512:1024], win(l, C_ZY, C_ZY + 512), t_wz1)
            lw = ar.alloc([16, 128], BF16); t_lw = wt("lw")
            load_w(lw, dr["lruW"][l].rearrange("d g c p m -> p (d g c) m"), t_lw)
            return wz, lw

        def phase_lru(l, o_lru, t_olru, lruw):
            m = ar.mark()
            wz, lw = lruw
            t_wz = wt("wz0"); t_wz1 = wt("wz1"); t_lw = wt("lw")
            ZP = T + 6
            zx = ar.alloc([ZP], F32); t_zx = Trk()
            u = ar.alloc([T], F32); t_u = Trk()
            ub = ar.alloc([T], BF16); t_ub = Trk()
            gy = ar.alloc([T], F32); t_gy = Trk()
            a_ = ar.alloc([T], F32); t_a = Trk()
            b_ = ar.alloc([T], F32); t_b = Trk()
            hf = ar.alloc([T], F32); t_hf = Trk()
            hb = ar.alloc([T], F32); t_hb = Trk()
            fw.memset("pool", zx, 0.0, W=[t_zx])
            for c in range(4):
                for bi, (t0, nb) in enumerate(BLOCKS):
                    pb, pt = ring.next()
                    for k in range(KC):
                        fw.mm(pb[:, 0:nb], wz[:, k, c * 128:(c + 1) * 128], hT[:, k, t0:t0 + nb], start=(k == 0),
                              stop=(k == KC - 1), R=[t_wz, t_h[bi]], W=[pt])
                    zo = 1 if bi == 0 else t0 + 4
                    fw.copy("act", zx[:, zo:zo + nb], pb[:, 0:nb], R=[pt], W=[t_zx])
                for bi, (t0, nb) in enumerate(BLOCKS):
                    pb, pt = ring.next()
                    for k in range(KC):
                        fw.mm(pb[:, 0:nb], wz[:, k, 512 + c * 128:512 + (c + 1) * 128], hT[:, k, t0:t0 + nb], start=(k == 0),
                              stop=(k == KC - 1), R=[t_wz1, t_h[bi]], W=[pt])
                    fw.act(gy[:, t0:t0 + nb], pb[:, 0:nb], AF.Gelu_apprx_tanh, R=[pt], W=[t_gy])
                for (d0, n, base) in ((0, NCTX, 1), (NCTX, SEQ, 260)):
                    for j in range(4):
                        wj = pvc(("conv_w", l), j * 4 + c)
                        src = zx[:, base - 1 + j:base - 1 + j + n]
                        if j == 0:
                            fw.ts("dve", u[:, d0:d0 + n], src, wj, pvc(("conv_b", l), c), ALU.mult, ALU.add,
                                  R=[t_zx, t_pv], W=[t_u])
                        else:
                            fw.stt("dve", u[:, d0:d0 + n], src, wj, u[:, d0:d0 + n], ALU.mult, ALU.add,
                                   R=[t_zx, t_pv, t_u], W=[t_u])
                fw.copy("act", ub, u, R=[t_u], W=[t_ub])
                for dr_ in range(2):
                    gi = dr_ * 4 + c
                    for bi, (t0, nb) in enumerate(BLOCKS):
                        pb, pt = ring.next()
                        fw.mm(pb[:, 0:nb], lw[:, (dr_ * 2 + 0) * 4 + c, :], ub[:, t0:t0 + nb], R=[t_lw, t_ub], W=[pt])
                        fw.act(a_[:, t0:t0 + nb], pb[:, 0:nb], AF.Sigmoid, bias=pvc(("lru_b_a", l), gi), R=[pt, t_pv], W=[t_a])
                    for bi, (t0, nb) in enumerate(BLOCKS):
                        pb, pt = ring.next()
                        fw.mm(pb[:, 0:nb], lw[:, (dr_ * 2 + 1) * 4 + c, :], ub[:, t0:t0 + nb], R=[t_lw, t_ub], W=[pt])
                        fw.act(b_[:, t0:t0 + nb], pb[:, 0:nb], AF.Sigmoid, bias=pvc(("lru_b_i", l), gi), R=[pt, t_pv], W=[t_b])
                    for bi, (t0, nb) in enumerate(BLOCKS):
                        fw.act(hb[:, t0:t0 + nb], a_[:, t0:t0 + nb], AF.Exp, scale=cneg2[:, gi:gi + 1], R=[t_a, t_cneg], W=[t_hb])
                    for bi, (t0, nb) in enumerate(BLOCKS):
                        fw.act(a_[:, t0:t0 + nb], a_[:, t0:t0 + nb], AF.Exp, scale=cneg[:, gi:gi + 1], R=[t_a, t_cneg], W=[t_a])
                    fw.tt("dve", b_, b_, u, ALU.mult, R=[t_b, t_u], W=[t_b])
                    for bi, (t0, nb) in enumerate(BLOCKS):
                        fw.act(hb[:, t0:t0 + nb], hb[:, t0:t0 + nb], AF.Sqrt, bias=1.0, scale=-1.0, R=[t_hb], W=[t_hb])
                    fw.tt("dve", b_, b_, hb, ALU.mult, R=[t_b, t_hb], W=[t_b])
                    if dr_ == 0:
                        fw.scan(hf, a_, b_, 0.0, R=[t_a, t_b], W=[t_hf])
                    else:
                        fw.scan(hb[:, 0:NCTX][:, ::-1], a_[:, 0:NCTX][:, ::-1], b_[:, 0:NCTX][:, ::-1], 0.0,
                                R=[t_a, t_b], W=[t_hb])
                        fw.scan(hb[:, NCTX:T][:, ::-1], a_[:, NCTX:T][:, ::-1], b_[:, NCTX:T][:, ::-1], hb[:, 0:1],
                                R=[t_a, t_b, t_hb], W=[t_hb])
                fw.tt("dve", hf, hf, hb, ALU.add, R=[t_hf, t_hb], W=[t_hf])
                fw.tt("dve", o_lru[:, c, :], hf, gy, ALU.mult, R=[t_hf, t_gy], W=[t_olru])
            fw.barrier()
            ar.release(m)
        def norm_rope(pb, pt, nb, t0, gain, dst, dst_t, wk):
            (sq, t_sq), (rstd, t_rstd), (kn, t_kn), (cs, t_cs), (t1, t_t1), (t2, t_t2) = wk
            fw.act(sq[:, 0:nb], pb[:, 0:nb], AF.Square, R=[pt], W=[t_sq])
            pb2, pt2 = ring.next()
            fw.mm(pb2[:, 0:nb], bones, sq[:, 0:nb], R=[t_sq, t_c], W=[pt2])
            rms_rstd(pb2, pt2, nb, 1.0 / 64.0, rstd, t_rstd)
            fw.stt("dve", kn[:, 0:nb], pb[:, 0:nb], gain, rstd[:, 0:nb], ALU.mult, ALU.mult, R=[pt, t_pv, t_rstd], W=[t_kn])
            pb3, pt3 = ring.next()
            fw.mm(pb3[:, 0:nb], permG, kn[:, 0:nb], R=[t_kn, t_c], W=[pt3])
            fw.dma("sp", cs[:, :, 0:nb], dr["ropeG"][:, :, t0:t0 + nb].rearrange("c p t -> p c t"), W=[t_cs])
            fw.tt("dve", t1[:, 0:nb], kn[:, 0:nb], cs[:, 0, 0:nb], ALU.mult, R=[t_kn, t_cs], W=[t_t1])
            fw.tt("dve", t2[:, 0:nb], pb3[:, 0:nb], cs[:, 1, 0:nb], ALU.mult, R=[pt3, t_cs], W=[t_t2])
            if isinstance(dst, list):
                for (dap, p0, p1) in dst:
                    fw.tt("pool", dap, t1[p0:p1, 0:nb], t2[p0:p1, 0:nb], ALU.add, R=[t_t1, t_t2], W=[dst_t])
            else:
                fw.tt("pool", dst, t1[:, 0:nb], t2[:, 0:nb], ALU.add, R=[t_t1, t_t2], W=[dst_t])
        def rope_m(src, t_src, nb, t0, dst, dst_t, wk, dst_parts=(0, 96)):
            (cs, t_cs), (t1, t_t1), (t2, t_t2) = wk
            pb3, pt3 = ring.next()
            fw.mm(pb3[0:96, 0:nb], permM, src[0:96, 0:nb], R=[t_src, t_c], W=[pt3])
            fw.dma("sp", cs[0:96, :, 0:nb], dr["ropeM"][:, :, t0:t0 + nb].rearrange("c p t -> p c t"), W=[t_cs])
            fw.tt("dve", t1[0:96, 0:nb], src[0:96, 0:nb], cs[0:96, 0, 0:nb], ALU.mult, R=[t_src, t_cs], W=[t_t1])
            fw.tt("dve", t2[0:96, 0:nb], pb3[0:96, 0:nb], cs[0:96, 1, 0:nb], ALU.mult, R=[pt3, t_cs], W=[t_t2])
            p0, p1 = dst_parts
            fw.tt("pool", dst, t1[p0:p1, 0:nb], t2[p0:p1, 0:nb], ALU.add, R=[t_t1, t_t2], W=[dst_t])
        def mk_work():
            sq = ar.alloc([3, 512], F32); rstd = ar.alloc([512], F32); kn = ar.alloc([512], BF16)
            cs = ar.alloc([2, 512], F32)
            t_cs = Trk(); t_sq = Trk()
            return dict(sq=(sq, t_sq), rstd=(rstd, Trk()), kn=(kn, Trk()), cs=(cs, t_cs), t1=(sq[:, 1, :], t_sq), t2=(sq[:, 2, :], t_sq),
                        csm=(cs, t_cs))
        def phase_kv(l, kv):
            m = ar.mark()
            kTg, Vg, kTm, Vm, t_kTg, t_Vg, t_kTm, t_Vm = kv
            wk_ = ar.alloc([KC, 128], BF16); wv_ = ar.alloc([KC, 128], BF16); wc_ = ar.alloc([KC, 256], BF16)
            wr_ = ar.alloc([KC, 96], BF16); wkvb = ar.alloc([2, 1024], BF16)
            t_wk, t_wv, t_wc, t_wr, t_wkvb = wt("wk"), wt("wv"), wt("wc"), wt("wr"), wt("wkvb")
            fw.memset("pool", wr_, 0.0, W=[t_wr])
            load_w(wk_, win(l, C_K, C_K + 128), t_wk)
            load_w(wv_, win(l, C_V, C_V + 128), t_wv)
            load_w(wc_, win(l, C_CKV, C_CKV + 256), t_wc)
            load_w(wr_[:, :, 64:96], win(l, C_KR, C_KR + 32), t_wr)
            load_w(wkvb, dr["w_kvb"][l].rearrange("(j p) n -> p j n", p=128), t_wkvb)
            fw.memset("pool", Vg[:, :, :, 64:65], 1.0, W=[t_Vg])
            fw.memset("pool", Vm[:, :, :, 64:65], 1.0, W=[t_Vm])
            W_ = mk_work()
            ckvT = ar.alloc([2, 512], BF16); t_ckv = Trk()
            krs = ar.alloc([512], BF16); t_krs = Trk()
            krr = ar.alloc([512], BF16); t_krr = Trk()
            sq, t_sq = W_["sq"]; rstd, t_rstd = W_["rstd"]
            for bi, (t0, nb) in enumerate(BLOCKS):
                pb, pt = ring.next()
                for k in range(KC):
                    fw.mm(pb[:, 0:nb], wk_[:, k, :], hT[:, k, t0:t0 + nb], start=(k == 0), stop=(k == KC - 1),
                          R=[t_wk, t_h[bi]], W=[pt])
                norm_rope(pb, pt, nb, t0, pvc(("gk", l)), kTg[:, t0:t0 + nb], t_kTg,
                          ((sq[:, 0, :], t_sq), W_["rstd"], W_["kn"], W_["cs"], W_["t1"], W_["t2"]))
                for ti in range(nb // 128):
                    tt_ = t0 // 128 + ti
                    pb, pt = ring.next()
                    for k in range(KC):
                        fw.mm(pb[:, 0:128], hT[:, k, tt_ * 128:(tt_ + 1) * 128], wv_[:, k, :], start=(k == 0), stop=(k == KC - 1),
                              R=[t_wv, t_h[bi]], W=[pt])
                    fw.copy("act", Vg[:, tt_, :, 0:64], pb[:, 0:128].rearrange("p (h d) -> p h d", h=2), R=[pt], W=[t_Vg])
                pbs = [ring.next() for _ in range(2)]
                for j in range(2):
                    for k in range(KC):
                        fw.mm(pbs[j][0][:, 0:nb], wc_[:, k, j * 128:(j + 1) * 128], hT[:, k, t0:t0 + nb], start=(k == 0),
                              stop=(k == KC - 1), R=[t_wc, t_h[bi]], W=[pbs[j][1]])
                    fw.act(sq[:, j, 0:nb], pbs[j][0][:, 0:nb], AF.Square, R=[pbs[j][1]], W=[t_sq])
                pb2, pt2 = ring.next()
                for j in range(2):
                    fw.mm(pb2[:, 0:nb], ones32, sq[:, j, 0:nb], start=(j == 0), stop=(j == 1), R=[t_sq, t_c], W=[pt2])
                rms_rstd(pb2, pt2, nb, 1.0 / 256.0, rstd, t_rstd)
                for j in range(2):
                    fw.stt("dve", ckvT[:, j, 0:nb], pbs[j][0][:, 0:nb], pvc(("gckv", l), j), rstd[:, 0:nb], ALU.mult, ALU.mult,
                           R=[pbs[j][1], t_pv, t_rstd], W=[t_ckv])
                for h in range(8):
                    pb, pt = ring.next()
                    for j in range(2):
                        fw.mm(pb[0:64, 0:nb], wkvb[:, j, h * 64:(h + 1) * 64], ckvT[:, j, 0:nb], start=(j == 0), stop=(j == 1),
                              R=[t_wkvb, t_ckv], W=[pt])
                    fw.copy("act" if h % 2 else "dve", kTm[0:64, h, t0:t0 + nb], pb[0:64, 0:nb], R=[pt], W=[t_kTm])
                for ti in range(nb // 128):
                    tt_ = t0 // 128 + ti
                    pb, pt = ring.next()
                    for j in range(2):
                        fw.mm(pb[:, 0:512], ckvT[:, j, ti * 128:(ti + 1) * 128], wkvb[:, j, 512:1024], start=(j == 0), stop=(j == 1),
                              R=[t_wkvb, t_ckv], W=[pt])
                    fw.copy("act" if ti % 2 else "dve", Vm[:, tt_, :, 0:64], pb[:, 0:512].rearrange("p (h d) -> p h d", h=8),
                            R=[pt], W=[t_Vm])
                pb, pt = ring.next()
                for k in range(KC):
                    fw.mm(pb[0:96, 0:nb], wr_[:, k, :], hT[:, k, t0:t0 + nb], start=(k == 0), stop=(k == KC - 1),
                          R=[t_wr, t_h[bi]], W=[pt])
                fw.copy("act", krs[0:96, 0:nb], pb[0:96, 0:nb], R=[pt], W=[t_krs])
                rope_m(krs, t_krs, nb, t0, krr[64:96, 0:nb], t_krr, (W_["csm"], W_["t1"], W_["t2"]), dst_parts=(64, 96))
                for h in range(8):
                    fw.copy("pool" if h % 2 else "dve", kTm[64:96, h, t0:t0 + nb], krr[64:96, 0:nb], R=[t_krr], W=[t_kTm])
            fw.barrier()
            ar.release(m)
        def phase_attn(l, blocks, kv, o_attn, t_oattn, o_mla, t_omla):
            m = ar.mark()
            kTg, Vg, kTm, Vm, t_kTg, t_Vg, t_kTm, t_Vm = kv
            wq = ar.alloc([KC, 512], BF16); wcq = ar.alloc([KC, 384], BF16); wqb = ar.alloc([3, 768], BF16)
            t_wq, t_wcq, t_wqb = wt("wq"), wt("wcq"), wt("wqb")
            load_w(wq, win(l, C_Q, C_Q + 512), t_wq)
            load_w(wcq, win(l, C_CQ, C_CQ + 384), t_wcq)
            load_w(wqb, dr["w_qb"][l].rearrange("(j p) n -> p j n", p=128), t_wqb)
            W_ = mk_work()
            sq, t_sq = W_["sq"]; rstd, t_rstd = W_["rstd"]
            qTg = ar.alloc([8, 512], BF16); t_qTg = [Trk() for _ in range(4)]
            for j in range(4):
                fw.memset("pool", qTg[:, 2 * j:2 * j + 2, :], 0.0, W=[t_qTg[j]])
            cqT = ar.alloc([3, 512], BF16); t_cq = Trk()
            qraw = ar.alloc([512], BF16); t_qraw = Trk()
            qm = [ar.alloc([512], BF16) for _ in range(2)]; t_qm = [Trk() for _ in range(2)]
            pT = [ar.alloc([512], BF16) for _ in range(2)]; t_pT = [Trk() for _ in range(2)]
            osb = ar.alloc([512], F32); t_osb = Trk()
            fw.memset("pool", osb, 0.0, W=[t_osb])
            LA = 2
            for bi in blocks:
                t0, nb = BLOCKS[bi]
                ktiles = list(range(2)) if bi == 0 else list(range(NT))
                nk = len(ktiles)
                for j in range(4):
                    pb, pt = ring.next()
                    for k in range(KC):
                        fw.mm(pb[:, 0:nb], wq[:, k, j * 128:(j + 1) * 128], hT[:, k, t0:t0 + nb], start=(k == 0), stop=(k == KC - 1),
                              R=[t_wq, t_h[bi]], W=[pt])
                    norm_rope(pb, pt, nb, t0, pvc(("gq", l)),
                              [(qTg[0:64, 2 * j, 0:nb], 0, 64), (qTg[64:128, 2 * j + 1, 0:nb], 64, 128)], t_qTg[j],
                              ((sq[:, 0, :], t_sq), W_["rstd"], W_["kn"], W_["cs"], W_["t1"], W_["t2"]))
                pbs = [ring.next() for _ in range(3)]
                for j in range(3):
                    for k in range(KC):
                        fw.mm(pbs[j][0][:, 0:nb], wcq[:, k, j * 128:(j + 1) * 128], hT[:, k, t0:t0 + nb], start=(k == 0),
                              stop=(k == KC - 1), R=[t_wcq, t_h[bi]], W=[pbs[j][1]])
                    fw.act(sq[:, j, 0:nb], pbs[j][0][:, 0:nb], AF.Square, R=[pbs[j][1]], W=[t_sq])
                pb2, pt2 = ring.next()
                for j in range(3):
                    fw.mm(pb2[:, 0:nb], ones32, sq[:, j, 0:nb], start=(j == 0), stop=(j == 2), R=[t_sq, t_c], W=[pt2])
                rms_rstd(pb2, pt2, nb, 1.0 / 384.0, rstd, t_rstd)
                for j in range(3):
                    fw.stt("dve", cqT[:, j, 0:nb], pbs[j][0][:, 0:nb], pvc(("gcq", l), j), rstd[:, 0:nb], ALU.mult, ALU.mult,
                           R=[pbs[j][1], t_pv, t_rstd], W=[t_cq])
                jobs = []
                for j in range(4):
                    for half in range(2):
                        hd = j + 4 * half
                        p0 = 64 * half
                        jobs.append(dict(
                            mla=None,
                            k_of=(lambda kt: kTg[:, kt * 128:(kt + 1) * 128]),
                            q_ap=qTg[:, 2 * j + half, 0:nb],
                            v_of=(lambda kt, half=half: Vg[:, kt, half, 0:65]),
                            scale=0.125,
                            dst=o_attn[64 * (hd % 2):64 * (hd % 2) + 64, hd // 2, t0:t0 + nb], dst_t=t_oattn,
                            Rk=[t_kTg], Rq=[t_qTg[j]], Rv=[t_Vg]))
                for h in range(8):
                    s = h % 2
                    jobs.append(dict(
                        mla=h,
                        k_of=(lambda kt, h=h: kTm[0:96, h, kt * 128:(kt + 1) * 128]),
                        q_ap=qm[s][0:96, 0:nb],
                        v_of=(lambda kt, h=h: Vm[:, kt, h, 0:65]),
                        scale=96.0 ** -0.5,
                        dst=o_mla[64 * (h % 2):64 * (h % 2) + 64, h // 2, t0:t0 + nb], dst_t=t_omla,
                        Rk=[t_kTm], Rq=[t_qm[s]], Rv=[t_Vm]))

                def prep_a(job):
                    h = job["mla"]
                    if h is None:
                        return
                    pb, pt = ring.next()
                    for j in range(3):
                        fw.mm(pb[0:96, 0:nb], wqb[:, j, h * 96:(h + 1) * 96], cqT[:, j, 0:nb], start=(j == 0), stop=(j == 2),
                              R=[t_wqb, t_cq], W=[pt])
                    fw.copy("dve", qraw[0:96, 0:nb], pb[0:96, 0:nb], R=[pt], W=[t_qraw])

                def prep_b(job):
                    h = job["mla"]
                    if h is None:
                        return
                    s = h % 2
                    rope_m(qraw, t_qraw, nb, t0, qm[s][0:96, 0:nb], t_qm[s], (W_["csm"], W_["t1"], W_["t2"]))

                def emit_S(job, kt):
                    pb, pt = ring.next()
                    fw.mm(pb[:, 0:nb], job["k_of"](kt), job["q_ap"], R=job["Rk"] + job["Rq"], W=[pt])
                    return pb, pt

                def finish_a(job, acc, acct):
                    fw.copy("dve", osb[0:65, 0:nb], acc[0:65, 0:nb], R=[acct], W=[t_osb])
                    fw.recip(osb[64:65, 0:nb], osb[64:65, 0:nb], R=[t_osb], W=[t_osb])

                def finish_b(job):
                    pb, pt = ring.next()
                    fw.mm(pb[0:64, 0:nb], sel64, osb[:, 0:nb], R=[t_osb, t_c], W=[pt])
                    fw.tt("dve", job["dst"], osb[0:64, 0:nb], pb[0:64, 0:nb], ALU.mult, R=[t_osb, pt], W=[job["dst_t"]])

                flat = [(ji, ki) for ji in range(len(jobs)) for ki in range(nk)]
                done_a = set(); done_b = set()

                def ensure_prep(jx, upto_b=True):
                    if jx not in done_a:
                        prep_a(jobs[jx]); done_a.add(jx)
                    if upto_b and jx not in done_b:
                        prep_b(jobs[jx]); done_b.add(jx)

                inflight = []
                for i0 in range(min(LA, len(flat))):
                    ji, ki = flat[i0]
                    ensure_prep(ji)
                    inflight.append(emit_S(jobs[ji], ktiles[ki]))
                pend_fin = None
                acc = acct = None
                kb = min(8, nk - 1)
                for i, (ji, ki) in enumerate(flat):
                    job = jobs[ji]
                    if ki == 0:
                        acc, acct = accr.next()
                        if ji + 1 < len(jobs):
                            ensure_prep(ji + 1, upto_b=False)
                    if ki == kb and ji + 1 < len(jobs):
                        ensure_prep(ji + 1)
                    if i + LA < len(flat):
                        ji2, ki2 = flat[i + LA]
                        ensure_prep(ji2)
                        inflight.append(emit_S(jobs[ji2], ktiles[ki2]))
                    pb, pt = inflight.pop(0)
                    s = i % 2
                    fw.act(pT[s][:, 0:nb], pb[:, 0:nb], AF.Exp, scale=job["scale"], R=[pt], W=[t_pT[s]])
                    fw.mm(acc[0:65, 0:nb], job["v_of"](ktiles[ki]), pT[s][:, 0:nb], start=(ki == 0), stop=(ki == nk - 1),
                          R=job["Rv"] + [t_pT[s]], W=[acct])
                    if ki == min(10, nk - 1) and pend_fin is not None:
                        finish_b(pend_fin)
                        pend_fin = None
                    if ki == nk - 1:
                        if pend_fin is not None:
                            finish_b(pend_fin)
                        finish_a(job, acc, acct)
                        pend_fin = job
                if pend_fin is not None:
                    finish_b(pend_fin)
            fw.barrier()
            ar.release(m)
        def load_wo(l, wo):
            wsrc = dr["w_out"][l].rearrange("(k p) n -> p k n", p=128)
            load_w(wo[:, :, 0:512], wsrc[:, :, 0:512], wt("wo0"))
            load_w(wo[:, :, 512:1024], wsrc[:, :, 512:1024], wt("wo1"))

        def phase_merge(l, blocks, outs, merged, t_mg, wo):
            m = ar.mark()
            wg = ar.alloc([3, KC, 512], BF16); wb = ar.alloc([3, 4, 512], BF16)
            sg = [ar.alloc([512], F32) for _ in range(2)]; t_sg = [Trk() for _ in range(2)]
            mm_ = ar.alloc([512], F32); t_mm = Trk()
            tt2 = ar.alloc([512], F32); t_tt2 = Trk()
            wnames = ("w_ba", "w_bl", "w_bm")
            t_wg = [wt("wg%d" % i) for i in range(3)]; t_wb = [wt("wb%d" % i) for i in range(3)]
            for half in range(2):
                for br in range(3):
                    c0 = C_G + br * 1024 + half * 512
                    load_w(wg[:, br, :, :], win(l, c0, c0 + 512), t_wg[br])
                    load_w(wb[:, br, :, :], dr[wnames[br]][l].rearrange("(j p) n -> p j n", p=128)[:, :, half * 512:(half + 1) * 512], t_wb[br])
                if half == 1:
                    load_wo(l, wo)
                for bi in blocks:
                    t0, nb = BLOCKS[bi]
                    for ff in range(4):
                        f = half * 4 + ff
                        for br in range(3):
                            o_br, t_obr = outs[br]
                            pg_, ptg = ring.next()
                            for k in range(KC):
                                fw.mm(pg_[:, 0:nb], wg[:, br, k, ff * 128:(ff + 1) * 128], hT[:, k, t0:t0 + nb], start=(k == 0),
                                      stop=(k == KC - 1), R=[t_wg[br], t_h[bi]], W=[ptg])
                            s = br % 2
                            fw.act(sg[s][:, 0:nb], pg_[:, 0:nb], AF.Sigmoid, R=[ptg], W=[t_sg[s]])
                            pb, pt = ring.next()
                            for j in range(4):
                                fw.mm(pb[:, 0:nb], wb[:, br, j, ff * 128:(ff + 1) * 128], o_br[:, j, t0:t0 + nb], start=(j == 0),
                                      stop=(j == 3), R=[t_wb[br], t_obr], W=[pt])
                            if br == 0:
                                fw.tt("dve", mm_[:, 0:nb], sg[s][:, 0:nb], pb[:, 0:nb], ALU.mult, R=[t_sg[s], pt], W=[t_mm])
                            else:
                                fw.tt("dve", tt2[:, 0:nb], sg[s][:, 0:nb], pb[:, 0:nb], ALU.mult, R=[t_sg[s], pt], W=[t_tt2])
                                if br == 1:
                                    fw.tt("dve", mm_[:, 0:nb], mm_[:, 0:nb], tt2[:, 0:nb], ALU.add, R=[t_mm, t_tt2], W=[t_mm])
                                else:
                                    fw.tt("dve", merged[:, f, t0:t0 + nb], mm_[:, 0:nb], tt2[:, 0:nb], ALU.add,
                                          R=[t_mm, t_tt2], W=[t_mg[bi]])
            fw.barrier()
            ar.release(m)
        def phase_wout(l, blocks, merged, t_mg, wo):
            m = ar.mark()
            t_wo = [wt("wo0"), wt("wo1")]
            xb = [ar.alloc([KC, 512], F32) for _ in range(2)]; t_xb = [Trk() for _ in range(2)]
            for bi in blocks:
                t0, nb = BLOCKS[bi]
                s = blk_s(bi)
                bs = bi % 2
                fw.dma("sp", xb[bs][:, :, 0:nb], xs_d[:, :, t0:t0 + nb], R=[xs_t[bi]], W=[t_xb[bs]], key=t_xb[bs])
                for f in range(KC):
                    pb, pt = ring.next()
                    for k in range(KC):
                        fw.mm(pb[:, 0:nb], wo[:, k, f * 128:(f + 1) * 128], merged[:, k, t0:t0 + nb], start=(k == 0),
                              stop=(k == KC - 1), R=[t_wo[f // 4], t_mg[bi]], W=[pt])
                    fw.stt("dve", xb[bs][:, f, 0:nb], pb[:, 0:nb], der[:, 2, 2 * f + s:2 * f + s + 1], xb[bs][:, f, 0:nb],
                           ALU.mult, ALU.add, R=[pt, t_der, t_xb[bs]], W=[t_xb[bs]])
                fw.dma("sp", xs_d[:, :, t0:t0 + nb], xb[bs][:, :, 0:nb], R=[t_xb[bs]], W=[xs_t[bi]], key=t_xb[bs])
            fw.barrier()
            ar.release(m)
        def moe_weights():
            moe_weights.base = ar.top
            wgu = [ar.alloc([KC, 1024], BF16) for _ in range(2)]
            wdn = [ar.alloc([4, D], BF16) for _ in range(2)]
            t_we = [[wt("we%d_%d" % (s_, i)) for i in range(2)] for s_ in range(2)]
            return wgu, wdn, t_we

        def phase_moe(l, blocks, combT, t_comb, yacc, t_y, moew):
            m = ar.mark()
            selE = ar.alloc([NE, 128], F32, parts=NE); t_sel = Trk()
            fw.dma("sp", selE, dr["selE"].rearrange("k (e m) -> k e m", e=NE), W=[t_sel])
            wgu, wdn, t_we = moew
            cb = ar.alloc([512], F32); t_cb = Trk()
            ss = [ar.alloc([512], F32) for _ in range(2)]; t_ss = [Trk() for _ in range(2)]
            tu = [ar.alloc([512], F32) for _ in range(2)]; t_tu = [Trk() for _ in range(2)]
            actT = [ar.alloc([4, 512], BF16) for _ in range(2)]; t_act = [Trk() for _ in range(2)]
            def load_e(e):
                s = e % 2
                load_w(wgu[s], dr["wgu"][l, e].rearrange("(k p) n -> p k n", p=128), t_we[s][0])
                load_w(wdn[s], dr["wd"][l, e].rearrange("(j p) n -> p j n", p=128), t_we[s][1])
            def emit_gu(e, bi, idx):
                s = e % 2
                t0, nb = BLOCKS[bi]
                pb, pt = ring.next()
                fw.mm(pb[:, 0:nb], selE[0:NE, e, :], combT[0:NE, t0:t0 + nb], R=[t_sel, t_comb], W=[pt])
                fw.copy("act", cb[:, 0:nb], pb[:, 0:nb], R=[pt], W=[t_cb])
                a_s = idx % 2
                for ff in range(4):
                    pg_, ptg = ring.next()
                    for k in range(KC):
                        fw.mm(pg_[:, 0:nb], wgu[s][:, k, ff * 128:(ff + 1) * 128], hT[:, k, t0:t0 + nb], start=(k == 0),
                              stop=(k == KC - 1), R=[t_we[s][0], t_h[bi]], W=[ptg])
                    pu_, ptu = ring.next()
                    for k in range(KC):
                        fw.mm(pu_[:, 0:nb], wgu[s][:, k, 512 + ff * 128:512 + (ff + 1) * 128], hT[:, k, t0:t0 + nb], start=(k == 0),
                              stop=(k == KC - 1), R=[t_we[s][0], t_h[bi]], W=[ptu])
                    q = ff % 2
                    fw.act(ss[q][:, 0:nb], pg_[:, 0:nb], AF.Silu, R=[ptg], W=[t_ss[q]])
                    fw.tt("dve", tu[q][:, 0:nb], ss[q][:, 0:nb], pu_[:, 0:nb], ALU.mult, R=[t_ss[q], ptu], W=[t_tu[q]])
                    fw.tt("pool", actT[a_s][:, ff, 0:nb], tu[q][:, 0:nb], cb[:, 0:nb], ALU.mult, R=[t_tu[q], t_cb], W=[t_act[a_s]])

            def emit_down(e, bi, idx):
                s = e % 2
                t0, nb = BLOCKS[bi]
                a_s = idx % 2
                for f in range(KC):
                    pb, pt = ring.next()
                    for j in range(4):
                        fw.mm(pb[:, 0:nb], wdn[s][:, j, f * 128:(f + 1) * 128], actT[a_s][:, j, 0:nb], start=(j == 0),
                              stop=(j == 3), R=[t_we[s][1], t_act[a_s]], W=[pt])
                    if e == 0:
                        fw.copy("act", yacc[:, f, t0:t0 + nb], pb[:, 0:nb], R=[pt], W=[t_y[bi]])
                    else:
                        fw.tt("dve", yacc[:, f, t0:t0 + nb], yacc[:, f, t0:t0 + nb], pb[:, 0:nb], ALU.add,
                              R=[pt, t_y[bi]], W=[t_y[bi]])

            items = [(e, bi) for e in range(NE) for bi in blocks]
            prev = None
            for idx, (e, bi) in enumerate(items):
                emit_gu(e, bi, idx)
                if prev is not None:
                    emit_down(*prev)
                if bi == blocks[0] and e + 1 < NE:
                    load_e(e + 1)
                prev = (e, bi, idx)
            emit_down(*prev)
            fw.barrier()
            ar.release(m)
        def phase_ffn_res(l, blocks, yacc, t_y, last):
            m = ar.mark()
            save_top = ar.top
            ar.top = moe_weights.base
            xb = [ar.alloc([KC, 512], F32) for _ in range(2)]; t_xb = [Trk() for _ in range(2)]
            if last:
                sq = ar.alloc([KC, 512], F32); t_sq = Trk()
            assert ar.top <= moe_weights.base + 12288
            ar.top = save_top
            if last:
                rstd = ar.alloc([512], F32); t_rstd = Trk()
                ot = [ar.alloc([D], F32) for _ in range(2)]; t_ot = [Trk() for _ in range(2)]
            oi = 0
            for bi in blocks:
                t0, nb = BLOCKS[bi]
                s = blk_s(bi)
                bs = bi % 2
                fw.dma("sp", xb[bs][:, :, 0:nb], xs_d[:, :, t0:t0 + nb], R=[xs_t[bi]], W=[t_xb[bs]], key=t_xb[bs])
                for f in range(KC):
                    fw.stt("dve", xb[bs][:, f, 0:nb], yacc[:, f, t0:t0 + nb], der[:, 5, 2 * f + s:2 * f + s + 1], xb[bs][:, f, 0:nb],
                           ALU.mult, ALU.add, R=[t_y[bi], t_der, t_xb[bs]], W=[t_xb[bs]])
                if not last:
                    fw.dma("sp", xs_d[:, :, t0:t0 + nb], xb[bs][:, :, 0:nb], R=[t_xb[bs]], W=[xs_t[bi]], key=t_xb[bs])
                    continue
                fw.act(sq[:, :, 0:nb], xb[bs][:, :, 0:nb], AF.Square, R=[t_xb[bs]], W=[t_sq])
                pb, pt = ring.next()
                for k in range(KC):
                    fw.mm(pb[:, 0:nb], ones32, sq[:, k, 0:nb], start=(k == 0), stop=(k == KC - 1), R=[t_sq, t_c], W=[pt])
                rms_rstd(pb, pt, nb, 1.0 / D, rstd, t_rstd)
                for k in range(KC):
                    fw.stt("dve", xb[bs][:, k, 0:nb], xb[bs][:, k, 0:nb], pvc("final_norm", k), rstd[:, 0:nb], ALU.mult, ALU.mult,
                           R=[t_xb[bs], t_pv, t_rstd], W=[t_xb[bs]])
                for ti in range(nb // 128):
                    os_ = oi % 2
                    oi += 1
                    for hf in range(2):
                        pb, pt = ring.next()
                        for kk in range(4):
                            k = hf * 4 + kk
                            fw.tr(pb[:, kk * 128:(kk + 1) * 128], xb[bs][:, k, ti * 128:(ti + 1) * 128], ident,
                                  R=[t_xb[bs], t_c], W=[pt])
                        fw.copy("act" if hf == 0 else "dve", ot[os_][:, hf * 512:(hf + 1) * 512], pb[:, 0:512], R=[pt], W=[t_ot[os_]])
                    r0 = t0 - NCTX + ti * 128
                    fw.dma("sp", out_d[r0:r0 + 128, :], ot[os_], R=[t_ot[os_]], W=[t_out], key=t_ot[os_])
            fw.barrier()
            ar.release(m)
        def ck(name, bufs):
            if stop != name:
                return
            fw.barrier()
            for key, ap in bufs.items():
                if key not in dumps:
                    continue
                d = nc.dram_tensor("dbg_" + key, list(ap.shape), F32, kind="ExternalOutput").ap()
                fw.dma("pool", d, ap, W=[Trk()])
            raise _Stop()
        ALLB = [0, 1, 2, 3, 4]
        LATB = [1, 2, 3, 4]
        try:
            phase_load()
            ck("load", dict(xs=xs_d))
            for l in range(depth):
                last = (l == DEPTH - 1)
                qblocks = LATB if last else ALLB
                lm = ar.mark()
                o_lru = ar.alloc([4, T], BF16); t_olru = Trk()
                om = ar.mark()
                lruw = lru_weights(l)
                phase_mod(l)
                ck("mod%d" % l, dict(mod=modsb, der=der, cneg=cneg))
                phase_norm(l, 1, ALLB)
                ck("norm%d" % l, dict(hT=hT))
                phase_lru(l, o_lru, t_olru, lruw)
                ck("lru%d" % l, dict(o_lru=o_lru))
                ar.release(om)
                o_attn = ar.alloc([4, T], BF16); t_oattn = Trk()
                o_mla = ar.alloc([4, T], BF16); t_omla = Trk()
                kvm = ar.mark()
                kTg = ar.alloc([T], BF16); Vg = ar.alloc([NT, 2, 65], BF16)
                kTm = ar.alloc([8, T], BF16, parts=96); Vm = ar.alloc([NT, 8, 65], BF16)
                kv = (kTg, Vg, kTm, Vm, Trk(), Trk(), Trk(), Trk())
                phase_kv(l, kv)
                ck("kv%d" % l, dict(kTg=kTg, kTm=kTm, Vg=Vg, Vm=Vm))
                phase_attn(l, qblocks, kv, o_attn, t_oattn, o_mla, t_omla)
                ck("attn%d" % l, dict(o_attn=o_attn, o_mla=o_mla))
                ar.release(kvm)
                merged = ar.alloc([KC, T], BF16); t_mg = [Trk() for _ in range(5)]
                wo = ar.alloc([KC, D], BF16)
                phase_merge(l, qblocks, ((o_attn, t_oattn), (o_lru, t_olru), (o_mla, t_omla)), merged, t_mg, wo)
                ck("merge%d" % l, dict(merged=merged))
                phase_wout(l, qblocks, merged, t_mg, wo)
                ck("wout%d" % l, dict(xs=xs_d))
                ar.release(lm)
                combT = ar.alloc([T], F32, parts=NE); t_comb = Trk()
                moew = moe_weights()
                _wgu, _wdn, _twe = moew
                hooks = {
                    0: (lambda l=l: load_w(_wgu[0], dr["wgu"][l, 0].rearrange("(k p) n -> p k n", p=128), _twe[0][0])),
                    2: (lambda l=l: load_w(_wdn[0], dr["wd"][l, 0].rearrange("(j p) n -> p j n", p=128), _twe[0][1])),
                }
                phase_norm(l, 2, qblocks, combT, t_comb, hooks=hooks)
                ck("normf%d" % l, dict(hT=hT, combT=combT))
                yacc = ar.alloc([KC, T], F32); t_y = [Trk() for _ in range(5)]
                phase_moe(l, qblocks, combT, t_comb, yacc, t_y, moew)
                ck("moe%d" % l, dict(yacc=yacc))
                phase_ffn_res(l, qblocks, yacc, t_y, last)
                ck("res%d" % l, dict(xs=xs_d))
                ar.release(lm)
        except _Stop:
            pass
        fw.wait_all("sp", [t_out] + xs_t)
        fw.barrier()
        fw.emit()
        build_nc.stats = (fw.ninst, fw.nwait, len(fw.dsems), ar.peak)
    return nc

_CACHE = {}

def kernel(**inputs):
    shared, per_core = _prepare(inputs)
    if "nc" not in _CACHE:
        _CACHE["nc"] = build_nc()
    nc = _CACHE["nc"]
    in_maps = []
    for pc in per_core:
        d = dict(shared)
        d.update(pc)
        in_maps.append(d)
    res = run_bass_kernel_spmd(nc, in_maps, core_ids=list(range(len(in_maps))))
    out = np.stack([np.asarray(r["out"], np.float32) for r in res.results], 0)
    return out
```

```python
import numpy as np
from contextlib import ExitStack
import concourse.bass as bass
import concourse.mybir as mybir
from concourse.bass_utils import run_bass_kernel_spmd
F32 = mybir.dt.float32
BF16 = mybir.dt.bfloat16
AF = mybir.ActivationFunctionType
ALU = mybir.AluOpType
AX = mybir.AxisListType
D = 1024
KC = 8
NCTX = 256
SEQ = 2048
T = NCTX + SEQ
NT = T // 128
DEPTH = 2
BLOCKS = [(0, 256), (256, 512), (768, 512), (1280, 512), (1792, 512)]
IN_WIDTH = 5536
C_Q, C_K, C_V, C_ZX, C_ZY, C_CQ, C_CKV, C_KR, C_G = 0, 512, 640, 768, 1280, 1792, 2176, 2432, 2464
EPS = 1e-6
NE = 16
import os
NLOADS = int(os.environ.get("K_NLOADS", "1"))

class Trk:
    __slots__ = ("name", "w", "r", "sem")
    def __init__(self, name=""):
        self.name = name
        self.w = None
        self.r = {}
        self.sem = None

class FW:
    def __init__(self, nc, stack):
        self.nc = nc
        self.stack = stack
        self.engs = ("pe", "act", "dve", "pool", "sp")
        self.prog = {e: [] for e in self.engs}
        self.sem = {}
        self.cnt = {}
        for e in ("pe", "act", "dve", "pool"):
            self.sem[e] = stack.enter_context(nc.semaphore("s_" + e))
            self.cnt[e] = 0
        self.known = {e: {} for e in self.engs}
        self.dsems = []
        self.ninst = 0
        self.nwait = 0
    def sb(self, name, shape, dt):
        return self.stack.enter_context(self.nc.sbuf_tensor(name, list(shape), dt))
    def ps(self, name, shape, dt=F32):
        return self.stack.enter_context(self.nc.psum_tensor(name, list(shape), dt))
    def _wait(self, e, tok):
        if tok is None:
            return
        key, semh, val = tok
        if key == e and e == "pe":
            return
        kn = self.known[e]
        if kn.get(key, 0) >= val:
            return
        self.prog[e].append(lambda eng, s=semh, v=val: eng.wait_ge(s, v))
        kn[key] = val
        self.nwait += 1
    def _deps(self, e, R, W):
        for t in R:
            self._wait(e, t.w)
        for t in W:
            self._wait(e, t.w)
            for tok in t.r.values():
                self._wait(e, tok)
    @staticmethod
    def _commit(tok, R, W):
        for t in R:
            t.r[tok[0]] = tok
        for t in W:
            t.w = tok
            t.r = {}
    def op(self, e, fn, R=(), W=()):
        self._deps(e, R, W)
        self.cnt[e] += 1
        semh = self.sem[e]
        self.prog[e].append(lambda eng, f=fn, s=semh: f(eng).then_inc(s, 1))
        self._commit((e, semh, self.cnt[e]), R, W)
        self.ninst += 1
    def mm(self, out, lhsT, rhs, start=True, stop=True, R=(), W=()):
        self.op("pe", lambda eng: eng.matmul(out, lhsT, rhs, start=start, stop=stop), R, W)
    def tr(self, out, in_, ident, R=(), W=()):
        self.op("pe", lambda eng: eng.transpose(out, in_, ident), R, W)
    def act(self, out, in_, func, bias=None, scale=None, R=(), W=()):
        kw = {}
        if bias is not None:
            kw["bias"] = bias
        if scale is not None:
            kw["scale"] = scale
        self.op("act", lambda eng: eng.activation(out, in_, func, **kw), R, W)
    def copy(self, e, out, in_, R=(), W=()):
        if e == "act":
            self.op("act", lambda eng: eng.activation(out, in_, AF.Copy), R, W)
        else:
            self.op(e, lambda eng: eng.tensor_copy(out, in_), R, W)
    def tt(self, e, out, in0, in1, op, R=(), W=()):
        self.op(e, lambda eng: eng.tensor_tensor(out, in0, in1, op), R, W)
    def ts(self, e, out, in0, s1, s2, op0, op1=None, R=(), W=()):
        if op1 is None:
            self.op(e, lambda eng: eng.tensor_scalar(out, in0, s1, None, op0), R, W)
        else:
            self.op(e, lambda eng: eng.tensor_scalar(out, in0, s1, s2, op0, op1), R, W)
    def stt(self, e, out, in0, scalar, in1, op0, op1, R=(), W=()):
        self.op(e, lambda eng: eng.scalar_tensor_tensor(out, in0, scalar, in1, op0, op1), R, W)
    def memset(self, e, ap, val, W=()):
        self.op(e, lambda eng: eng.memset(ap, val), (), W)
    def recip(self, out, in_, R=(), W=()):
        self.op("dve", lambda eng: eng.reciprocal(out, in_), R, W)
    def reduce(self, out, in_, op, R=(), W=()):
        self.op("dve", lambda eng: eng.tensor_reduce(out, in_, AX.X, op), R, W)
    def scan(self, out, d0, d1, init, R=(), W=()):
        self.op("dve", lambda eng: eng.tensor_tensor_scan(out, d0, d1, init, ALU.mult, ALU.add), R, W)
    def dma(self, q, out, in_, R=(), W=(), key=None, **kw):
        if key is None:
            key = W[0]
        if key.sem is None:
            n = len(self.dsems)
            key.sem = [self.stack.enter_context(self.nc.semaphore("d%d" % n)), 0, "d%d" % n]
            self.dsems.append(key.sem)
        semh, c, kname = key.sem
        self._deps(q, R, W)
        if c > 0:
            self._wait(q, (kname, semh, c))
        self.prog[q].append(lambda eng, o=out, i=in_, s=semh, k=kw: eng.dma_start(out=o, in_=i, **k).then_inc(s, 16))
        key.sem[1] = c + 16
        self._commit((kname, semh, c + 16), R, W)
    def wait_all(self, e, trks):
        for t in trks:
            self._wait(e, t.w)
            for tok in t.r.values():
                self._wait(e, tok)
    def barrier(self):
        for e in self.engs:
            for c in ("pe", "act", "dve", "pool"):
                if self.cnt[c] > 0 and not (c == e == "pe"):
                    self._wait(e, (c, self.sem[c], self.cnt[c]))
            for semh, cval, kname in self.dsems:
                if cval > 0:
                    self._wait(e, (kname, semh, cval))
    def emit(self):
        prog = self.prog
        with self.nc.Block() as block:
            @block.sync
            def _(eng):
                for f in prog["sp"]:
                    f(eng)
            @block.tensor
            def _(eng):
                for f in prog["pe"]:
                    f(eng)
            @block.scalar
            def _(eng):
                for f in prog["act"]:
                    f(eng)
            @block.vector
            def _(eng):
                for f in prog["dve"]:
                    f(eng)
            @block.gpsimd
            def _(eng):
                for f in prog["pool"]:
                    f(eng)

class Arena:
    def __init__(self, tensor, words):
        self.t = tensor
        self.cap = words
        self.top = 0
    def mark(self):
        return self.top
    def release(self, m):
        self.top = m
    def alloc(self, free_shape, dt, parts=128):
        n = int(np.prod(free_shape))
        words = n if dt == F32 else (n + 1) // 2
        assert self.top + words <= self.cap, ("arena overflow", self.top, words, self.cap)
        ap = self.t[0:parts, self.top:self.top + words]
        self.top += words
        self.peak = max(getattr(self, 'peak', 0), self.top)
        if dt != F32:
            ap = ap.bitcast(dt)[:, 0:n]
        if len(free_shape) == 2:
            ap = ap.rearrange("p (a b) -> p a b", a=free_shape[0], b=free_shape[1])
        elif len(free_shape) == 3:
            ap = ap.rearrange("p (a b c) -> p a b c", a=free_shape[0], b=free_shape[1], c=free_shape[2])
        return ap

class Ring:
    def __init__(self, items):
        self.items = items
        self.i = 0
    def next(self):
        it = self.items[self.i]
        self.i = (self.i + 1) % len(self.items)
        return it

def _pv_layout():
    off = {}
    n = 0
    def add(name, w):
        nonlocal n
        off[name] = n
        n += w
    for l in range(DEPTH):
        add(("norm_mix", l), 8)
        add(("norm_ffn", l), 8)
        add(("gq", l), 1)
        add(("gk", l), 1)
        add(("conv_w", l), 16)
        add(("conv_b", l), 4)
        add(("lru_b_a", l), 8)
        add(("lru_b_i", l), 8)
        add(("lru_lam", l), 8)
        add(("gcq", l), 3)
        add(("gckv", l), 2)
        add(("b_mod", l), 48)
    add("final_norm", 8)
    add("eps", 1)
    return off, n

PV_OFF, NPV = _pv_layout()

def _fm(v, k):
    return np.ascontiguousarray(np.asarray(v, np.float32).reshape(k, 128).T)

def _rope_tables():
    s = np.arange(SEQ)
    row = (s // 64).astype(np.float32)
    col = (s % 64).astype(np.float32)
    def tables(dim):
        quarter = dim // 4
        inv = (np.float32(10000.0) ** (-np.arange(quarter, dtype=np.float32) / np.float32(quarter))).astype(np.float32)
        ar = row[:, None] * inv[None, :]
        ac = col[:, None] * inv[None, :]
        cos = np.ones((dim, T), np.float32)
        sin = np.zeros((dim, T), np.float32)
        half = dim // 2
        for d in range(dim):
            ang = ar if d < half else ac
            j = d % quarter
            first = (d % half) < quarter
            cos[d, NCTX:] = np.cos(ang[:, j])
            sin[d, NCTX:] = (-1.0 if first else 1.0) * np.sin(ang[:, j])
        return cos, sin
    cg, sg = tables(64)
    ropeG = np.stack([np.concatenate([cg, cg], 0), np.concatenate([sg, sg], 0)], 0)
    cm, sm = tables(32)
    cosM = np.ones((96, T), np.float32)
    sinM = np.zeros((96, T), np.float32)
    cosM[64:] = cm
    sinM[64:] = sm
    ropeM = np.stack([cosM, sinM], 0)
    def perm(dim):
        quarter = dim // 4
        half = dim // 2
        P = np.zeros((dim, dim), np.float32)
        for m in range(dim):
            first = (m % half) < quarter
            P[m + quarter if first else m - quarter, m] = 1.0
        return P
    pg = np.zeros((128, 128), np.float32)
    pg[0:64, 0:64] = perm(64)
    pg[64:128, 64:128] = perm(64)
    pm = np.zeros((96, 96), np.float32)
    pm[64:96, 64:96] = perm(32)
    return ropeG.astype(np.float32), ropeM.astype(np.float32), pg, pm

def _prepare(inp):
    f32 = np.float32
    shared = {}
    qperm = []
    for j in range(4):
        qperm += list(range(j * 64, j * 64 + 64)) + list(range((4 + j) * 64, (4 + j) * 64 + 64))
    cols = np.array(qperm + list(range(512, IN_WIDTH)))
    shared["w_in"] = np.ascontiguousarray(np.asarray(inp["w_in"], f32)[:, :, cols])
    shared["w_mod"] = np.ascontiguousarray(np.asarray(inp["w_mod"], f32))
    shared["w_qb"] = np.ascontiguousarray(np.asarray(inp["mla_w_qb"], f32))
    kvb = np.asarray(inp["mla_w_kvb"], f32).reshape(DEPTH, 256, 8, 128)
    shared["w_kvb"] = np.ascontiguousarray(
        np.concatenate([kvb[:, :, :, 0:64].reshape(DEPTH, 256, 512), kvb[:, :, :, 64:128].reshape(DEPTH, 256, 512)], -1))
    shared["w_ba"] = np.ascontiguousarray(np.asarray(inp["w_branch_attn"], f32))
    shared["w_bl"] = np.ascontiguousarray(np.asarray(inp["w_branch_lru"], f32))
    shared["w_bm"] = np.ascontiguousarray(np.asarray(inp["w_branch_mla"], f32))
    shared["w_out"] = np.ascontiguousarray(np.asarray(inp["w_out"], f32))
    shared["router_w"] = np.ascontiguousarray(np.asarray(inp["router_w"], f32))
    shared["rbias"] = np.ascontiguousarray(np.broadcast_to(np.asarray(inp["router_bias"], f32)[None, :], (128, NE)))
    shared["wgu"] = np.ascontiguousarray(np.concatenate([np.asarray(inp["moe_w_gate"], f32), np.asarray(inp["moe_w_up"], f32)], -1))
    shared["wd"] = np.ascontiguousarray(np.asarray(inp["moe_w_down"], f32))
    lw = np.zeros((DEPTH, 2, 2, 4, 128, 128), f32)
    for gi, nm in enumerate(("lru_w_a", "lru_w_i")):
        w = np.asarray(inp[nm], f32)
        for c in range(4):
            lw[:, :, gi, c, 0:64, 0:64] = w[:, :, 2 * c]
            lw[:, :, gi, c, 64:128, 64:128] = w[:, :, 2 * c + 1]
    shared["lruW"] = lw
    ropeG, ropeM, pg, pm = _rope_tables()
    shared["ropeG"] = ropeG
    shared["ropeM"] = ropeM
    shared["permG"] = pg
    shared["permM"] = pm
    shared["ident"] = np.eye(128, dtype=f32)
    bo = np.zeros((128, 128), f32)
    bo[0:64, 0:64] = 1.0
    bo[64:, 64:] = 1.0
    shared["blockones"] = bo
    sel = np.zeros((NE, NE, 128), f32)
    for e in range(NE):
        sel[e, e, :] = 1.0
    shared["selE"] = sel.reshape(NE, NE * 128)
    pv = np.zeros((128, NPV), f32)
    def put(key, arr):
        o = PV_OFF[key]
        pv[:, o:o + arr.shape[1]] = arr
    for l in range(DEPTH):
        put(("norm_mix", l), _fm(inp["norm_mix"][l], 8))
        put(("norm_ffn", l), _fm(inp["norm_ffn"][l], 8))
        put(("gq", l), np.tile(np.asarray(inp["gqa_q_norm"][l], f32), 2)[:, None])
        put(("gk", l), np.tile(np.asarray(inp["gqa_k_norm"][l], f32), 2)[:, None])
        cw = np.asarray(inp["conv_w"][l], f32)
        put(("conv_w", l), np.concatenate([_fm(cw[j], 4) for j in range(4)], 1))
        put(("conv_b", l), _fm(inp["conv_b"][l], 4))
        for nm in ("lru_b_a", "lru_b_i", "lru_lam"):
            a = np.asarray(inp[nm][l], f32)
            put((nm, l), np.concatenate([_fm(a[0], 4), _fm(a[1], 4)], 1))
        put(("gcq", l), _fm(inp["mla_q_a_norm"][l], 3))
        put(("gckv", l), _fm(inp["mla_kv_a_norm"][l], 2))
        put(("b_mod", l), _fm(inp["b_mod"][l], 48))
    put("final_norm", _fm(inp["final_norm"], 8))
    pv[:, PV_OFF["eps"]] = EPS
    shared["pv"] = pv
    per_core = []
    x = np.asarray(inp["x"], f32)
    ctx = np.asarray(inp["ctx"], f32)
    c = np.asarray(inp["c"], f32)
    cc = np.asarray(inp["c_ctx"], f32)
    for b in range(x.shape[0]):
        xin = np.ascontiguousarray(np.concatenate([ctx[b], x[b]], 0))
        cv = np.stack([_fm(c[b], 8), _fm(cc, 8)], -1).reshape(128, 16)
        per_core.append({"xin": xin, "cvec": np.ascontiguousarray(cv)})
    return shared, per_core

SHARED_SHAPES = {
    "w_in": [DEPTH, D, IN_WIDTH], "w_mod": [DEPTH, D, 6 * D], "w_qb": [DEPTH, 384, 768], "w_kvb": [DEPTH, 256, 1024],
    "w_ba": [DEPTH, 512, D], "w_bl": [DEPTH, 512, D], "w_bm": [DEPTH, 512, D], "w_out": [DEPTH, D, D],
    "router_w": [D, NE], "rbias": [128, NE], "wgu": [DEPTH, NE, D, 1024],
    "wd": [DEPTH, NE, 512, D], "lruW": [DEPTH, 2, 2, 4, 128, 128], "ropeG": [2, 128, T], "ropeM": [2, 96, T],
    "permG": [128, 128], "permM": [96, 96], "ident": [128, 128], "blockones": [128, 128], "selE": [NE, NE * 128],
    "pv": [128, NPV], "xin": [T, D], "cvec": [128, 16],
}

class _Stop(Exception):
    pass
def build_nc(depth=DEPTH, stop=None, dumps=()):
    nc = bass.Bass("TRN2", target_bir_lowering=False)
    dr = {k: nc.dram_tensor(k, v, F32, kind="ExternalInput").ap() for k, v in SHARED_SHAPES.items()}
    out_d = nc.dram_tensor("out", [SEQ, D], F32, kind="ExternalOutput").ap()
    xs_d = nc.dram_tensor("xs_scratch", [128, KC, T], F32, kind="Internal").ap()
    with ExitStack() as st:
        fw = FW(nc, st)
        ARW = 53200
        ar = Arena(fw.sb("arena", [128, ARW], F32), ARW)
        pp_ = [fw.ps("pp%d" % i, [128, 1024], F32) for i in range(4)]
        psb = [pp_[i // 2][:, (i % 2) * 512:(i % 2 + 1) * 512] for i in range(8)]
        pst = [Trk("ps%d" % i) for i in range(8)]
        ring = Ring([(psb[i], pst[i]) for i in range(6)])
        accr = Ring([(psb[i], pst[i]) for i in (6, 7)])
        pv = ar.alloc([NPV], F32); t_pv = Trk("pv")
        ident = ar.alloc([128], F32); t_c = Trk("consts")
        ones32 = ar.alloc([128], F32)
        bones = ar.alloc([128], F32)
        sel64 = ar.alloc([64], F32)
        permG = ar.alloc([128], BF16)
        permM = ar.alloc([96], BF16, parts=96)
        cvec = ar.alloc([16], F32)
        actc = ar.alloc([16], F32)
        rbias = ar.alloc([NE], F32)
        rw = ar.alloc([KC, NE], F32)
        modsb = ar.alloc([96], F32); t_mod = Trk("mod")
        der = ar.alloc([6, 16], F32); t_der = Trk("der")
        cneg = ar.alloc([8], F32); t_cneg = Trk("cneg")
        cneg2 = ar.alloc([8], F32)
        hT = ar.alloc([KC, T], BF16)
        t_h = [Trk("h%d" % b) for b in range(5)]
        xs_t = [Trk("xs%d" % b) for b in range(5)]
        t_out = Trk("out")
        eps_ap = pv[:, PV_OFF["eps"]:PV_OFF["eps"] + 1]
        fw.dma("sp", pv, dr["pv"], W=[t_pv])
        fw.dma("sp", ident, dr["ident"], W=[t_c])
        fw.dma("sp", bones, dr["blockones"], W=[t_c])
        fw.dma("pool", permG, dr["permG"], W=[t_c])
        fw.dma("pool", permM, dr["permM"], W=[t_c])
        fw.dma("sp", cvec, dr["cvec"], W=[t_c])
        fw.dma("sp", rbias, dr["rbias"], W=[t_c])
        fw.dma("sp", rw, dr["router_w"].rearrange("(k p) e -> p k e", p=128), W=[t_c])
        fw.memset("dve", ones32, 1.0, W=[t_c])
        fw.memset("dve", sel64, 0.0, W=[t_c])
        fw.memset("dve", sel64[64:65, :], 1.0, W=[t_c])
        fw.act(actc, cvec, AF.Silu, R=[t_c], W=[t_c])
        base_mark = ar.mark()
        def pvc(key, j=0, n=1):
            o = PV_OFF[key] + j
            return pv[:, o:o + n]
        def blk_s(b):
            return 1 if b == 0 else 0
        def phase_load():
            m = ar.mark()
            xt = [ar.alloc([D], F32) for _ in range(2)]; t_xt = [Trk() for _ in range(2)]
            xo = [ar.alloc([KC, 128], F32) for _ in range(2)]; t_xo = [Trk() for _ in range(2)]
            for i in range(NT):
                bi = next(b for b, (t0, nb) in enumerate(BLOCKS) if t0 <= i * 128 < t0 + nb)
                s = i % 2
                fw.dma("sp", xt[s], dr["xin"][i * 128:(i + 1) * 128, :], W=[t_xt[s]])
                for hf in range(2):
                    pb, pt = ring.next()
                    for kk in range(4):
                        k = hf * 4 + kk
                        fw.tr(pb[:, kk * 128:(kk + 1) * 128], xt[s][:, k * 128:(k + 1) * 128], ident,
                              R=[t_xt[s], t_c], W=[pt])
                    fw.copy("act" if hf == 0 else "dve", xo[s][:, hf * 4:(hf + 1) * 4, :],
                            pb[:, :].rearrange("p (a b) -> p a b", a=4), R=[pt], W=[t_xo[s]])
                fw.dma("sp", xs_d[:, :, i * 128:(i + 1) * 128], xo[s], R=[t_xo[s]], W=[xs_t[bi]])
            fw.barrier()
            ar.release(m)
        def phase_mod(l):
            m = ar.mark()
            wm = [ar.alloc([KC, 512], F32) for _ in range(2)]; t_wm = [Trk() for _ in range(2)]
            pb, pt = accr.next()
            wsrc = dr["w_mod"][l].rearrange("(k p) n -> p k n", p=128)
            for g in range(12):
                s = g % 2
                fw.dma("sp", wm[s], wsrc[:, :, g * 512:(g + 1) * 512], W=[t_wm[s]])
                for jj in range(4):
                    j = g * 4 + jj
                    for k in range(KC):
                        fw.mm(pb[:, 2 * j:2 * j + 2], wm[s][:, k, jj * 128:(jj + 1) * 128], actc[:, 2 * k:2 * k + 2],
                              start=(k == 0), stop=(k == KC - 1), R=[t_wm[s], t_c], W=[pt])
            bm = pvc(("b_mod", l), 0, 48)
            for s in range(2):
                fw.tt("dve", modsb[:, s:96:2], pb[:, s:96:2], bm, ALU.add, R=[pt, t_pv], W=[t_mod])
            def modv(mi):
                return modsb[:, mi * 16:(mi + 1) * 16]
            for (di, mi_scale, gkey) in ((0, 1, ("norm_mix", l)), (3, 4, ("norm_ffn", l))):
                for s in range(2):
                    fw.stt("dve", der[:, di, s:16:2], modv(mi_scale)[:, s:16:2], 1.0, pvc(gkey, 0, 8), ALU.add, ALU.mult,
                           R=[t_mod, t_pv], W=[t_der])
            for (di, mi) in ((1, 0), (2, 2), (4, 3), (5, 5)):
                fw.copy("dve", der[:, di, :], modv(mi), R=[t_mod], W=[t_der])
            tmp = ar.alloc([6, 8], F32); t_tmp = Trk()
            lam = pvc(("lru_lam", l), 0, 8)
            e_, l_, p_, mk, a_, b_ = (tmp[:, i, :] for i in range(6))
            fw.act(e_, lam, AF.Exp, scale=-1.0, R=[t_pv], W=[t_tmp])
            fw.act(l_, e_, AF.Ln, bias=1.0, R=[t_tmp], W=[t_tmp])
            fw.ts("dve", p_, e_, -0.2, 0.25, ALU.mult, ALU.add, R=[t_tmp], W=[t_tmp])
            for cst in (1.0 / 3.0, 0.5, 1.0):
                fw.tt("dve", p_, p_, e_, ALU.mult, R=[t_tmp], W=[t_tmp])
                fw.ts("dve", p_, p_, -1.0, cst, ALU.mult, ALU.add, R=[t_tmp], W=[t_tmp])
            fw.tt("dve", p_, p_, e_, ALU.mult, R=[t_tmp], W=[t_tmp])
            fw.ts("dve", mk, e_, 0.1, None, ALU.is_lt, R=[t_tmp], W=[t_tmp])
            fw.tt("dve", a_, p_, l_, ALU.subtract, R=[t_tmp], W=[t_tmp])
            fw.tt("dve", a_, a_, mk, ALU.mult, R=[t_tmp], W=[t_tmp])
            fw.tt("dve", a_, a_, l_, ALU.add, R=[t_tmp], W=[t_tmp])
            fw.ts("dve", cneg, a_, -8.0, None, ALU.mult, R=[t_tmp], W=[t_cneg])
            fw.ts("dve", cneg2, a_, -16.0, None, ALU.mult, R=[t_tmp, t_cneg], W=[t_cneg])
            fw.barrier()
            ar.release(m)
        def rms_rstd(ssq_ps, pt, n, inv_n, rstd, t_rstd, parts=128):
            fw.act(rstd[0:parts, 0:n], ssq_ps[0:parts, 0:n], AF.Ln, bias=eps_ap[0:parts, :], scale=inv_n,
                   R=[pt, t_pv], W=[t_rstd])
            fw.act(rstd[0:parts, 0:n], rstd[0:parts, 0:n], AF.Exp, scale=-0.5, R=[t_rstd], W=[t_rstd])
        def phase_norm(l, which, blocks, combT=None, t_comb=None, hooks=None):
            m = ar.mark()
            di = 0 if which == 1 else 3
            xb = [ar.alloc([KC, 512], F32) for _ in range(2)]; t_xb = [Trk() for _ in range(2)]
            sq = ar.alloc([KC, 512], F32); t_sq = Trk()
            rstd = ar.alloc([512], F32); t_rstd = Trk()
            tmp = [ar.alloc([512], F32) for _ in range(2)]; t_tmp = [Trk() for _ in range(2)]
            if which == 2:
                h32 = ar.alloc([KC, 512], F32); t_h32 = Trk()
                rts = [ar.alloc([8, NE], F32) for _ in range(4)]; t_rts = [Trk() for _ in range(4)]
            for bidx, bi in enumerate(blocks):
                if hooks and bidx in hooks:
                    hooks[bidx]()
                t0, nb = BLOCKS[bi]
                s = blk_s(bi)
                bs = bi % 2
                fw.dma("sp", xb[bs][:, :, 0:nb], xs_d[:, :, t0:t0 + nb], R=[xs_t[bi]], W=[t_xb[bs]], key=t_xb[bs])
                fw.act(sq[:, :, 0:nb], xb[bs][:, :, 0:nb], AF.Square, R=[t_xb[bs]], W=[t_sq])
                pb, pt = ring.next()
                for k in range(KC):
                    fw.mm(pb[:, 0:nb], ones32, sq[:, k, 0:nb], start=(k == 0), stop=(k == KC - 1), R=[t_sq, t_c], W=[pt])
                rms_rstd(pb, pt, nb, 1.0 / D, rstd, t_rstd)
                for k in range(KC):
                    ts_ = k % 2
                    fw.stt("dve", tmp[ts_][:, 0:nb], xb[bs][:, k, 0:nb], der[:, di, 2 * k + s:2 * k + s + 1], rstd[:, 0:nb],
                           ALU.mult, ALU.mult, R=[t_xb[bs], t_der, t_rstd], W=[t_tmp[ts_]])
                    sh = der[:, di + 1, 2 * k + s:2 * k + s + 1]
                    if which == 1:
                        fw.act(hT[:, k, t0:t0 + nb], tmp[ts_][:, 0:nb], AF.Identity, bias=sh, R=[t_tmp[ts_], t_der], W=[t_h[bi]])
                    else:
                        fw.act(h32[:, k, 0:nb], tmp[ts_][:, 0:nb], AF.Identity, bias=sh, R=[t_tmp[ts_], t_der], W=[t_h32])
                        fw.copy("pool", hT[:, k, t0:t0 + nb], h32[:, k, 0:nb], R=[t_h32], W=[t_h[bi]])
                if which == 2:
                    ntl = nb // 128
                    chains = []
                    for ti in range(ntl):
                        pb, pt = ring.next()
                        for k in range(KC):
                            fw.mm(pb[:, 0:NE], h32[:, k, ti * 128:(ti + 1) * 128], rw[:, k, :], start=(k == 0),
                                  stop=(k == KC - 1), R=[t_h32, t_c], W=[pt])
                        rt_ = rts[ti]; trt = t_rts[ti]
                        sc, sel, tm, sel2, m1, m2, wsel, comb = (rt_[:, i, :] for i in range(8))
                        R_ = [trt]
                        g4 = lambda a: a.rearrange("p (g j) -> p g j", g=4)
                        ops = []
                        A = ops.append
                        A(lambda sc=sc, pb=pb, pt=pt, trt=trt: fw.act(sc, pb[:, 0:NE], AF.Sigmoid, R=[pt], W=[trt]))
                        A(lambda sel=sel, sc=sc, trt=trt: fw.tt("dve", sel, sc, rbias, ALU.add, R=[trt, t_c], W=[trt]))
                        A(lambda m1=m1, sel=sel, trt=trt, g4=g4: fw.reduce(m1[:, 0:4], g4(sel), ALU.max, R=[trt], W=[trt]))
                        for g in range(4):
                            A(lambda g=g, tm=tm, sel=sel, m1=m1, trt=trt: fw.ts("dve", tm[:, 4 * g:4 * g + 4], sel[:, 4 * g:4 * g + 4],
                                                                             m1[:, g:g + 1], -1e9, ALU.is_equal, ALU.mult, R=[trt], W=[trt]))
                        A(lambda sel2=sel2, sel=sel, tm=tm, trt=trt: fw.tt("dve", sel2, sel, tm, ALU.add, R=[trt], W=[trt]))
                        A(lambda m2=m2, sel2=sel2, trt=trt, g4=g4: fw.reduce(m2[:, 0:4], g4(sel2), ALU.max, R=[trt], W=[trt]))
                        A(lambda m1=m1, m2=m2, trt=trt: fw.tt("dve", m1[:, 4:8], m1[:, 0:4], m2[:, 0:4], ALU.add, R=[trt], W=[trt]))
                        A(lambda m1=m1, trt=trt: fw.reduce(m1[:, 8:9], m1[:, 4:8], ALU.max, R=[trt], W=[trt]))
                        A(lambda m1=m1, trt=trt: fw.ts("dve", m1[:, 12:16], m1[:, 4:8], m1[:, 8:9], None, ALU.is_equal, R=[trt], W=[trt]))
                        for g in range(4):
                            A(lambda g=g, tm=tm, sel=sel, m1=m1, m2=m2, trt=trt: fw.ts("dve", tm[:, 4 * g:4 * g + 4], sel[:, 4 * g:4 * g + 4],
                                                                                    m2[:, g:g + 1], m1[:, 12 + g:13 + g], ALU.is_ge, ALU.mult,
                                                                                    R=[trt], W=[trt]))
                        A(lambda wsel=wsel, tm=tm, sc=sc, trt=trt: fw.tt("dve", wsel, tm, sc, ALU.mult, R=[trt], W=[trt]))
                        A(lambda m2=m2, wsel=wsel, trt=trt: fw.reduce(m2[:, 8:9], wsel, ALU.add, R=[trt], W=[trt]))
                        A(lambda m2=m2, trt=trt: fw.recip(m2[:, 9:10], m2[:, 8:9], R=[trt], W=[trt]))
                        A(lambda comb=comb, wsel=wsel, m2=m2, trt=trt: fw.ts("dve", comb, wsel, m2[:, 9:10], None, ALU.mult, R=[trt], W=[trt]))
                        chains.append((ops, comb, trt, ti))
                    for k in range(max(len(c[0]) for c in chains)):
                        for ops, _, _, _ in chains:
                            if k < len(ops):
                                ops[k]()
                    for ops, comb, trt, ti in chains:
                        pb2, pt2 = ring.next()
                        fw.tr(pb2[0:NE, 0:128], comb, ident, R=[trt, t_c], W=[pt2])
                        c0 = t0 + ti * 128
                        fw.copy("act", combT[0:NE, c0:c0 + 128], pb2[0:NE, 0:128], R=[pt2], W=[t_comb])
            fw.barrier()
            ar.release(m)
        WTR = {}
        def wt(name):
            if name not in WTR:
                WTR[name] = Trk(name)
            return WTR[name]
        pend_loads = []
        def load_w(dst, src, trk, R=()):
            if len(pend_loads) >= NLOADS:
                fw._wait("pool", pend_loads.pop(0))
            fw.dma("pool", dst, src, R=list(R), W=[trk])
            pend_loads.append(trk.w)
        def win(l, c0, c1):
            return dr["w_in"][l].rearrange("(k p) n -> p k n", p=128)[:, :, c0:c1]
        def lru_weights(l):
            wz = ar.alloc([KC, 1024], BF16); t_wz = wt("wz0"); t_wz1 = wt("wz1")
            load_w(wz[:, :, 0:512], win(l, C_ZX, C_ZX + 512), t_wz)
            load_w(wz[:, :, 512:1024], win(l, C_ZY, C_ZY + 512), t_wz1)
            lw = ar.alloc([16, 128], BF16); t_lw = wt("lw")
            load_w(lw, dr["lruW"][l].rearrange("d g c p m -> p (d g c) m"), t_lw)
            return wz, lw

        def phase_lru(l, o_lru, t_olru, lruw):
            m = ar.mark()
            wz, lw = lruw
            t_wz = wt("wz0"); t_wz1 = wt("wz1"); t_lw = wt("lw")
            ZP = T + 6
            zx = ar.alloc([ZP], F32); t_zx = Trk()
            u = ar.alloc([T], F32); t_u = Trk()
            ub = ar.alloc([T], BF16); t_ub = Trk()
            gy = ar.alloc([T], F32); t_gy = Trk()
            a_ = ar.alloc([T], F32); t_a = Trk()
            b_ = ar.alloc([T], F32); t_b = Trk()
            hf = ar.alloc([T], F32); t_hf = Trk()
            hb = ar.alloc([T], F32); t_hb = Trk()
            fw.memset("pool", zx, 0.0, W=[t_zx])
            for c in range(4):
                for bi, (t0, nb) in enumerate(BLOCKS):
                    pb, pt = ring.next()
                    for k in range(KC):
                        fw.mm(pb[:, 0:nb], wz[:, k, c * 128:(c + 1) * 128], hT[:, k, t0:t0 + nb], start=(k == 0),
                              stop=(k == KC - 1), R=[t_wz, t_h[bi]], W=[pt])
                    zo = 1 if bi == 0 else t0 + 4
                    fw.copy("act", zx[:, zo:zo + nb], pb[:, 0:nb], R=[pt], W=[t_zx])
                for bi, (t0, nb) in enumerate(BLOCKS):
                    pb, pt = ring.next()
                    for k in range(KC):
                        fw.mm(pb[:, 0:nb], wz[:, k, 512 + c * 128:512 + (c + 1) * 128], hT[:, k, t0:t0 + nb], start=(k == 0),
                              stop=(k == KC - 1), R=[t_wz1, t_h[bi]], W=[pt])
                    fw.act(gy[:, t0:t0 + nb], pb[:, 0:nb], AF.Gelu_apprx_tanh, R=[pt], W=[t_gy])
                for (d0, n, base) in ((0, NCTX, 1), (NCTX, SEQ, 260)):
                    for j in range(4):
                        wj = pvc(("conv_w", l), j * 4 + c)
                        src = zx[:, base - 1 + j:base - 1 + j + n]
                        if j == 0:
                            fw.ts("dve", u[:, d0:d0 + n], src, wj, pvc(("conv_b", l), c), ALU.mult, ALU.add,
                                  R=[t_zx, t_pv], W=[t_u])
                        else:
                            fw.stt("dve", u[:, d0:d0 + n], src, wj, u[:, d0:d0 + n], ALU.mult, ALU.add,
                                   R=[t_zx, t_pv, t_u], W=[t_u])
                fw.copy("act", ub, u, R=[t_u], W=[t_ub])
                for dr_ in range(2):
                    gi = dr_ * 4 + c
                    for bi, (t0, nb) in enumerate(BLOCKS):
                        pb, pt = ring.next()
                        fw.mm(pb[:, 0:nb], lw[:, (dr_ * 2 + 0) * 4 + c, :], ub[:, t0:t0 + nb], R=[t_lw, t_ub], W=[pt])
                        fw.act(a_[:, t0:t0 + nb], pb[:, 0:nb], AF.Sigmoid, bias=pvc(("lru_b_a", l), gi), R=[pt, t_pv], W=[t_a])
                    for bi, (t0, nb) in enumerate(BLOCKS):
                        pb, pt = ring.next()
                        fw.mm(pb[:, 0:nb], lw[:, (dr_ * 2 + 1) * 4 + c, :], ub[:, t0:t0 + nb], R=[t_lw, t_ub], W=[pt])
                        fw.act(b_[:, t0:t0 + nb], pb[:, 0:nb], AF.Sigmoid, bias=pvc(("lru_b_i", l), gi), R=[pt, t_pv], W=[t_b])
                    for bi, (t0, nb) in enumerate(BLOCKS):
                        fw.act(hb[:, t0:t0 + nb], a_[:, t0:t0 + nb], AF.Exp, scale=cneg2[:, gi:gi + 1], R=[t_a, t_cneg], W=[t_hb])
                    for bi, (t0, nb) in enumerate(BLOCKS):
                        fw.act(a_[:, t0:t0 + nb], a_[:, t0:t0 + nb], AF.Exp, scale=cneg[:, gi:gi + 1], R=[t_a, t_cneg], W=[t_a])
                    fw.tt("dve", b_, b_, u, ALU.mult, R=[t_b, t_u], W=[t_b])
                    for bi, (t0, nb) in enumerate(BLOCKS):
                        fw.act(hb[:, t0:t0 + nb], hb[:, t0:t0 + nb], AF.Sqrt, bias=1.0, scale=-1.0, R=[t_hb], W=[t_hb])
                    fw.tt("dve", b_, b_, hb, ALU.mult, R=[t_b, t_hb], W=[t_b])
                    if dr_ == 0:
                        fw.scan(hf, a_, b_, 0.0, R=[t_a, t_b], W=[t_hf])
                    else:
                        fw.scan(hb[:, 0:NCTX][:, ::-1], a_[:, 0:NCTX][:, ::-1], b_[:, 0:NCTX][:, ::-1], 0.0,
                                R=[t_a, t_b], W=[t_hb])
                        fw.scan(hb[:, NCTX:T][:, ::-1], a_[:, NCTX:T][:, ::-1], b_[:, NCTX:T][:, ::-1], hb[:, 0:1],
                                R=[t_a, t_b, t_hb], W=[t_hb])
                fw.tt("dve", hf, hf, hb, ALU.add, R=[t_hf, t_hb], W=[t_hf])
                fw.tt("dve", o_lru[:, c, :], hf, gy, ALU.mult, R=[t_hf, t_gy], W=[t_olru])
            fw.barrier()
            ar.release(m)
        def norm_rope(pb, pt, nb, t0, gain, dst, dst_t, wk):
            (sq, t_sq), (rstd, t_rstd), (kn, t_kn), (cs, t_cs), (t1, t_t1), (t2, t_t2) = wk
            fw.act(sq[:, 0:nb], pb[:, 0:nb], AF.Square, R=[pt], W=[t_sq])
            pb2, pt2 = ring.next()
            fw.mm(pb2[:, 0:nb], bones, sq[:, 0:nb], R=[t_sq, t_c], W=[pt2])
            rms_rstd(pb2, pt2, nb, 1.0 / 64.0, rstd, t_rstd)
            fw.stt("dve", kn[:, 0:nb], pb[:, 0:nb], gain, rstd[:, 0:nb], ALU.mult, ALU.mult, R=[pt, t_pv, t_rstd], W=[t_kn])
            pb3, pt3 = ring.next()
            fw.mm(pb3[:, 0:nb], permG, kn[:, 0:nb], R=[t_kn, t_c], W=[pt3])
            fw.dma("sp", cs[:, :, 0:nb], dr["ropeG"][:, :, t0:t0 + nb].rearrange("c p t -> p c t"), W=[t_cs])
            fw.tt("dve", t1[:, 0:nb], kn[:, 0:nb], cs[:, 0, 0:nb], ALU.mult, R=[t_kn, t_cs], W=[t_t1])
            fw.tt("dve", t2[:, 0:nb], pb3[:, 0:nb], cs[:, 1, 0:nb], ALU.mult, R=[pt3, t_cs], W=[t_t2])
            if isinstance(dst, list):
                for (dap, p0, p1) in dst:
                    fw.tt("pool", dap, t1[p0:p1, 0:nb], t2[p0:p1, 0:nb], ALU.add, R=[t_t1, t_t2], W=[dst_t])
            else:
                fw.tt("pool", dst, t1[:, 0:nb], t2[:, 0:nb], ALU.add, R=[t_t1, t_t2], W=[dst_t])
        def rope_m(src, t_src, nb, t0, dst, dst_t, wk, dst_parts=(0, 96), rg=None):
            (cs, t_cs), (t1, t_t1), (t2, t_t2) = wk
            pb3, pt3 = (rg or ring).next()
            fw.mm(pb3[0:96, 0:nb], permM, src[0:96, 0:nb], R=[t_src, t_c], W=[pt3])
            fw.dma("sp", cs[0:96, :, 0:nb], dr["ropeM"][:, :, t0:t0 + nb].rearrange("c p t -> p c t"), W=[t_cs])
            fw.tt("dve", t1[0:96, 0:nb], src[0:96, 0:nb], cs[0:96, 0, 0:nb], ALU.mult, R=[t_src, t_cs], W=[t_t1])
            fw.tt("dve", t2[0:96, 0:nb], pb3[0:96, 0:nb], cs[0:96, 1, 0:nb], ALU.mult, R=[pt3, t_cs], W=[t_t2])
            p0, p1 = dst_parts
            fw.tt("pool", dst, t1[p0:p1, 0:nb], t2[p0:p1, 0:nb], ALU.add, R=[t_t1, t_t2], W=[dst_t])
        def mk_work():
            sq = ar.alloc([3, 512], F32); rstd = ar.alloc([512], F32); kn = ar.alloc([512], BF16)
            cs = ar.alloc([2, 512], F32)
            t_cs = Trk(); t_sq = Trk()
            return dict(sq=(sq, t_sq), rstd=(rstd, Trk()), kn=(kn, Trk()), cs=(cs, t_cs), t1=(sq[:, 1, :], t_sq), t2=(sq[:, 2, :], t_sq),
                        csm=(cs, t_cs))
        def phase_kv(l, kv):
            m = ar.mark()
            kTg, Vg, kTm, Vm, t_kTg, t_Vg, t_kTm, t_Vm = kv
            wk_ = ar.alloc([KC, 128], BF16); wv_ = ar.alloc([KC, 128], BF16); wc_ = ar.alloc([KC, 256], BF16)
            wr_ = ar.alloc([KC, 96], BF16); wkvb = ar.alloc([2, 1024], BF16)
            t_wk, t_wv, t_wc, t_wr, t_wkvb = wt("wk"), wt("wv"), wt("wc"), wt("wr"), wt("wkvb")
            fw.memset("pool", wr_, 0.0, W=[t_wr])
            load_w(wk_, win(l, C_K, C_K + 128), t_wk)
            load_w(wv_, win(l, C_V, C_V + 128), t_wv)
            load_w(wc_, win(l, C_CKV, C_CKV + 256), t_wc)
            load_w(wr_[:, :, 64:96], win(l, C_KR, C_KR + 32), t_wr)
            load_w(wkvb, dr["w_kvb"][l].rearrange("(j p) n -> p j n", p=128), t_wkvb)
            fw.memset("pool", Vg[:, :, :, 64:65], 1.0, W=[t_Vg])
            fw.memset("pool", Vm[:, :, :, 64:65], 1.0, W=[t_Vm])
            W_ = mk_work()
            ckvT = ar.alloc([2, 512], BF16); t_ckv = Trk()
            krs = ar.alloc([512], BF16); t_krs = Trk()
            krr = ar.alloc([512], BF16); t_krr = Trk()
            sq, t_sq = W_["sq"]; rstd, t_rstd = W_["rstd"]
            for bi, (t0, nb) in enumerate(BLOCKS):
                pb, pt = ring.next()
                for k in range(KC):
                    fw.mm(pb[:, 0:nb], wk_[:, k, :], hT[:, k, t0:t0 + nb], start=(k == 0), stop=(k == KC - 1),
                          R=[t_wk, t_h[bi]], W=[pt])
                norm_rope(pb, pt, nb, t0, pvc(("gk", l)), kTg[:, t0:t0 + nb], t_kTg,
                          ((sq[:, 0, :], t_sq), W_["rstd"], W_["kn"], W_["cs"], W_["t1"], W_["t2"]))
                for ti in range(nb // 128):
                    tt_ = t0 // 128 + ti
                    pb, pt = ring.next()
                    for k in range(KC):
                        fw.mm(pb[:, 0:128], hT[:, k, tt_ * 128:(tt_ + 1) * 128], wv_[:, k, :], start=(k == 0), stop=(k == KC - 1),
                              R=[t_wv, t_h[bi]], W=[pt])
                    fw.copy("act", Vg[:, tt_, :, 0:64], pb[:, 0:128].rearrange("p (h d) -> p h d", h=2), R=[pt], W=[t_Vg])
                pbs = [ring.next() for _ in range(2)]
                for j in range(2):
                    for k in range(KC):
                        fw.mm(pbs[j][0][:, 0:nb], wc_[:, k, j * 128:(j + 1) * 128], hT[:, k, t0:t0 + nb], start=(k == 0),
                              stop=(k == KC - 1), R=[t_wc, t_h[bi]], W=[pbs[j][1]])
                    fw.act(sq[:, j, 0:nb], pbs[j][0][:, 0:nb], AF.Square, R=[pbs[j][1]], W=[t_sq])
                pb2, pt2 = ring.next()
                for j in range(2):
                    fw.mm(pb2[:, 0:nb], ones32, sq[:, j, 0:nb], start=(j == 0), stop=(j == 1), R=[t_sq, t_c], W=[pt2])
                rms_rstd(pb2, pt2, nb, 1.0 / 256.0, rstd, t_rstd)
                for j in range(2):
                    fw.stt("dve", ckvT[:, j, 0:nb], pbs[j][0][:, 0:nb], pvc(("gckv", l), j), rstd[:, 0:nb], ALU.mult, ALU.mult,
                           R=[pbs[j][1], t_pv, t_rstd], W=[t_ckv])
                for h in range(8):
                    pb, pt = ring.next()
                    for j in range(2):
                        fw.mm(pb[0:64, 0:nb], wkvb[:, j, h * 64:(h + 1) * 64], ckvT[:, j, 0:nb], start=(j == 0), stop=(j == 1),
                              R=[t_wkvb, t_ckv], W=[pt])
                    fw.copy("act" if h % 2 else "dve", kTm[0:64, h, t0:t0 + nb], pb[0:64, 0:nb], R=[pt], W=[t_kTm])
                for ti in range(nb // 128):
                    tt_ = t0 // 128 + ti
                    pb, pt = ring.next()
                    for j in range(2):
                        fw.mm(pb[:, 0:512], ckvT[:, j, ti * 128:(ti + 1) * 128], wkvb[:, j, 512:1024], start=(j == 0), stop=(j == 1),
                              R=[t_wkvb, t_ckv], W=[pt])
                    fw.copy("act" if ti % 2 else "dve", Vm[:, tt_, :, 0:64], pb[:, 0:512].rearrange("p (h d) -> p h d", h=8),
                            R=[pt], W=[t_Vm])
                pb, pt = ring.next()
                for k in range(KC):
                    fw.mm(pb[0:96, 0:nb], wr_[:, k, :], hT[:, k, t0:t0 + nb], start=(k == 0), stop=(k == KC - 1),
                          R=[t_wr, t_h[bi]], W=[pt])
                fw.copy("act", krs[0:96, 0:nb], pb[0:96, 0:nb], R=[pt], W=[t_krs])
                rope_m(krs, t_krs, nb, t0, krr[64:96, 0:nb], t_krr, (W_["csm"], W_["t1"], W_["t2"]), dst_parts=(64, 96))
                for h in range(8):
                    fw.copy("pool" if h % 2 else "dve", kTm[64:96, h, t0:t0 + nb], krr[64:96, 0:nb], R=[t_krr], W=[t_kTm])
            fw.barrier()
            ar.release(m)
        def phase_attn(l, blocks, kv, o_attn, t_oattn, o_mla, t_omla):
            m = ar.mark()
            kTg, Vg, kTm, Vm, t_kTg, t_Vg, t_kTm, t_Vm = kv
            wq = ar.alloc([KC, 512], BF16); wcq = ar.alloc([KC, 384], BF16); wqb = ar.alloc([3, 768], BF16)
            t_wq, t_wcq, t_wqb = wt("wq"), wt("wcq"), wt("wqb")
            load_w(wq, win(l, C_Q, C_Q + 512), t_wq)
            load_w(wcq, win(l, C_CQ, C_CQ + 384), t_wcq)
            load_w(wqb, dr["w_qb"][l].rearrange("(j p) n -> p j n", p=128), t_wqb)
            W_ = mk_work()
            sq, t_sq = W_["sq"]; rstd, t_rstd = W_["rstd"]
            qTg = ar.alloc([8, 512], BF16); t_qTg = [Trk() for _ in range(4)]
            for j in range(4):
                fw.memset("pool", qTg[:, 2 * j:2 * j + 2, :], 0.0, W=[t_qTg[j]])
            cqT = ar.alloc([3, 512], BF16); t_cq = Trk()
            qraw = ar.alloc([512], BF16); t_qraw = Trk()
            qm = [ar.alloc([512], BF16) for _ in range(2)]; t_qm = [Trk() for _ in range(2)]
            pT = [ar.alloc([2, 512], BF16) for _ in range(2)]; t_pT = [Trk() for _ in range(2)]
            osb, t_osb = W_["rstd"]
            spair = [(pp_[j][:, :].rearrange("p (a b) -> p a b", a=2), (pst[2 * j], pst[2 * j + 1])) for j in range(2)]
            misc = Ring([(psb[4], pst[4]), (psb[5], pst[5])])
            for bi in blocks:
                t0, nb = BLOCKS[bi]
                ktiles = list(range(2)) if bi == 0 else list(range(NT))
                nk = len(ktiles)
                for j in range(4):
                    pb, pt = ring.next()
                    for k in range(KC):
                        fw.mm(pb[:, 0:nb], wq[:, k, j * 128:(j + 1) * 128], hT[:, k, t0:t0 + nb], start=(k == 0), stop=(k == KC - 1),
                              R=[t_wq, t_h[bi]], W=[pt])
                    norm_rope(pb, pt, nb, t0, pvc(("gq", l)),
                              [(qTg[0:64, 2 * j, 0:nb], 0, 64), (qTg[64:128, 2 * j + 1, 0:nb], 64, 128)], t_qTg[j],
                              ((sq[:, 0, :], t_sq), W_["rstd"], W_["kn"], W_["cs"], W_["t1"], W_["t2"]))
                pbs = [ring.next() for _ in range(3)]
                for j in range(3):
                    for k in range(KC):
                        fw.mm(pbs[j][0][:, 0:nb], wcq[:, k, j * 128:(j + 1) * 128], hT[:, k, t0:t0 + nb], start=(k == 0),
                              stop=(k == KC - 1), R=[t_wcq, t_h[bi]], W=[pbs[j][1]])
                    fw.act(sq[:, j, 0:nb], pbs[j][0][:, 0:nb], AF.Square, R=[pbs[j][1]], W=[t_sq])
                pb2, pt2 = ring.next()
                for j in range(3):
                    fw.mm(pb2[:, 0:nb], ones32, sq[:, j, 0:nb], start=(j == 0), stop=(j == 2), R=[t_sq, t_c], W=[pt2])
                rms_rstd(pb2, pt2, nb, 1.0 / 384.0, rstd, t_rstd)
                for j in range(3):
                    fw.stt("dve", cqT[:, j, 0:nb], pbs[j][0][:, 0:nb], pvc(("gcq", l), j), rstd[:, 0:nb], ALU.mult, ALU.mult,
                           R=[pbs[j][1], t_pv, t_rstd], W=[t_cq])
                jobs = []
                for j in range(4):
                    for half in range(2):
                        hd = j + 4 * half
                        p0 = 64 * half
                        jobs.append(dict(
                            mla=None,
                            k_of=(lambda kt: kTg[:, kt * 128:(kt + 1) * 128]),
                            q_ap=qTg[:, 2 * j + half, 0:nb],
                            v_of=(lambda kt, half=half: Vg[:, kt, half, 0:65]),
                            scale=0.125,
                            dst=o_attn[64 * (hd % 2):64 * (hd % 2) + 64, hd // 2, t0:t0 + nb], dst_t=t_oattn,
                            Rk=[t_kTg], Rq=[t_qTg[j]], Rv=[t_Vg]))
                for h in range(8):
                    s = h % 2
                    jobs.append(dict(
                        mla=h,
                        k_of=(lambda kt, h=h: kTm[0:96, h, kt * 128:(kt + 1) * 128]),
                        q_ap=qm[s][0:96, 0:nb],
                        v_of=(lambda kt, h=h: Vm[:, kt, h, 0:65]),
                        scale=96.0 ** -0.5,
                        dst=o_mla[64 * (h % 2):64 * (h % 2) + 64, h // 2, t0:t0 + nb], dst_t=t_omla,
                        Rk=[t_kTm], Rq=[t_qm[s]], Rv=[t_Vm]))

                def prep_a(job):
                    h = job["mla"]
                    if h is None:
                        return
                    pb, pt = misc.next()
                    for j in range(3):
                        fw.mm(pb[0:96, 0:nb], wqb[:, j, h * 96:(h + 1) * 96], cqT[:, j, 0:nb], start=(j == 0), stop=(j == 2),
                              R=[t_wqb, t_cq], W=[pt])
                    fw.copy("dve", qraw[0:96, 0:nb], pb[0:96, 0:nb], R=[pt], W=[t_qraw])

                def prep_b(job):
                    h = job["mla"]
                    if h is None:
                        return
                    s = h % 2
                    rope_m(qraw, t_qraw, nb, t0, qm[s][0:96, 0:nb], t_qm[s], (W_["csm"], W_["t1"], W_["t2"]), rg=misc)

                def emit_S(job, kt):
                    pb, pt = misc.next()
                    fw.mm(pb[:, 0:nb], job["k_of"](kt), job["q_ap"], R=job["Rk"] + job["Rq"], W=[pt])
                    return pb, pt

                def finish_a(job, acc, acct):
                    fw.copy("dve", osb[0:65, 0:nb], acc[0:65, 0:nb], R=[acct], W=[t_osb])
                    fw.recip(osb[64:65, 0:nb], osb[64:65, 0:nb], R=[t_osb], W=[t_osb])

                def finish_b(job):
                    pb, pt = misc.next()
                    fw.mm(pb[0:64, 0:nb], sel64, osb[:, 0:nb], R=[t_osb, t_c], W=[pt])
                    fw.tt("dve", job["dst"], osb[0:64, 0:nb], pb[0:64, 0:nb], ALU.mult, R=[t_osb, pt], W=[job["dst_t"]])

                npair = nk // 2
                flat = [(ji, pi) for ji in range(len(jobs)) for pi in range(npair)]
                done_a = set(); done_b = set()

                def ensure_prep(jx, upto_b=True):
                    if jx not in done_a:
                        prep_a(jobs[jx]); done_a.add(jx)
                    if upto_b and jx not in done_b:
                        prep_b(jobs[jx]); done_b.add(jx)

                def emit_Sp(job, pi, slot):
                    pv, trks = spair[slot]
                    for h2 in range(2):
                        kt = ktiles[2 * pi + h2]
                        fw.mm(pv[:, h2, 0:nb], job["k_of"](kt), job["q_ap"], R=job["Rk"] + job["Rq"], W=[trks[h2]])

                ensure_prep(0)
                emit_Sp(jobs[0], 0, 0)
                pend_fin = None
                acc = acct = None
                kb = min(4, npair - 1)
                fb = min(5, npair - 1)
                for i, (ji, pi) in enumerate(flat):
                    job = jobs[ji]
                    if pi == 0:
                        acc, acct = accr.next()
                        if ji + 1 < len(jobs):
                            ensure_prep(ji + 1, upto_b=False)
                    if pi == kb and ji + 1 < len(jobs):
                        ensure_prep(ji + 1)
                    if i + 1 < len(flat):
                        ji2, pi2 = flat[i + 1]
                        ensure_prep(ji2)
                        emit_Sp(jobs[ji2], pi2, (i + 1) % 2)
                    pv, trks = spair[i % 2]
                    s = i % 2
                    fw.act(pT[s][:, :, 0:nb], pv[:, :, 0:nb], AF.Exp, scale=job["scale"], R=list(trks), W=[t_pT[s]])
                    for h2 in range(2):
                        kt = ktiles[2 * pi + h2]
                        fw.mm(acc[0:65, 0:nb], job["v_of"](kt), pT[s][:, h2, 0:nb], start=(pi == 0 and h2 == 0),
                              stop=(pi == npair - 1 and h2 == 1), R=job["Rv"] + [t_pT[s]], W=[acct])
                    if pi == fb and pend_fin is not None:
                        finish_b(pend_fin)
                        pend_fin = None
                    if pi == npair - 1:
                        if pend_fin is not None:
                            finish_b(pend_fin)
                        finish_a(job, acc, acct)
                        pend_fin = job
                if pend_fin is not None:
                    finish_b(pend_fin)
            fw.barrier()
            ar.release(m)
        def load_wo(l, wo):
            wsrc = dr["w_out"][l].rearrange("(k p) n -> p k n", p=128)
            load_w(wo[:, :, 0:512], wsrc[:, :, 0:512], wt("wo0"))
            load_w(wo[:, :, 512:1024], wsrc[:, :, 512:1024], wt("wo1"))

        def phase_merge(l, blocks, outs, merged, t_mg, wo):
            m = ar.mark()
            wg = ar.alloc([3, KC, 512], BF16); wb = ar.alloc([3, 4, 512], BF16)
            sg = [ar.alloc([512], F32) for _ in range(2)]; t_sg = [Trk() for _ in range(2)]
            mm_ = ar.alloc([512], F32); t_mm = Trk()
            tt2 = ar.alloc([512], F32); t_tt2 = Trk()
            wnames = ("w_ba", "w_bl", "w_bm")
            t_wg = [wt("wg%d" % i) for i in range(3)]; t_wb = [wt("wb%d" % i) for i in range(3)]
            for half in range(2):
                for br in range(3):
                    c0 = C_G + br * 1024 + half * 512
                    load_w(wg[:, br, :, :], win(l, c0, c0 + 512), t_wg[br])
                    load_w(wb[:, br, :, :], dr[wnames[br]][l].rearrange("(j p) n -> p j n", p=128)[:, :, half * 512:(half + 1) * 512], t_wb[br])
                if half == 1:
                    load_wo(l, wo)
                for bi in blocks:
                    t0, nb = BLOCKS[bi]
                    for ff in range(4):
                        f = half * 4 + ff
                        for br in range(3):
                            o_br, t_obr = outs[br]
                            pg_, ptg = ring.next()
                            for k in range(KC):
                                fw.mm(pg_[:, 0:nb], wg[:, br, k, ff * 128:(ff + 1) * 128], hT[:, k, t0:t0 + nb], start=(k == 0),
                                      stop=(k == KC - 1), R=[t_wg[br], t_h[bi]], W=[ptg])
                            s = br % 2
                            fw.act(sg[s][:, 0:nb], pg_[:, 0:nb], AF.Sigmoid, R=[ptg], W=[t_sg[s]])
                            pb, pt = ring.next()
                            for j in range(4):
                                fw.mm(pb[:, 0:nb], wb[:, br, j, ff * 128:(ff + 1) * 128], o_br[:, j, t0:t0 + nb], start=(j == 0),
                                      stop=(j == 3), R=[t_wb[br], t_obr], W=[pt])
                            if br == 0:
                                fw.tt("dve", mm_[:, 0:nb], sg[s][:, 0:nb], pb[:, 0:nb], ALU.mult, R=[t_sg[s], pt], W=[t_mm])
                            else:
                                fw.tt("dve", tt2[:, 0:nb], sg[s][:, 0:nb], pb[:, 0:nb], ALU.mult, R=[t_sg[s], pt], W=[t_tt2])
                                if br == 1:
                                    fw.tt("dve", mm_[:, 0:nb], mm_[:, 0:nb], tt2[:, 0:nb], ALU.add, R=[t_mm, t_tt2], W=[t_mm])
                                else:
                                    fw.tt("dve", merged[:, f, t0:t0 + nb], mm_[:, 0:nb], tt2[:, 0:nb], ALU.add,
                                          R=[t_mm, t_tt2], W=[t_mg[bi]])
            fw.barrier()
            ar.release(m)
        def phase_wout(l, blocks, merged, t_mg, wo):
            m = ar.mark()
            t_wo = [wt("wo0"), wt("wo1")]
            xb = [ar.alloc([KC, 512], F32) for _ in range(2)]; t_xb = [Trk() for _ in range(2)]
            for bi in blocks:
                t0, nb = BLOCKS[bi]
                s = blk_s(bi)
                bs = bi % 2
                fw.dma("sp", xb[bs][:, :, 0:nb], xs_d[:, :, t0:t0 + nb], R=[xs_t[bi]], W=[t_xb[bs]], key=t_xb[bs])
                for f in range(KC):
                    pb, pt = ring.next()
                    for k in range(KC):
                        fw.mm(pb[:, 0:nb], wo[:, k, f * 128:(f + 1) * 128], merged[:, k, t0:t0 + nb], start=(k == 0),
                              stop=(k == KC - 1), R=[t_wo[f // 4], t_mg[bi]], W=[pt])
                    fw.stt("dve", xb[bs][:, f, 0:nb], pb[:, 0:nb], der[:, 2, 2 * f + s:2 * f + s + 1], xb[bs][:, f, 0:nb],
                           ALU.mult, ALU.add, R=[pt, t_der, t_xb[bs]], W=[t_xb[bs]])
                fw.dma("sp", xs_d[:, :, t0:t0 + nb], xb[bs][:, :, 0:nb], R=[t_xb[bs]], W=[xs_t[bi]], key=t_xb[bs])
            fw.barrier()
            ar.release(m)
        def moe_weights():
            moe_weights.base = ar.top
            wgu = [ar.alloc([KC, 1024], BF16) for _ in range(2)]
            wdn = [ar.alloc([4, D], BF16) for _ in range(2)]
            t_we = [[wt("we%d_%d" % (s_, i)) for i in range(2)] for s_ in range(2)]
            return wgu, wdn, t_we

        def phase_moe(l, blocks, combT, t_comb, yacc, t_y, moew):
            m = ar.mark()
            selE = ar.alloc([NE, 128], F32, parts=NE); t_sel = Trk()
            fw.dma("sp", selE, dr["selE"].rearrange("k (e m) -> k e m", e=NE), W=[t_sel])
            wgu, wdn, t_we = moew
            cb = ar.alloc([512], F32); t_cb = Trk()
            ss = [ar.alloc([512], F32) for _ in range(2)]; t_ss = [Trk() for _ in range(2)]
            tu = [ar.alloc([512], F32) for _ in range(2)]; t_tu = [Trk() for _ in range(2)]
            actT = [ar.alloc([4, 512], BF16) for _ in range(2)]; t_act = [Trk() for _ in range(2)]
            def load_e(e):
                s = e % 2
                load_w(wgu[s], dr["wgu"][l, e].rearrange("(k p) n -> p k n", p=128), t_we[s][0])
                load_w(wdn[s], dr["wd"][l, e].rearrange("(j p) n -> p j n", p=128), t_we[s][1])
            def emit_gu(e, bi, idx):
                s = e % 2
                t0, nb = BLOCKS[bi]
                pb, pt = ring.next()
                fw.mm(pb[:, 0:nb], selE[0:NE, e, :], combT[0:NE, t0:t0 + nb], R=[t_sel, t_comb], W=[pt])
                fw.copy("act", cb[:, 0:nb], pb[:, 0:nb], R=[pt], W=[t_cb])
                a_s = idx % 2
                for ff in range(4):
                    pg_, ptg = ring.next()
                    for k in range(KC):
                        fw.mm(pg_[:, 0:nb], wgu[s][:, k, ff * 128:(ff + 1) * 128], hT[:, k, t0:t0 + nb], start=(k == 0),
                              stop=(k == KC - 1), R=[t_we[s][0], t_h[bi]], W=[ptg])
                    pu_, ptu = ring.next()
                    for k in range(KC):
                        fw.mm(pu_[:, 0:nb], wgu[s][:, k, 512 + ff * 128:512 + (ff + 1) * 128], hT[:, k, t0:t0 + nb], start=(k == 0),
                              stop=(k == KC - 1), R=[t_we[s][0], t_h[bi]], W=[ptu])
                    q = ff % 2
                    fw.act(ss[q][:, 0:nb], pg_[:, 0:nb], AF.Silu, R=[ptg], W=[t_ss[q]])
                    fw.tt("dve", tu[q][:, 0:nb], ss[q][:, 0:nb], pu_[:, 0:nb], ALU.mult, R=[t_ss[q], ptu], W=[t_tu[q]])
                    fw.tt("pool", actT[a_s][:, ff, 0:nb], tu[q][:, 0:nb], cb[:, 0:nb], ALU.mult, R=[t_tu[q], t_cb], W=[t_act[a_s]])

            def emit_down(e, bi, idx):
                s = e % 2
                t0, nb = BLOCKS[bi]
                a_s = idx % 2
                for f in range(KC):
                    pb, pt = ring.next()
                    for j in range(4):
                        fw.mm(pb[:, 0:nb], wdn[s][:, j, f * 128:(f + 1) * 128], actT[a_s][:, j, 0:nb], start=(j == 0),
                              stop=(j == 3), R=[t_we[s][1], t_act[a_s]], W=[pt])
                    if e == 0:
                        fw.copy("act", yacc[:, f, t0:t0 + nb], pb[:, 0:nb], R=[pt], W=[t_y[bi]])
                    else:
                        fw.tt("dve", yacc[:, f, t0:t0 + nb], yacc[:, f, t0:t0 + nb], pb[:, 0:nb], ALU.add,
                              R=[pt, t_y[bi]], W=[t_y[bi]])

            items = [(e, bi) for e in range(NE) for bi in blocks]
            prev = None
            for idx, (e, bi) in enumerate(items):
                emit_gu(e, bi, idx)
                if prev is not None:
                    emit_down(*prev)
                if bi == blocks[0] and e + 1 < NE:
                    load_e(e + 1)
                prev = (e, bi, idx)
            emit_down(*prev)
            fw.barrier()
            ar.release(m)
        def phase_ffn_res(l, blocks, yacc, t_y, last):
            m = ar.mark()
            save_top = ar.top
            ar.top = moe_weights.base
            xb = [ar.alloc([KC, 512], F32) for _ in range(2)]; t_xb = [Trk() for _ in range(2)]
            if last:
                sq = ar.alloc([KC, 512], F32); t_sq = Trk()
            assert ar.top <= moe_weights.base + 12288
            ar.top = save_top
            if last:
                rstd = ar.alloc([512], F32); t_rstd = Trk()
                ot = [ar.alloc([D], F32) for _ in range(2)]; t_ot = [Trk() for _ in range(2)]
            oi = 0
            for bi in blocks:
                t0, nb = BLOCKS[bi]
                s = blk_s(bi)
                bs = bi % 2
                fw.dma("sp", xb[bs][:, :, 0:nb], xs_d[:, :, t0:t0 + nb], R=[xs_t[bi]], W=[t_xb[bs]], key=t_xb[bs])
                for f in range(KC):
                    fw.stt("dve", xb[bs][:, f, 0:nb], yacc[:, f, t0:t0 + nb], der[:, 5, 2 * f + s:2 * f + s + 1], xb[bs][:, f, 0:nb],
                           ALU.mult, ALU.add, R=[t_y[bi], t_der, t_xb[bs]], W=[t_xb[bs]])
                if not last:
                    fw.dma("sp", xs_d[:, :, t0:t0 + nb], xb[bs][:, :, 0:nb], R=[t_xb[bs]], W=[xs_t[bi]], key=t_xb[bs])
                    continue
                fw.act(sq[:, :, 0:nb], xb[bs][:, :, 0:nb], AF.Square, R=[t_xb[bs]], W=[t_sq])
                pb, pt = ring.next()
                for k in range(KC):
                    fw.mm(pb[:, 0:nb], ones32, sq[:, k, 0:nb], start=(k == 0), stop=(k == KC - 1), R=[t_sq, t_c], W=[pt])
                rms_rstd(pb, pt, nb, 1.0 / D, rstd, t_rstd)
                for k in range(KC):
                    fw.stt("dve", xb[bs][:, k, 0:nb], xb[bs][:, k, 0:nb], pvc("final_norm", k), rstd[:, 0:nb], ALU.mult, ALU.mult,
                           R=[t_xb[bs], t_pv, t_rstd], W=[t_xb[bs]])
                for ti in range(nb // 128):
                    os_ = oi % 2
                    oi += 1
                    for hf in range(2):
                        pb, pt = ring.next()
                        for kk in range(4):
                            k = hf * 4 + kk
                            fw.tr(pb[:, kk * 128:(kk + 1) * 128], xb[bs][:, k, ti * 128:(ti + 1) * 128], ident,
                                  R=[t_xb[bs], t_c], W=[pt])
                        fw.copy("act" if hf == 0 else "dve", ot[os_][:, hf * 512:(hf + 1) * 512], pb[:, 0:512], R=[pt], W=[t_ot[os_]])
                    r0 = t0 - NCTX + ti * 128
                    fw.dma("sp", out_d[r0:r0 + 128, :], ot[os_], R=[t_ot[os_]], W=[t_out], key=t_ot[os_])
            fw.barrier()
            ar.release(m)
        def ck(name, bufs):
            if stop != name:
                return
            fw.barrier()
            for key, ap in bufs.items():
                if key not in dumps:
                    continue
                d = nc.dram_tensor("dbg_" + key, list(ap.shape), F32, kind="ExternalOutput").ap()
                fw.dma("pool", d, ap, W=[Trk()])
            raise _Stop()
        ALLB = [0, 1, 2, 3, 4]
        LATB = [1, 2, 3, 4]
        try:
            phase_load()
            ck("load", dict(xs=xs_d))
            for l in range(depth):
                last = (l == DEPTH - 1)
                qblocks = LATB if last else ALLB
                lm = ar.mark()
                o_lru = ar.alloc([4, T], BF16); t_olru = Trk()
                om = ar.mark()
                lruw = lru_weights(l)
                phase_mod(l)
                ck("mod%d" % l, dict(mod=modsb, der=der, cneg=cneg))
                phase_norm(l, 1, ALLB)
                ck("norm%d" % l, dict(hT=hT))
                phase_lru(l, o_lru, t_olru, lruw)
                ck("lru%d" % l, dict(o_lru=o_lru))
                ar.release(om)
                o_attn = ar.alloc([4, T], BF16); t_oattn = Trk()
                o_mla = ar.alloc([4, T], BF16); t_omla = Trk()
                kvm = ar.mark()
                kTg = ar.alloc([T], BF16); Vg = ar.alloc([NT, 2, 65], BF16)
                kTm = ar.alloc([8, T], BF16, parts=96); Vm = ar.alloc([NT, 8, 65], BF16)
                kv = (kTg, Vg, kTm, Vm, Trk(), Trk(), Trk(), Trk())
                phase_kv(l, kv)
                ck("kv%d" % l, dict(kTg=kTg, kTm=kTm, Vg=Vg, Vm=Vm))
                phase_attn(l, qblocks, kv, o_attn, t_oattn, o_mla, t_omla)
                ck("attn%d" % l, dict(o_attn=o_attn, o_mla=o_mla))
                ar.release(kvm)
                merged = ar.alloc([KC, T], BF16); t_mg = [Trk() for _ in range(5)]
                wo = ar.alloc([KC, D], BF16)
                phase_merge(l, qblocks, ((o_attn, t_oattn), (o_lru, t_olru), (o_mla, t_omla)), merged, t_mg, wo)
                ck("merge%d" % l, dict(merged=merged))
                phase_wout(l, qblocks, merged, t_mg, wo)
                ck("wout%d" % l, dict(xs=xs_d))
                ar.release(lm)
                combT = ar.alloc([T], F32, parts=NE); t_comb = Trk()
                moew = moe_weights()
                _wgu, _wdn, _twe = moew
                hooks = {
                    0: (lambda l=l: load_w(_wgu[0], dr["wgu"][l, 0].rearrange("(k p) n -> p k n", p=128), _twe[0][0])),
                    2: (lambda l=l: load_w(_wdn[0], dr["wd"][l, 0].rearrange("(j p) n -> p j n", p=128), _twe[0][1])),
                }
                phase_norm(l, 2, qblocks, combT, t_comb, hooks=hooks)
                ck("normf%d" % l, dict(hT=hT, combT=combT))
                yacc = ar.alloc([KC, T], F32); t_y = [Trk() for _ in range(5)]
                phase_moe(l, qblocks, combT, t_comb, yacc, t_y, moew)
                ck("moe%d" % l, dict(yacc=yacc))
                phase_ffn_res(l, qblocks, yacc, t_y, last)
                ck("res%d" % l, dict(xs=xs_d))
                ar.release(lm)
        except _Stop:
            pass
        fw.wait_all("sp", [t_out] + xs_t)
        fw.barrier()
        fw.emit()
        build_nc.stats = (fw.ninst, fw.nwait, len(fw.dsems), ar.peak)
    return nc

_CACHE = {}

def kernel(**inputs):
    shared, per_core = _prepare(inputs)
    if "nc" not in _CACHE:
        _CACHE["nc"] = build_nc()
    nc = _CACHE["nc"]
    in_maps = []
    for pc in per_core:
        d = dict(shared)
        d.update(pc)
        in_maps.append(d)
    res = run_bass_kernel_spmd(nc, in_maps, core_ids=list(range(len(in_maps))))
    out = np.stack([np.asarray(r["out"], np.float32) for r in res.results], 0)
    return out
```
